# Optimizing a Trainium2 kernel written in Bass

```python
import jax, jax.numpy as jnp
from jax import lax
import numpy as np

D_MODEL = 1024
BATCH = 8
SEQ = 4096
DEPTH = 2

HEAD_DIM = 64
N_HEADS_FOX = 6
N_HEADS_SB = 6
N_HEADS_DSA = 4
N_IDX_HEADS = 4
IDX_DIM = 64
TOPK_MAX = 256
D_FF = 2816
ROPE_THETA = 500000.0
ROT_DIM = HEAD_DIM // 4
Q_BLOCK = 128
EPS = 1e-6
N_BRANCHES = 3

W_FOX = N_HEADS_FOX * HEAD_DIM
W_SB = N_HEADS_SB * HEAD_DIM
W_DSA = N_HEADS_DSA * HEAD_DIM
IN_SPLITS = (W_FOX, W_FOX, W_FOX, N_HEADS_FOX,
             W_SB, W_SB, W_SB,
             W_DSA, W_DSA, W_DSA,
             N_IDX_HEADS * IDX_DIM, IDX_DIM, N_IDX_HEADS,
             D_MODEL, D_MODEL, D_MODEL)
N_IN = sum(IN_SPLITS)

kernel_name = "hybrid_fox_stickbreak_dsa_macaron"


def _rmsnorm(x, g):
    x32 = x.astype(jnp.float32)
    y = x32 * lax.rsqrt(jnp.mean(x32 * x32, axis=-1, keepdims=True) + EPS)
    return (y * g.astype(jnp.float32)).astype(x.dtype)


def _rope_partial(x, positions):
    half = ROT_DIM // 2
    inv_freq = jnp.power(ROPE_THETA, -jnp.arange(half, dtype=jnp.float32) * 2.0 / ROT_DIM)
    ang = positions.astype(jnp.float32)[..., None] * inv_freq
    cos = jnp.cos(ang)[:, :, None, :]
    sin = jnp.sin(ang)[:, :, None, :]
    x32 = x.astype(jnp.float32)
    x1 = x32[..., :half]
    x2 = x32[..., half:ROT_DIM]
    out = jnp.concatenate([x1 * cos - x2 * sin, x2 * cos + x1 * sin, x32[..., ROT_DIM:]], axis=-1)
    return out.astype(x.dtype)


def _swiglu(h, w_gate, w_up, w_down):
    a = jnp.einsum('bsd,df->bsf', h, w_gate)
    u = jnp.einsum('bsd,df->bsf', h, w_up)
    return jnp.einsum('bsf,fd->bsd', jax.nn.silu(a) * u, w_down)


def _to_blocks(a):
    b, s = a.shape[:2]
    a = a.reshape((b, s // Q_BLOCK, Q_BLOCK) + a.shape[2:])
    return jnp.moveaxis(a, 1, 0)


def _from_blocks(a):
    a = jnp.moveaxis(a, 0, 1)
    return a.reshape((a.shape[0], a.shape[1] * a.shape[2]) + a.shape[3:])


def _query_pos_blocks(s):
    return jnp.arange(s).reshape(s // Q_BLOCK, Q_BLOCK)


def _forgetting_attention(q, k, v, log_f):
    s_len, dh = q.shape[1], q.shape[-1]
    scale = dh ** -0.5
    cum = jnp.cumsum(log_f, axis=1)
    cum_k = jnp.transpose(cum, (0, 2, 1))
    kpos = jnp.arange(s_len)

    def block(args):
        qb, cq, tq = args
        logits = jnp.einsum('bqhd,bkhd->bhqk', qb, k, preferred_element_type=jnp.float32) * scale
        logits = logits + jnp.transpose(cq, (0, 2, 1))[..., :, None] - cum_k[..., None, :]
        causal = kpos[None, :] <= tq[:, None]
        p = jax.nn.softmax(jnp.where(causal, logits, -jnp.inf), axis=-1)
        return jnp.einsum('bhqk,bkhd->bqhd', p.astype(v.dtype), v)

    out = lax.map(block, (_to_blocks(q), _to_blocks(cum), _query_pos_blocks(s_len)))
    return _from_blocks(out)


def _stick_breaking_attention(q, k, v):
    s_len, dh = q.shape[1], q.shape[-1]
    scale = dh ** -0.5
    kpos = jnp.arange(s_len)

    def block(args):
        qb, tq = args
        z = jnp.einsum('bqhd,bkhd->bhqk', qb, k, preferred_element_type=jnp.float32) * scale
        strict = kpos[None, :] < tq[:, None]
        log_one_minus = jnp.where(strict, jax.nn.log_sigmoid(-z), 0.0)
        suffix = lax.cumsum(log_one_minus, axis=3, reverse=True) - log_one_minus
        log_a = jax.nn.log_sigmoid(z) + suffix
        a = jnp.where(strict, jnp.exp(log_a), 0.0)
        return jnp.einsum('bhqk,bkhd->bqhd', a.astype(v.dtype), v)

    out = lax.map(block, (_to_blocks(q), _query_pos_blocks(s_len)))
    return _from_blocks(out)


def _dsa_attention(q, k, v, q_idx, k_idx, w_idx, topk):
    s_len, dh = q.shape[1], q.shape[-1]
    scale = dh ** -0.5
    idx_scale = IDX_DIM ** -0.5
    w_scale = N_IDX_HEADS ** -0.5
    kpos = jnp.arange(s_len)
    gather = jax.vmap(lambda tb, ib: tb[ib])

    def block(args):
        qb, qib, wib, tq = args
        dots = jnp.einsum('bqhd,bkd->bqhk', qib, k_idx, preferred_element_type=jnp.float32) * idx_scale
        score = jnp.einsum('bqh,bqhk->bqk', wib.astype(jnp.float32) * w_scale, jax.nn.relu(dots))
        causal = kpos[None, :] <= tq[:, None]
        score = jnp.where(causal[None], score, -jnp.inf)
        _, sel = lax.top_k(score, topk)
        valid = sel <= tq[None, :, None]
        k_sel = gather(k, sel)
        v_sel = gather(v, sel)
        logits = jnp.einsum('bqhd,bqkhd->bhqk', qb, k_sel, preferred_element_type=jnp.float32) * scale
        p = jax.nn.softmax(jnp.where(valid[:, None], logits, -jnp.inf), axis=-1)
        return jnp.einsum('bhqk,bqkhd->bqhd', p.astype(v.dtype), v_sel)

    out = lax.map(block, (_to_blocks(q), _to_blocks(q_idx), _to_blocks(w_idx), _query_pos_blocks(s_len)))
    return _from_blocks(out)


def _hybrid_mixer(h, positions, w_in, b_forget, b_gates, qn_f, kn_f, qn_s, kn_s, qn_c, kn_c,
                  w_br_f, w_br_s, w_br_c, w_out, topk):
    b, s, _ = h.shape
    offs = np.cumsum(IN_SPLITS)[:-1].tolist()
    z = jnp.einsum('bsd,dn->bsn', h, w_in)
    (qf, kf, vf, ff, qs, ks, vs, qc, kc, vc, qi, ki, wi, ga, gb, gc) = jnp.split(z, offs, axis=-1)

    def heads(t):
        return t.reshape(b, s, -1, HEAD_DIM)

    log_f = jax.nn.log_sigmoid(ff.astype(jnp.float32) + b_forget.astype(jnp.float32))
    o_f = _forgetting_attention(_rmsnorm(heads(qf), qn_f), _rmsnorm(heads(kf), kn_f), heads(vf), log_f)
    o_s = _stick_breaking_attention(_rmsnorm(heads(qs), qn_s), _rmsnorm(heads(ks), kn_s), heads(vs))
    q_c = _rope_partial(_rmsnorm(heads(qc), qn_c), positions)
    k_c = _rope_partial(_rmsnorm(heads(kc), kn_c), positions)
    q_i = _rope_partial(qi.reshape(b, s, N_IDX_HEADS, IDX_DIM), positions)
    k_i = _rope_partial(ki[:, :, None, :], positions)[:, :, 0]
    o_c = _dsa_attention(q_c, k_c, heads(vc), q_i, k_i, wi, topk)

    br_f = jnp.einsum('bsm,md->bsd', o_f.reshape(b, s, W_FOX), w_br_f)
    br_s = jnp.einsum('bsm,md->bsd', o_s.reshape(b, s, W_SB), w_br_s)
    br_c = jnp.einsum('bsm,md->bsd', o_c.reshape(b, s, W_DSA), w_br_c)
    merged = (jax.nn.sigmoid(ga + b_gates[0]) * br_f
              + jax.nn.sigmoid(gb + b_gates[1]) * br_s
              + jax.nn.sigmoid(gc + b_gates[2]) * br_c)
    return jnp.einsum('bsd,de->bse', merged, w_out)


def setup_inputs(seed: int = 0) -> dict:
    key = jax.random.key(seed)
    ks = jax.random.split(key, 24)
    f32 = jnp.float32

    def w(k, shape, fan_in):
        return jax.random.normal(k, shape, f32) * (fan_in ** -0.5)

    def gain(k, shape):
        return 1.0 + 0.02 * jax.random.normal(k, shape, f32)

    return {
        "x": jax.random.normal(ks[0], (BATCH, SEQ, D_MODEL), f32),
        "positions": jnp.broadcast_to(jnp.arange(SEQ, dtype=jnp.int32)[None, :], (BATCH, SEQ)),
        "ffn1_norm": gain(ks[1], (DEPTH, D_MODEL)),
        "ffn1_w_gate": w(ks[2], (DEPTH, D_MODEL, D_FF), D_MODEL),
        "ffn1_w_up": w(ks[3], (DEPTH, D_MODEL, D_FF), D_MODEL),
        "ffn1_w_down": w(ks[4], (DEPTH, D_FF, D_MODEL), D_FF),
        "mix_norm": gain(ks[5], (DEPTH, D_MODEL)),
        "w_in": w(ks[6], (DEPTH, D_MODEL, N_IN), D_MODEL),
        "b_forget": 1.0 + 3.0 * jax.random.uniform(ks[7], (DEPTH, N_HEADS_FOX), f32),
        "b_gates": 0.02 * jax.random.normal(ks[8], (DEPTH, N_BRANCHES, D_MODEL), f32),
        "q_norm_fox": gain(ks[9], (DEPTH, HEAD_DIM)),
        "k_norm_fox": gain(ks[10], (DEPTH, HEAD_DIM)),
        "q_norm_sb": gain(ks[11], (DEPTH, HEAD_DIM)),
        "k_norm_sb": gain(ks[12], (DEPTH, HEAD_DIM)),
        "q_norm_dsa": gain(ks[13], (DEPTH, HEAD_DIM)),
        "k_norm_dsa": gain(ks[14], (DEPTH, HEAD_DIM)),
        "w_branch_fox": w(ks[15], (DEPTH, W_FOX, D_MODEL), W_FOX),
        "w_branch_sb": w(ks[16], (DEPTH, W_SB, D_MODEL), W_SB),
        "w_branch_dsa": w(ks[17], (DEPTH, W_DSA, D_MODEL), W_DSA),
        "w_out": w(ks[18], (DEPTH, D_MODEL, D_MODEL), D_MODEL),
        "ffn2_norm": gain(ks[19], (DEPTH, D_MODEL)),
        "ffn2_w_gate": w(ks[20], (DEPTH, D_MODEL, D_FF), D_MODEL),
        "ffn2_w_up": w(ks[21], (DEPTH, D_MODEL, D_FF), D_MODEL),
        "ffn2_w_down": w(ks[22], (DEPTH, D_FF, D_MODEL), D_FF),
    }


def reference(x, positions, ffn1_norm, ffn1_w_gate, ffn1_w_up, ffn1_w_down, mix_norm, w_in,
              b_forget, b_gates, q_norm_fox, k_norm_fox, q_norm_sb, k_norm_sb, q_norm_dsa, k_norm_dsa,
              w_branch_fox, w_branch_sb, w_branch_dsa, w_out, ffn2_norm, ffn2_w_gate, ffn2_w_up,
              ffn2_w_down):
    topk = min(TOPK_MAX, x.shape[1] // 4)
    for l in range(DEPTH):
        x = x + 0.5 * _swiglu(_rmsnorm(x, ffn1_norm[l]), ffn1_w_gate[l], ffn1_w_up[l], ffn1_w_down[l])
        x = x + _hybrid_mixer(_rmsnorm(x, mix_norm[l]), positions, w_in[l], b_forget[l], b_gates[l],
                              q_norm_fox[l], k_norm_fox[l], q_norm_sb[l], k_norm_sb[l],
                              q_norm_dsa[l], k_norm_dsa[l], w_branch_fox[l], w_branch_sb[l],
                              w_branch_dsa[l], w_out[l], topk)
        x = x + 0.5 * _swiglu(_rmsnorm(x, ffn2_norm[l]), ffn2_w_gate[l], ffn2_w_up[l], ffn2_w_down[l])
    return x
```

```python
import numpy as np
import concourse.bass as bass
import concourse.mybir as mybir
from concourse.bass_utils import run_bass_kernel_spmd

F32 = mybir.dt.float32
BF16 = mybir.dt.bfloat16
I32 = mybir.dt.int32
AF = mybir.ActivationFunctionType
ALU = mybir.AluOpType
AX = mybir.AxisListType

D = 1024
S = 4096
DEPTH = 2
DFF = 2816
NFC = DFF // 128
NDC = D // 128
TT = 512
NTT = S // TT
NKT = S // 128
EPS = 1e-6
BIG = 30000.0
TOPK = 256
NBIS = 24

C_QF, C_KF, C_VF, C_FF = 0, 384, 768, 1152
C_QS, C_KS, C_VS = 1158, 1542, 1926
C_QC, C_KC, C_VC = 2310, 2566, 2822
C_QI, C_KI, C_WI = 3078, 3334, 3398
C_G = 3402
NIN = 6474


class Sched:
    ENG = ("pe", "act", "dve", "pool", "sp")

    def __init__(self, nc):
        self.nc = nc
        self._new_phase()
        self._reset()

    def _new_phase(self):
        self.known = {e: {} for e in self.ENG}
        self.chan_n = {}
        self.n = {e: 0 for e in self.ENG}
        self.sig_base = {e: 0 for e in self.ENG}
        self.sems = {}
        self.chan_sems = {}

    def _reset(self):
        self.ops = {e: [] for e in self.ENG}
        self.lastw = {}
        self.readers = {}
        self.signal = set()

    def add(self, eng, emit, reads=(), writes=(), dma=False, chan=None):
        deps = {}

        def need(d):
            for sk, o in d.items():
                if deps.get(sk, 0) < o:
                    deps[sk] = o

        for r in reads:
            need(self.lastw.get(r, {}))
        for w in writes:
            need(self.lastw.get(w, {}))
            need(self.readers.get(w, {}))
        if dma:
            self.chan_n[chan] = self.chan_n.get(chan, 0) + 1
            me = (("ch", chan), self.chan_n[chan])
        else:
            self.n[eng] += 1
            me = (eng, self.n[eng])
        waits = []
        kn = self.known[eng]
        for sk, o in deps.items():
            if sk == "pe" and eng == "pe" and not dma:
                continue
            if kn.get(sk, 0) >= o:
                continue
            kn[sk] = o
            waits.append((sk, o))
            if not isinstance(sk, tuple):
                self.signal.add((sk, o))
        for w in writes:
            self.lastw[w] = {me[0]: me[1]}
            self.readers[w] = {}
        for r in reads:
            rd = self.readers.setdefault(r, {})
            if rd.get(me[0], 0) < me[1]:
                rd[me[0]] = me[1]
        self.ops[eng].append(dict(emit=emit, waits=waits, me=me, dma=dma))
        return me

    def end_phase(self):
        nc = self.nc
        waits = []
        for chan, n in self.chan_n.items():
            sk = ("ch", chan)
            if self.known["sp"].get(sk, 0) < n:
                waits.append((sk, n))
                self.known["sp"][sk] = n
        self.ops["sp"].append(dict(emit=None, waits=waits, me=None, dma=False))
        self._pid = getattr(self, "_pid", 0) + 1
        snap = nc.snapshot_sems()
        for e in self.ENG:
            self.sems[e] = nc.alloc_semaphore(f"s{self._pid}_{e}")
        for i, ch in enumerate(self.chan_n):
            self.chan_sems[ch] = nc.alloc_semaphore(f"s{self._pid}_ch{i}")
        sigrank = {}
        cnt = {}
        for e in self.ENG:
            ords = sorted(o for (sk, o) in self.signal if sk == e)
            for i, o in enumerate(ords):
                sigrank[(e, o)] = self.sig_base[e] + i + 1
            cnt[e] = len(ords)

        def semval(sk, o):
            if isinstance(sk, tuple):
                return self.chan_sems[sk[1]], 16 * o
            return self.sems[sk], sigrank[(sk, o)]

        ops = self.ops
        signal = self.signal

        def run(eng_name):
            def body(eng):
                for op in ops[eng_name]:
                    for sk, o in op["waits"]:
                        s, v = semval(sk, o)
                        eng.wait_ge(s, v)
                    if op["emit"] is None:
                        continue
                    ins = op["emit"](eng)
                    me = op["me"]
                    if op["dma"]:
                        ins.then_inc(self.chan_sems[me[0][1]], 16)
                    elif me in signal:
                        ins.then_inc(self.sems[me[0]], 1)
            return body

        with nc.Block() as block:
            block.tensor(run("pe"))
            block.scalar(run("act"))
            block.vector(run("dve"))
            block.gpsimd(run("pool"))
            block.sync(run("sp"))
        nc.clear_and_free_semaphores(nc.allocated_since(snap))
        nc.all_engine_barrier()
        self._new_phase()
        self._reset()

    def close(self):
        pass


class Builder:
    def __init__(self):
        self.nc = bass.Bass("TRN2", target_bir_lowering=False)
        self.sc = Sched(self.nc)
        self._glob = []
        self._scope = []

    def din(self, name, shape, dt=F32):
        return self.nc.dram_tensor(name, list(shape), dt, kind="ExternalInput").ap()

    def dout(self, name, shape, dt=F32):
        return self.nc.dram_tensor(name, list(shape), dt, kind="ExternalOutput").ap()

    def dscr(self, name, shape, dt=F32):
        return self.nc.dram_tensor(name, list(shape), dt, kind="Internal").ap()

    def sbuf(self, name, shape, dt, glob=False):
        self._uid = getattr(self, "_uid", 0) + 1
        g = self.nc.sbuf_tensor(f"{name}_{self._uid}", list(shape), dt)
        t = g.__enter__()
        (self._glob if glob else self._scope).append(g)
        return t

    def end_phase(self):
        self.sc.end_phase()
        for g in reversed(self._scope):
            g.__exit__(None, None, None)
        self._scope = []

    def pe(self, emit, reads, writes):
        return self.sc.add("pe", emit, reads, writes)

    def act(self, emit, reads, writes):
        return self.sc.add("act", emit, reads, writes)

    def dve(self, emit, reads, writes):
        return self.sc.add("dve", emit, reads, writes)

    def pool(self, emit, reads, writes):
        return self.sc.add("pool", emit, reads, writes)

    def group(self, keys, chan):
        n = self.sc.chan_n[chan]
        for k in keys:
            self.sc.lastw[k] = {("ch", chan): n}

    def dma(self, q, out, in_, reads, writes, chan, slow=False):
        return self.sc.add(q, lambda e: e.dma_start(out=out, in_=in_, allow_slow_non_contiguous=slow),
                           reads, writes, dma=True, chan=chan)


def _consts():
    c = {}
    c["c_ones"] = np.ones((128, 128), np.float32)
    bo = np.zeros((128, 128), np.float32)
    bo[:64, :64] = 1
    bo[64:, 64:] = 1
    c["c_blockones"] = bo
    rm = np.zeros((128, 128), np.float32)
    for hh in (0, 64):
        for j in range(8):
            rm[hh + 8 + j, hh + j] = -1.0
            rm[hh + j, hh + 8 + j] = 1.0
    c["c_rmat"] = rm
    c["c_ident"] = np.eye(128, dtype=np.float32)
    j = np.arange(128)[:, None]
    s = np.arange(128)[None, :]
    c["c_negtri"] = -(j >= s).astype(np.float32)
    t = np.arange(512)[None, None, :]
    i4 = np.arange(4)[None, :, None]
    jj = np.arange(128)[:, None, None]
    kg = 128 * i4 + jj
    c["c_mbig_fox"] = (-BIG * (kg > t)).astype(np.float32)
    c["c_mbig_sb"] = (-BIG * (kg >= t)).astype(np.float32)
    c["c_m01_sb"] = (kg < t).astype(np.float32)
    es = np.zeros((128, 32, 64), np.float32)
    for kt in range(32):
        es[:, kt, kt] = 1
        es[:, kt, 32 + kt] = 1
    c["c_esel2"] = es
    ns = np.zeros((64, 32, 128), np.float32)
    for kt in range(32):
        for r in range(64):
            if (r % 32) > kt:
                ns[r, kt, :] = -1
    c["c_negsel"] = ns
    c["c_nbident"] = (-BIG * np.eye(128)).astype(np.float32)
    q = np.arange(128)[:, None]
    k = np.arange(128)[None, :]
    c["c_causal"] = (-1e30 * (k > q)).astype(np.float32)
    invf = np.zeros((1, 128), np.float32)
    half = 8
    f = (500000.0 ** (-np.arange(half, dtype=np.float32) * 2.0 / 16.0)).astype(np.float32)
    for hh in (0, 64):
        invf[0, hh:hh + 8] = f
        invf[0, hh + 8:hh + 16] = f
    c["c_invf"] = invf
    ed = np.zeros((65, 64), np.float32)
    ed[64, :] = 1
    c["c_edenom"] = ed
    return c


_CONST = _consts()


def build_program(phases=("all",)):
    B = Builder()
    nc = B.nc
    ALLP = "all" in phases

    def on(p):
        return ALLP or p in phases

    xT_in = B.din("xT", [D, S])
    pos_in = B.din("pos", [1, S], I32)
    ffn_w = {}
    for nm in ("ffn1", "ffn2"):
        ffn_w[nm] = dict(
            norm=B.din(nm + "_norm", [DEPTH, D]),
            wg=B.din(nm + "_w_gate", [DEPTH, D, DFF]),
            wu=B.din(nm + "_w_up", [DEPTH, D, DFF]),
            wd=B.din(nm + "_w_down", [DEPTH, DFF, D]),
        )
    mix_norm = B.din("mix_norm", [DEPTH, D])
    w_in = B.din("w_in", [DEPTH, D, NIN])
    b_forget = B.din("b_forget", [DEPTH, 6])
    b_gates = B.din("b_gates", [DEPTH, 3, D])
    hn_names = ("q_norm_fox", "k_norm_fox", "q_norm_sb", "k_norm_sb", "q_norm_dsa", "k_norm_dsa")
    hn = {n: B.din(n, [DEPTH, 64]) for n in hn_names}
    w_br = [B.din("w_branch_fox", [DEPTH, 384, D]), B.din("w_branch_sb", [DEPTH, 384, D]),
            B.din("w_branch_dsa", [DEPTH, 256, D])]
    w_out = B.din("w_out", [DEPTH, D, D])
    cd = {k: B.din(k, list(v.shape)) for k, v in _CONST.items()}
    outT = B.dout("outT", [D, S])

    QfA = B.dscr("QfA", [6, 68, S], BF16)
    KfA = B.dscr("KfA", [6, 68, S], BF16)
    QsT = B.dscr("QsT", [6, 64, S], BF16)
    KsT = B.dscr("KsT", [6, 64, S], BF16)
    QcT = B.dscr("QcT", [4, 64, S], BF16)
    KcT = B.dscr("KcT", [4, 64, S], BF16)
    QiT = B.dscr("QiT", [4, 64, S], BF16)
    KiT = B.dscr("KiT", [64, S], BF16)
    Vtok = B.dscr("Vtok", [S, 1024], BF16)
    WiD = B.dscr("WiD", [S, 4], F32)
    NLF = B.dscr("NLF", [6, S], F32)
    CosD = B.dscr("CosD", [128, S], F32)
    SinD = B.dscr("SinD", [128, S], F32)
    dbg = [p for p in phases if p.startswith("dbgOT")]
    if dbg:
        OT = B.dout("OT", [D, S], BF16)
    else:
        OT = B.dscr("OT", [D, S], BF16)

    ones = B.sbuf("ones", [128, 128], BF16, glob=True)
    blockones = B.sbuf("blockones", [128, 128], BF16, glob=True)
    rmat = B.sbuf("rmat", [128, 128], BF16, glob=True)
    ident = B.sbuf("ident", [128, 128], BF16, glob=True)
    negtri = B.sbuf("negtri", [128, 128], BF16, glob=True)
    nbident = B.sbuf("nbident", [128, 128], BF16, glob=True)
    mbz = B.sbuf("mbz", [128, 512], BF16, glob=True)
    epsb = B.sbuf("epsb", [128, 1], F32, glob=True)
    oneb = B.sbuf("oneb", [128, 1], F32, glob=True)
    negpi = B.sbuf("negpi", [128, 1], F32, glob=True)
    gvec = B.sbuf("gvec", [128, 6, NDC], F32, glob=True)
    hg = B.sbuf("hg", [128, DEPTH, 6], F32, glob=True)
    negb = B.sbuf("negb", [6, DEPTH], F32, glob=True)
    bgate = B.sbuf("bgate", [128, DEPTH, 3, NDC], F32, glob=True)

    psg = [nc.psum_tensor(f"ps{i}", [128, 512], F32) for i in range(8)]
    ps = [g.__enter__() for g in psg]

    def phase0():
        B.dma("pool", ones[:], cd["c_ones"][:, :], [], ["c"], "c0")
        B.dma("pool", blockones[:], cd["c_blockones"][:, :], [], ["c"], "c0")
        B.dma("pool", rmat[:], cd["c_rmat"][:, :], [], ["c"], "c0")
        B.dma("pool", ident[:], cd["c_ident"][:, :], [], ["c"], "c0")
        B.dma("pool", negtri[:], cd["c_negtri"][:, :], [], ["c"], "c0")
        B.dma("pool", nbident[:], cd["c_nbident"][:, :], [], ["c"], "c0")
        B.dve(lambda e: e.memset(epsb[:], EPS), [], ["epsb"])
        B.dve(lambda e: e.memset(oneb[:], 1.0), [], ["oneb"])
        B.dve(lambda e: e.memset(negpi[:], -float(np.pi)), [], ["negpi"])
        B.dve(lambda e: e.memset(mbz[:], -BIG), [], ["mbz"])
        for l in range(DEPTH):
            for wi, src in enumerate((ffn_w["ffn1"]["norm"], mix_norm, ffn_w["ffn2"]["norm"])):
                B.dma("sp", gvec[:, l * 3 + wi, :], src[l].rearrange("(c p) -> p c", p=128),
                      [], [("gvec", l, wi)], "c_gvec", slow=True)
            for ni, n in enumerate(hn_names):
                for hh in (0, 64):
                    B.dma("sp", hg[hh:hh + 64, l, ni:ni + 1], hn[n][l].rearrange("(d o) -> d o", o=1),
                          [], [("hg", l, ni, hh)], "c_hg", slow=True)
            for br in range(3):
                B.dma("sp", bgate[:, l, br, :], b_gates[l, br].rearrange("(c p) -> p c", p=128),
                      [], [("bgate", l, br)], "c_bgate", slow=True)
        B.group(["hg"], "c_hg")
        B.dma("sp", negb[:], b_forget.rearrange("l h -> h l"), [], ["negb"], "c_negb", slow=True)
        B.dve(lambda e: e.tensor_scalar(out=negb[:], in0=negb[:], scalar1=-1.0, scalar2=None, op0=ALU.mult),
              ["negb"], ["negb"])
        for ni in (0, 2, 4):
            B.dve(lambda e, ni=ni: e.tensor_scalar(out=hg[:, :, ni:ni + 1], in0=hg[:, :, ni:ni + 1], scalar1=0.125,
                                                   scalar2=None, op0=ALU.mult), ["hg"], ["hg"])
        posi = B.sbuf("posi", [1, S], I32)
        posf = B.sbuf("posf", [1, S], F32)
        invf = B.sbuf("invf", [1, 128], F32)
        ang = B.sbuf("ang", [128, 512], F32)
        angi = B.sbuf("angi", [128, 512], I32)
        angf = B.sbuf("angf", [128, 512], F32)
        tb = [B.sbuf(f"tb{i}", [128, 512], F32) for i in range(2)]
        B.dma("sp", posi[:], pos_in[:, :], [], ["posi"], "c2")
        B.dma("sp", invf[:], cd["c_invf"][:, :], [], ["invf"], "c2")
        B.dve(lambda e: e.tensor_copy(out=posf[:], in_=posi[:]), ["posi"], ["posf"])
        for i in range(NTT):
            t0 = i * TT
            B.pe(lambda e, t0=t0: e.matmul(ps[0][:], invf[:], posf[:, t0:t0 + TT], start=True, stop=True),
                 ["invf", "posf"], [("ps", 0)])
            for k, (shift, dst) in enumerate(((0.0, SinD), (0.25, CosD))):
                B.dve(lambda e, shift=shift: e.tensor_scalar(out=ang[:], in0=ps[0][:], scalar1=float(1.0 / (2 * np.pi)),
                                                             scalar2=float(shift), op0=ALU.mult, op1=ALU.add),
                      [("ps", 0)], ["ang"])
                B.dve(lambda e: e.tensor_copy(out=angi[:], in_=ang[:]), ["ang"], ["angi"])
                B.dve(lambda e: e.tensor_copy(out=angf[:], in_=angi[:]), ["angi"], ["angf"])
                B.dve(lambda e: e.tensor_tensor(out=ang[:], in0=ang[:], in1=angf[:], op=ALU.subtract), ["ang", "angf"], ["ang"])
                B.dve(lambda e: e.tensor_scalar(out=angf[:], in0=ang[:], scalar1=0.5, scalar2=None, op0=ALU.is_gt), ["ang"], ["angf"])
                B.dve(lambda e: e.tensor_tensor(out=ang[:], in0=ang[:], in1=angf[:], op=ALU.subtract), ["ang", "angf"], ["ang"])
                B.act(lambda e, k=k: e.activation(out=tb[k][:], in_=ang[:], func=AF.Sin, scale=float(2 * np.pi)),
                      ["ang"], [("tb", k)])
                B.dma("sp", dst[:, t0:t0 + TT], tb[k][:], [("tb", k)], [], ("tb", k))
        B.end_phase()

    def rmsnorm_tile(X, xk, gi, hT, sq, rstd):
        for c in range(NDC):
            B.dve(lambda e, c=c: e.tensor_tensor(out=sq[:, c, :], in0=X[:, c, :], in1=X[:, c, :], op=ALU.mult),
                  [xk], [("sq", c)])
        for c in range(NDC):
            B.pe(lambda e, c=c: e.matmul(ps[0][:], ones[:], sq[:, c, :], start=(c == 0), stop=(c == NDC - 1)),
                 [("sq", c)], [("ps", 0)])
        B.act(lambda e: e.activation(out=rstd[:], in_=ps[0][:], func=AF.Sqrt, scale=1.0 / D, bias=epsb[:, 0:1]),
              [("ps", 0)], ["rstd"])
        B.dve(lambda e: e.reciprocal(out=rstd[:], in_=rstd[:]), ["rstd"], ["rstd"])
        for c in range(NDC):
            B.dve(lambda e, c=c: e.scalar_tensor_tensor(out=hT[:, c, :], in0=X[:, c, :], scalar=gvec[:, gi, c:c + 1],
                                                        in1=rstd[:], op0=ALU.mult, op1=ALU.mult),
                  [xk, "rstd"], [("hT", c)])

    def ffn_phase(l, wi, src, dst, first):
        nm = ("ffn1", "ffn2")[wi]
        w = ffn_w[nm]
        gi = l * 3 + (0, 2)[wi]
        Wg = B.sbuf("Wg", [128, NDC, DFF], BF16)
        Wu = B.sbuf("Wu", [128, NDC, DFF], BF16)
        Wd = B.sbuf("Wd", [128, NFC, D], BF16)
        xt = [B.sbuf(f"xt{i}", [128, NDC, TT], F32) for i in range(2)]
        hT = B.sbuf("hT", [128, NDC, TT], BF16)
        actT = B.sbuf("actT", [128, NFC, TT], BF16)
        sq = actT
        rstd = B.sbuf("rstd", [128, TT], F32)
        sil = [B.sbuf(f"sil{i}", [128, TT], F32) for i in range(2)]
        for c in range(NDC):
            B.dma("pool", Wg[:, c, :], w["wg"][l, c * 128:(c + 1) * 128, :], [], [("Wg", c)], "w0")
            B.dma("pool", Wu[:, c, :], w["wu"][l, c * 128:(c + 1) * 128, :], [], [("Wu", c)], "w1")
        for j in range(NFC):
            B.dma("pool", Wd[:, j, :], w["wd"][l, j * 128:(j + 1) * 128, :], [], [("Wd", j)], "w2")
        B.group([("Wg", c) for c in range(NDC)], "w0")
        B.group([("Wu", c) for c in range(NDC)], "w1")
        B.group([("Wd", j) for j in range(NFC)], "w2")

        def load_x(i):
            t0 = i * TT
            B.dma("sp", xt[i % 2][:], src[:, t0:t0 + TT].rearrange("(c p) t -> p c t", p=128),
                  [], [("xt", i % 2)], ("xt", i % 2))

        load_x(0)
        for i in range(NTT):
            if i + 1 < NTT:
                load_x(i + 1)
            X = xt[i % 2]
            xk = ("xt", i % 2)
            rmsnorm_tile(X, xk, gi, hT, sq, rstd)
            for j in range(NFC):
                pa = 1 + (j % 2)
                pu = 3 + (j % 2)
                for c in range(NDC):
                    B.pe(lambda e, c=c, j=j, pa=pa: e.matmul(ps[pa][:], Wg[:, c, j * 128:(j + 1) * 128], hT[:, c, :],
                                                             start=(c == 0), stop=(c == NDC - 1)),
                         [("Wg", c), ("hT", c)], [("ps", pa)])
                for c in range(NDC):
                    B.pe(lambda e, c=c, j=j, pu=pu: e.matmul(ps[pu][:], Wu[:, c, j * 128:(j + 1) * 128], hT[:, c, :],
                                                             start=(c == 0), stop=(c == NDC - 1)),
                         [("Wu", c), ("hT", c)], [("ps", pu)])
                B.act(lambda e, j=j, pa=pa: e.activation(out=sil[j % 2][:], in_=ps[pa][:], func=AF.Silu),
                      [("ps", pa)], [("sil", j % 2)])
                B.dve(lambda e, j=j, pu=pu: e.tensor_tensor(out=actT[:, j, :], in0=ps[pu][:], in1=sil[j % 2][:], op=ALU.mult),
                      [("ps", pu), ("sil", j % 2)], [("sq", j)])
            for m in range(NDC):
                py = 5 + (m % 2)
                for j in range(NFC):
                    B.pe(lambda e, m=m, j=j, py=py: e.matmul(ps[py][:], Wd[:, j, m * 128:(m + 1) * 128], actT[:, j, :],
                                                             start=(j == 0), stop=(j == NFC - 1)),
                         [("Wd", j), ("sq", j)], [("ps", py)])
                B.dve(lambda e, m=m, py=py, X=X: e.scalar_tensor_tensor(out=X[:, m, :], in0=ps[py][:], scalar=0.5, in1=X[:, m, :],
                                                                       op0=ALU.mult, op1=ALU.add),
                      [("ps", py), xk], [xk])
            t0 = i * TT
            B.dma("sp", dst[:, t0:t0 + TT].rearrange("(c p) t -> p c t", p=128), X[:],
                  [xk], [], ("xt", i % 2))
        B.end_phase()

    def proj_phase(l, src):
        gi = l * 3 + 1
        NW = C_G
        Win = B.sbuf("Win", [128, NDC, NW], BF16)
        xt = [B.sbuf(f"xt{i}", [128, NDC, TT], F32) for i in range(2)]
        hT = B.sbuf("hT", [128, NDC, TT], BF16)
        sq = B.sbuf("sq", [128, NDC, TT], BF16)
        rstd = B.sbuf("rstd", [128, TT], F32)
        q32 = B.sbuf("q32", [128, TT], F32)
        qsq = B.sbuf("qsq", [128, TT], BF16)
        qr = B.sbuf("qr", [128, TT], F32)
        qn32 = B.sbuf("qn32", [128, TT], F32)
        qnt = B.sbuf("qnt", [128, TT], BF16)
        t1 = B.sbuf("t1", [128, TT], F32)
        t2 = B.sbuf("t2", [128, TT], F32)
        qnb = [B.sbuf(f"qnb{i}", [128, TT], BF16) for i in range(2)]
        cosT = B.sbuf("cosT", [128, TT], F32)
        sinT = B.sbuf("sinT", [128, TT], F32)
        vst = B.sbuf("vst", [128, 4, 1024], BF16)
        wist = B.sbuf("wist", [128, 4, 4], F32)
        lfe = B.sbuf("lfe", [6, TT], F32)
        nlft = B.sbuf("nlft", [6, TT], F32)

        for c in range(NDC):
            B.dma("pool", Win[:, c, :], w_in[l, c * 128:(c + 1) * 128, 0:NW], [], [("Win", c)], "w0")
        B.group([("Win", c) for c in range(NDC)], "w0")

        def load_x(i):
            t0 = i * TT
            B.dma("sp", xt[i % 2][:], src[:, t0:t0 + TT].rearrange("(c p) t -> p c t", p=128),
                  [], [("xt", i % 2)], ("xt", i % 2))

        pairs = []
        for p in range(3):
            pairs.append((C_QF + 128 * p, 128, 0, False, ("A", QfA, 2 * p)))
            pairs.append((C_KF + 128 * p, 128, 1, False, ("A", KfA, 2 * p)))
            pairs.append((C_QS + 128 * p, 128, 2, False, ("T", QsT, 2 * p)))
            pairs.append((C_KS + 128 * p, 128, 3, False, ("T", KsT, 2 * p)))
        for p in range(2):
            pairs.append((C_QC + 128 * p, 128, 4, True, ("T", QcT, 2 * p)))
            pairs.append((C_KC + 128 * p, 128, 5, True, ("T", KcT, 2 * p)))
            pairs.append((C_QI + 128 * p, 128, None, True, ("T", QiT, 2 * p)))
        pairs.append((C_KI, 64, None, True, ("K", KiT, 0)))

        load_x(0)
        for i in range(NTT):
            t0 = i * TT
            if i + 1 < NTT:
                load_x(i + 1)
            X = xt[i % 2]
            xk = ("xt", i % 2)
            B.dma("sp", cosT[:], CosD[:, t0:t0 + TT], [], ["cosT"], "cosT")
            B.dma("sp", sinT[:], SinD[:, t0:t0 + TT], [], ["sinT"], "sinT")
            rmsnorm_tile(X, xk, gi, hT, sq, rstd)
            for pi, (col0, M, gidx, rope, dstd) in enumerate(pairs):
                pb = 1 + (pi % 2)
                slot = pi % 2
                for c in range(NDC):
                    B.pe(lambda e, c=c, col0=col0, M=M, pb=pb: e.matmul(ps[pb][0:M, :], Win[:, c, col0:col0 + M], hT[:, c, :],
                                                                         start=(c == 0), stop=(c == NDC - 1)),
                         [("Win", c), ("hT", c)], [("ps", pb)])
                final = qnb[slot]
                fk = ("qnb", slot)
                if gidx is not None:
                    B.act(lambda e, pb=pb: e.activation(out=q32[:], in_=ps[pb][:], func=AF.Copy), [("ps", pb)], ["q32"])
                    B.dve(lambda e: e.tensor_tensor(out=qsq[:], in0=q32[:], in1=q32[:], op=ALU.mult), ["q32"], ["qsq"])
                    B.pe(lambda e: e.matmul(ps[3][:], blockones[:], qsq[:], start=True, stop=True), ["qsq"], [("ps", 3)])
                    B.act(lambda e: e.activation(out=qr[:], in_=ps[3][:], func=AF.Sqrt, scale=1.0 / 64, bias=epsb[:, 0:1]),
                          [("ps", 3)], ["qr"])
                    B.dve(lambda e: e.reciprocal(out=qr[:], in_=qr[:]), ["qr"], ["qr"])
                    tgt = qn32 if rope else final
                    B.dve(lambda e, tgt=tgt, gidx=gidx: e.scalar_tensor_tensor(out=tgt[:], in0=q32[:], scalar=hg[:, l, gidx:gidx + 1],
                                                                              in1=qr[:], op0=ALU.mult, op1=ALU.mult),
                          ["q32", "qr"], ["qn32" if rope else fk])
                else:
                    B.act(lambda e, pb=pb, M=M: e.activation(out=qn32[0:M, :], in_=ps[pb][0:M, :], func=AF.Copy),
                          [("ps", pb)], ["qn32"])
                if rope:
                    B.dve(lambda e, M=M: e.tensor_copy(out=qnt[0:M, :], in_=qn32[0:M, :]), ["qn32"], ["qnt"])
                    B.pe(lambda e, M=M: e.matmul(ps[4][0:M, :], rmat[0:M, 0:M], qnt[0:M, :], start=True, stop=True),
                         ["qnt"], [("ps", 4)])
                    B.dve(lambda e, M=M: e.tensor_tensor(out=t1[0:M, :], in0=qn32[0:M, :], in1=cosT[0:M, :], op=ALU.mult),
                          ["qn32", "cosT"], ["t1"])
                    B.dve(lambda e, M=M: e.tensor_tensor(out=t2[0:M, :], in0=ps[4][0:M, :], in1=sinT[0:M, :], op=ALU.mult),
                          [("ps", 4), "sinT"], ["t2"])
                    B.dve(lambda e, M=M, final=final: e.tensor_tensor(out=final[0:M, :], in0=t1[0:M, :], in1=t2[0:M, :], op=ALU.add),
                          ["t1", "t2"], [fk])
                kind, dt_, h0 = dstd
                if kind == "A":
                    for hh in range(2):
                        B.dma("sp", dt_[h0 + hh, 0:64, t0:t0 + TT], final[hh * 64:(hh + 1) * 64, :], [fk], [], fk)
                elif kind == "T":
                    B.dma("sp", dt_[h0:h0 + 2, :, t0:t0 + TT].rearrange("h d t -> (h d) t"), final[:], [fk], [], fk)
                else:
                    B.dma("sp", dt_[:, t0:t0 + TT], final[0:64, :], [fk], [], fk)
            k = 0
            for sub in range(4):
                for (col, n, d0) in ((C_VF, 384, 0), (C_VS, 384, 384), (C_VC, 256, 768)):
                    pv = 5 + (k % 2)
                    k += 1
                    for c in range(NDC):
                        B.pe(lambda e, c=c, sub=sub, col=col, n=n, pv=pv: e.matmul(
                            ps[pv][:, 0:n], hT[:, c, sub * 128:(sub + 1) * 128], Win[:, c, col:col + n],
                            start=(c == 0), stop=(c == NDC - 1)), [("Win", c), ("hT", c)], [("ps", pv)])
                    B.act(lambda e, sub=sub, n=n, d0=d0, pv=pv: e.activation(out=vst[:, sub, d0:d0 + n], in_=ps[pv][:, 0:n], func=AF.Copy),
                          [("ps", pv)], ["vst"])
                for c in range(NDC):
                    B.pe(lambda e, c=c, sub=sub: e.matmul(ps[7][:, 0:4], hT[:, c, sub * 128:(sub + 1) * 128], Win[:, c, C_WI:C_WI + 4],
                                                          start=(c == 0), stop=(c == NDC - 1)), [("Win", c), ("hT", c)], [("ps", 7)])
                B.dve(lambda e, sub=sub: e.tensor_scalar(out=wist[:, sub, :], in0=ps[7][:, 0:4], scalar1=1.0 / 16, scalar2=None, op0=ALU.mult),
                      [("ps", 7)], ["wist"])
            B.dma("sp", Vtok[t0:t0 + TT, :].rearrange("(s p) n -> p s n", p=128), vst[:], ["vst"], [], "vst")
            B.dma("sp", WiD[t0:t0 + TT, :].rearrange("(s p) n -> p s n", p=128), wist[:], ["wist"], [], "wist", slow=True)
            for c in range(NDC):
                B.pe(lambda e, c=c: e.matmul(ps[0][0:6, :], Win[:, c, C_FF:C_FF + 6], hT[:, c, :], start=(c == 0), stop=(c == NDC - 1)),
                     [("Win", c), ("hT", c)], [("ps", 0)])
            B.act(lambda e: e.activation(out=lfe[:], in_=ps[0][0:6, :], func=AF.Exp, scale=-1.0, bias=negb[:, l:l + 1]),
                  [("ps", 0), "negb"], ["lfe"])
            B.act(lambda e: e.activation(out=nlft[:], in_=lfe[:], func=AF.Ln, bias=oneb[0:6, 0:1], scale=1.0),
                  ["lfe"], ["nlft"])
            B.dma("sp", NLF[:, t0:t0 + TT], nlft[:], ["nlft"], [], "nlft")
        B.end_phase()
        nlf = B.sbuf("nlf", [6, S], F32)
        ncum = B.sbuf("ncum", [6, S], F32)
        one6 = B.sbuf("one6", [6, S], F32)
        hi6 = B.sbuf("hi6", [6, S], BF16)
        lo6 = B.sbuf("lo6", [6, S], BF16)
        nhi6 = B.sbuf("nhi6", [6, S], BF16)
        nlo6 = B.sbuf("nlo6", [6, S], BF16)
        ob6 = B.sbuf("ob6", [6, S], BF16)
        B.dma("sp", nlf[:], NLF[:, :], [], ["nlf"], "nlf")
        B.dve(lambda e: e.memset(one6[:], 1.0), [], ["one6"])
        B.dve(lambda e: e.memset(ob6[:], 1.0), [], ["ob6"])
        B.dve(lambda e: e.tensor_tensor_scan(out=ncum[:], data0=one6[:], data1=nlf[:], initial=0.0, op0=ALU.mult, op1=ALU.add),
              ["one6", "nlf"], ["ncum"])
        B.dve(lambda e: e.tensor_copy(out=hi6[:], in_=ncum[:]), ["ncum"], ["hi6"])
        B.dve(lambda e: e.tensor_tensor(out=lo6[:], in0=ncum[:], in1=hi6[:], op=ALU.subtract), ["ncum", "hi6"], ["lo6"])
        B.dve(lambda e: e.tensor_scalar(out=nhi6[:], in0=hi6[:], scalar1=-1.0, scalar2=None, op0=ALU.mult), ["hi6"], ["nhi6"])
        B.dve(lambda e: e.tensor_scalar(out=nlo6[:], in0=lo6[:], scalar1=-1.0, scalar2=None, op0=ALU.mult), ["lo6"], ["nlo6"])
        for row, t_, k_ in ((64, nhi6, "nhi6"), (65, nlo6, "nlo6"), (66, ob6, "ob6"), (67, ob6, "ob6")):
            B.dma("sp", QfA[:, row, :], t_[:], [k_], [], "aug")
        for row, t_, k_ in ((64, ob6, "ob6"), (65, ob6, "ob6"), (66, hi6, "hi6"), (67, lo6, "lo6")):
            B.dma("sp", KfA[:, row, :], t_[:], [k_], [], "aug")
        B.end_phase()

    def fox_phase():
        Ka = [B.sbuf(f"Ka{i}", [68, S], BF16) for i in range(2)]
        Qa = [B.sbuf(f"Qa{i}", [68, S], BF16) for i in range(2)]
        V = [B.sbuf(f"V{i}", [128, NKT, 65], BF16) for i in range(2)]
        mb = B.sbuf("mb", [128, 4, 512], BF16)
        ed = B.sbuf("ed", [65, 64], F32)
        pT = [B.sbuf(f"pT{i}", [128, 512], BF16) for i in range(3)]
        osb = B.sbuf("osb", [65, 512], F32)
        rec = B.sbuf("rec", [64, 512], F32)
        ost = [B.sbuf(f"ost{i}", [64, 512], BF16) for i in range(2)]
        B.dma("pool", mb[:], cd["c_mbig_fox"][:, :, :], [], ["mb"], "c0")
        B.dma("sp", ed[:], cd["c_edenom"][:, :], [], ["ed"], "c2")
        for i in range(2):
            B.dve(lambda e, i=i: e.memset(V[i][:, :, 64:65], 1.0), [], [("Vone", i)])

        def load_head(h):
            b = h % 2
            B.dma("sp", Ka[b][:], KfA[h, :, :], [], [("Ka", b)], ("Ka", b))
            B.dma("sp", Qa[b][:], QfA[h, :, :], [], [("Qa", b)], ("Qa", b))
            B.dma("sp", V[b][:, :, 0:64], Vtok[:, h * 64:(h + 1) * 64].rearrange("(kt p) d -> p kt d", p=128),
                  [], [("V", b)], ("V", b))

        load_head(0)
        it = 0
        for h in range(6):
            if h + 1 < 6:
                load_head(h + 1)
            b = h % 2
            for qb in range(8):
                po = 3 + (qb % 2)
                nk = 4 * (qb + 1)
                for kt in range(nk):
                    pb = it % 3
                    it += 1
                    diag = kt >= 4 * qb
                    B.pe(lambda e, kt=kt, qb=qb, pb=pb, b=b, diag=diag: e.matmul(
                        ps[pb][:], Ka[b][:, kt * 128:(kt + 1) * 128], Qa[b][:, qb * 512:(qb + 1) * 512],
                        start=True, stop=not diag), [("Ka", b), ("Qa", b)], [("ps", pb)])
                    if diag:
                        B.pe(lambda e, kt=kt, qb=qb, pb=pb: e.matmul(ps[pb][:], ident[:], mb[:, kt - 4 * qb, :],
                                                                     start=False, stop=True), ["mb"], [("ps", pb)])
                    B.act(lambda e, pb=pb: e.activation(out=pT[pb][:], in_=ps[pb][:], func=AF.Exp),
                          [("ps", pb)], [("pT", pb)])
                    B.pe(lambda e, kt=kt, pb=pb, po=po, b=b, nk=nk: e.matmul(
                        ps[po][0:65, :], V[b][:, kt, :], pT[pb][:], start=(kt == 0), stop=(kt == nk - 1)),
                        [("V", b), ("Vone", b), ("pT", pb)], [("ps", po)])
                B.act(lambda e, po=po: e.activation(out=osb[:], in_=ps[po][0:65, :], func=AF.Copy), [("ps", po)], ["osb"])
                B.pe(lambda e: e.matmul(ps[5][0:64, :], ed[:], osb[:], start=True, stop=True), ["ed", "osb"], [("ps", 5)])
                B.dve(lambda e: e.reciprocal(out=rec[:], in_=ps[5][0:64, :]), [("ps", 5)], ["rec"])
                so = qb % 2
                B.dve(lambda e, so=so: e.tensor_tensor(out=ost[so][:], in0=osb[0:64, :], in1=rec[:], op=ALU.mult),
                      ["osb", "rec"], [("ost", so)])
                B.dma("sp", OT[h * 64:(h + 1) * 64, qb * 512:(qb + 1) * 512], ost[so][:], [("ost", so)], [], ("ost", so))
        B.end_phase()

    def sb_phase():
        Kt = [B.sbuf(f"Kt{i}", [64, S], BF16) for i in range(2)]
        Qt = [B.sbuf(f"Qt{i}", [64, S], BF16) for i in range(2)]
        V = [B.sbuf(f"V{i}", [128, NKT, 64], BF16) for i in range(2)]
        mb = B.sbuf("mb", [128, 4, 512], BF16)
        m01 = B.sbuf("m01", [128, 4, 512], BF16)
        esel = B.sbuf("esel", [128, 32, 64], BF16)
        nsel = B.sbuf("nsel", [64, 32, 128], BF16)
        spT = B.sbuf("spT", [128, NKT, 512], BF16)
        e32 = [B.sbuf(f"e32{i}", [128, 512], F32) for i in range(2)]
        csh = B.sbuf("csh", [64, 512], BF16)
        hi2 = B.sbuf("hi2", [64, 512], BF16)
        aT = [B.sbuf(f"aT{i}", [128, 512], BF16) for i in range(3)]
        ost = [B.sbuf(f"ost{i}", [64, 512], BF16) for i in range(2)]
        B.dma("pool", mb[:], cd["c_mbig_sb"][:, :, :], [], ["mb"], "c0")
        B.dma("pool", m01[:], cd["c_m01_sb"][:, :, :], [], ["m01"], "c0")
        B.dma("pool", esel[:], cd["c_esel2"][:, :, :], [], ["esel"], "c0")
        B.dma("pool", nsel[:], cd["c_negsel"][:, :, :], [], ["nsel"], "c0")
        B.group(["mb", "m01", "esel", "nsel"], "c0")

        def load_head(h):
            b = h % 2
            B.dma("sp", Kt[b][:], KsT[h, :, :], [], [("Kt", b)], ("Kt", b))
            B.dma("sp", Qt[b][:], QsT[h, :, :], [], [("Qt", b)], ("Qt", b))
            B.dma("sp", V[b][:], Vtok[:, 384 + h * 64:384 + (h + 1) * 64].rearrange("(kt p) d -> p kt d", p=128),
                  [], [("V", b)], ("V", b))

        load_head(0)
        it = 0
        it2 = 0
        for h in range(6):
            if h + 1 < 6:
                load_head(h + 1)
            b = h % 2
            for qb in range(8):
                nk = 4 * (qb + 1)
                qs = slice(qb * 512, (qb + 1) * 512)
                for kt in range(nk):
                    pb = it % 2
                    it += 1
                    B.pe(lambda e, kt=kt, qs=qs, pb=pb, b=b: e.matmul(ps[pb][:], Kt[b][:, kt * 128:(kt + 1) * 128], Qt[b][:, qs],
                                                                      start=True, stop=True), [("Kt", b), ("Qt", b)], [("ps", pb)])
                    B.act(lambda e, pb=pb: e.activation(out=e32[pb][:], in_=ps[pb][:], func=AF.Exp), [("ps", pb)], [("e32", pb)])
                    B.act(lambda e, pb=pb, kt=kt: e.activation(out=spT[:, kt, :], in_=e32[pb][:], func=AF.Ln, bias=oneb[:, 0:1], scale=1.0),
                          [("e32", pb)], [("sp", kt)])
                    if kt >= 4 * qb:
                        B.dve(lambda e, kt=kt, qb=qb: e.tensor_tensor(out=spT[:, kt, :], in0=spT[:, kt, :], in1=m01[:, kt - 4 * qb, :], op=ALU.mult),
                              [("sp", kt), "m01"], [("sp", kt)])
                    B.pe(lambda e, kt=kt, nk=nk: e.matmul(ps[2][0:64, :], esel[:, kt, :], spT[:, kt, :], start=(kt == 0), stop=(kt == nk - 1)),
                         ["esel", ("sp", kt)], [("ps", 2)])
                B.dve(lambda e: e.tensor_copy(out=csh[0:32, :], in_=ps[2][0:32, :]), [("ps", 2)], ["csh0"])
                B.dve(lambda e: e.tensor_copy(out=hi2[32:64, :], in_=ps[2][32:64, :]), [("ps", 2)], ["hi2"])
                B.dve(lambda e: e.tensor_tensor(out=csh[32:64, :], in0=ps[2][32:64, :], in1=hi2[32:64, :], op=ALU.subtract),
                      [("ps", 2), "hi2"], ["csh1"])
                po = 6 + (qb % 2)
                for kt in range(nk):
                    pl = 3 + (it2 % 3)
                    sl = it2 % 3
                    it2 += 1
                    diag = kt >= 4 * qb
                    B.pe(lambda e, kt=kt, qs=qs, pl=pl, b=b: e.matmul(ps[pl][:], Kt[b][:, kt * 128:(kt + 1) * 128], Qt[b][:, qs],
                                                                      start=True, stop=False), [("Kt", b), ("Qt", b)], [("ps", pl)])
                    B.pe(lambda e, kt=kt, pl=pl: e.matmul(ps[pl][:], negtri[:], spT[:, kt, :], start=False, stop=False),
                         [("sp", kt)], [("ps", pl)])
                    B.pe(lambda e, kt=kt, pl=pl, diag=diag: e.matmul(ps[pl][:], nsel[:, kt, :], csh[:], start=False, stop=not diag),
                         ["nsel", "csh0", "csh1"], [("ps", pl)])
                    if diag:
                        B.pe(lambda e, kt=kt, qb=qb, pl=pl: e.matmul(ps[pl][:], ident[:], mb[:, kt - 4 * qb, :], start=False, stop=True),
                             ["mb"], [("ps", pl)])
                    B.act(lambda e, pl=pl, sl=sl: e.activation(out=aT[sl][:], in_=ps[pl][:], func=AF.Exp), [("ps", pl)], [("aT", sl)])
                    B.pe(lambda e, kt=kt, sl=sl, po=po, b=b, nk=nk: e.matmul(ps[po][0:64, :], V[b][:, kt, :], aT[sl][:],
                                                                             start=(kt == 0), stop=(kt == nk - 1)),
                         [("V", b), ("aT", sl)], [("ps", po)])
                so = qb % 2
                B.act(lambda e, po=po, so=so: e.activation(out=ost[so][:], in_=ps[po][0:64, :], func=AF.Copy), [("ps", po)], [("ost", so)])
                B.dma("sp", OT[384 + h * 64:384 + (h + 1) * 64, qs], ost[so][:], [("ost", so)], [], ("ost", so))
        B.end_phase()

    def dsa_phase():
        Kc = B.sbuf("Kc", [64, 4, S], BF16)
        Ki = B.sbuf("Ki", [64, S], BF16)
        V = B.sbuf("V", [128, NKT, 4, 65], BF16)
        Qc = [B.sbuf(f"Qc{i}", [64, 4, 512], BF16) for i in range(2)]
        Qi = [B.sbuf(f"Qi{i}", [64, 4, 512], BF16) for i in range(2)]
        wi = [B.sbuf(f"wi{i}", [128, 4, 4], F32) for i in range(2)]
        score = B.sbuf("score", [128, 4, S], F32)
        Mc = B.sbuf("Mc", [128, 4, S], BF16)
        junk = B.sbuf("junk", [128, S], BF16)
        rl = [B.sbuf(f"rl{i}", [128, 512], F32) for i in range(2)]
        cz = B.sbuf("cz", [128, 128], F32)
        ed = B.sbuf("ed", [65, 64], F32)
        mx = B.sbuf("mx", [128, 4], F32)
        mn = B.sbuf("mn", [128, 4], F32)
        w0 = B.sbuf("w0", [128, 4], F32)
        lo = B.sbuf("lo", [128, 4], F32)
        mid = B.sbuf("mid", [128, 4], F32)
        cnt = B.sbuf("cnt", [128, 4], F32)
        prd = B.sbuf("prd", [128, 4], F32)
        pT = [B.sbuf(f"pT{i}", [128, 512], BF16) for i in range(3)]
        osb = B.sbuf("osb", [65, 512], F32)
        rec = B.sbuf("rec", [64, 512], F32)
        ost = [B.sbuf(f"ost{i}", [64, 512], BF16) for i in range(2)]
        B.dma("sp", cz[:], cd["c_causal"][:, :], [], ["cz"], "c2")
        B.dma("sp", ed[:], cd["c_edenom"][:, :], [], ["ed"], "c2")
        B.group(["cz", "ed"], "c2")
        for h in range(4):
            B.dma("sp", Kc[:, h, :], KcT[h, :, :], [], [("Kc", h)], "Kc")
        B.group([("Kc", h) for h in range(4)], "Kc")
        B.dma("sp", Ki[:], KiT[:, :], [], ["Ki"], "Ki")
        B.dve(lambda e: e.memset(V[:, :, :, 64:65], 1.0), [], ["Vone"])
        for h in range(4):
            B.dma("sp", V[:, :, h, 0:64], Vtok[:, 768 + h * 64:768 + (h + 1) * 64].rearrange("(kt p) d -> p kt d", p=128),
                  [], [("V", h)], "V")
        B.group([("V", h) for h in range(4)], "V")

        def load_q(qb):
            b = qb % 2
            qs = slice(qb * 512, (qb + 1) * 512)
            for h in range(4):
                B.dma("sp", Qc[b][:, h, :], QcT[h, :, qs], [], [("Qc", b, h)], ("Qc", b))
                B.dma("sp", Qi[b][:, h, :], QiT[h, :, qs], [], [("Qi", b, h)], ("Qi", b))
            B.group([("Qc", b, h) for h in range(4)], ("Qc", b))
            B.group([("Qi", b, h) for h in range(4)], ("Qi", b))
            B.dma("sp", wi[b][:], WiD[qs, :].rearrange("(s p) n -> p s n", p=128), [], [("wi", b)], ("wi", b), slow=True)

        load_q(0)
        it = 0
        ir = 0
        for qb in range(8):
            if qb + 1 < 8:
                load_q(qb + 1)
            b = qb % 2
            for g in range(4):
                qt = qb * 4 + g
                kmax = (qt + 1) * 128
                nch = (kmax + 511) // 512
                for kc in range(nch):
                    wdt = min(512, kmax - kc * 512)
                    ks = slice(kc * 512, kc * 512 + wdt)
                    for h in range(4):
                        pb = it % 2
                        it += 1
                        B.pe(lambda e, g=g, h=h, ks=ks, wdt=wdt, pb=pb, b=b: e.matmul(
                            ps[pb][:, 0:wdt], Qi[b][:, h, g * 128:(g + 1) * 128], Ki[:, ks], start=True, stop=True),
                            [("Qi", b, h), "Ki"], [("ps", pb)])
                        r = ir % 2
                        ir += 1
                        B.act(lambda e, pb=pb, wdt=wdt, r=r: e.activation(out=rl[r][:, 0:wdt], in_=ps[pb][:, 0:wdt], func=AF.Relu),
                              [("ps", pb)], [("rl", r)])
                        if h == 0:
                            B.dve(lambda e, g=g, ks=ks, wdt=wdt, r=r, b=b: e.tensor_scalar(
                                out=score[:, g, ks], in0=rl[r][:, 0:wdt], scalar1=wi[b][:, g, 0:1], scalar2=None, op0=ALU.mult),
                                [("rl", r), ("wi", b)], [("score", g)])
                        else:
                            B.dve(lambda e, g=g, h=h, ks=ks, wdt=wdt, r=r, b=b: e.scalar_tensor_tensor(
                                out=score[:, g, ks], in0=rl[r][:, 0:wdt], scalar=wi[b][:, g, h:h + 1], in1=score[:, g, ks],
                                op0=ALU.mult, op1=ALU.add), [("rl", r), ("wi", b), ("score", g)], [("score", g)])
                B.dve(lambda e, g=g, kmax=kmax: e.tensor_reduce(out=mx[:, g:g + 1], in_=score[:, g, 0:kmax], axis=AX.X, op=ALU.max),
                      [("score", g)], ["mx"])
                B.dve(lambda e, g=g, kmax=kmax: e.tensor_reduce(out=mn[:, g:g + 1], in_=score[:, g, 0:kmax], axis=AX.X, op=ALU.min),
                      [("score", g)], ["mn"])
                B.dve(lambda e, g=g, kmax=kmax: e.tensor_tensor(out=score[:, g, kmax - 128:kmax], in0=score[:, g, kmax - 128:kmax],
                                                                in1=cz[:], op=ALU.add), [("score", g), "cz"], [("score", g)])
            B.dve(lambda e: e.tensor_tensor(out=w0[:], in0=mx[:], in1=mn[:], op=ALU.subtract), ["mx", "mn"], ["w0"])
            B.dve(lambda e: e.tensor_scalar(out=w0[:], in0=w0[:], scalar1=1.0001, scalar2=1e-6, op0=ALU.mult, op1=ALU.add), ["w0"], ["w0"])
            B.dve(lambda e: e.tensor_copy(out=lo[:], in_=mn[:]), ["mn"], ["lo"])
            for s_ in range(NBIS):
                hf = 0.5 ** (s_ + 1)
                B.dve(lambda e, hf=hf: e.scalar_tensor_tensor(out=mid[:], in0=w0[:], scalar=hf, in1=lo[:], op0=ALU.mult, op1=ALU.add),
                      ["w0", "lo"], ["mid"])
                B.dve(lambda e: e.memset(cnt[:], 0.0), [], ["cnt"])
                for g in range(4):
                    kmax = (qb * 4 + g + 1) * 128
                    B.dve(lambda e, g=g, kmax=kmax: e.tensor_scalar(out=junk[:, 0:kmax], in0=score[:, g, 0:kmax], scalar1=mid[:, g:g + 1],
                                                                    scalar2=0.0, op0=ALU.is_ge, op1=ALU.add, accum_out=cnt[:, g:g + 1]),
                          [("score", g), "mid", "cnt"], ["junk", "cnt"])
                B.dve(lambda e: e.tensor_scalar(out=prd[:], in0=cnt[:], scalar1=float(TOPK), scalar2=None, op0=ALU.is_ge), ["cnt"], ["prd"])
                B.dve(lambda e: e.tensor_tensor(out=prd[:], in0=prd[:], in1=w0[:], op=ALU.mult), ["prd", "w0"], ["prd"])
                B.dve(lambda e, hf=hf: e.scalar_tensor_tensor(out=lo[:], in0=prd[:], scalar=hf, in1=lo[:], op0=ALU.mult, op1=ALU.add),
                      ["prd", "lo"], ["lo"])
            for g in range(4):
                kmax = (qb * 4 + g + 1) * 128
                B.dve(lambda e, g=g, kmax=kmax: e.tensor_scalar(out=Mc[:, g, 0:kmax], in0=score[:, g, 0:kmax], scalar1=lo[:, g:g + 1],
                                                                scalar2=None, op0=ALU.is_lt), [("score", g), "lo"], [("Mc", g)])
            nk = 4 * (qb + 1)
            for h in range(4):
                po = 6 + (h % 2)
                for kt in range(nk):
                    pl = 3 + (it % 3)
                    sl = it % 3
                    it += 1
                    gmin = max(0, kt - 4 * qb)
                    B.pe(lambda e, kt=kt, h=h, pl=pl, b=b: e.matmul(ps[pl][:], Kc[:, h, kt * 128:(kt + 1) * 128], Qc[b][:, h, :],
                                                                    start=True, stop=False), [("Kc", h), ("Qc", b, h)], [("ps", pl)])
                    if gmin > 0:
                        B.pe(lambda e, pl=pl, gmin=gmin: e.matmul(ps[pl][:, 0:gmin * 128], ident[:], mbz[:, 0:gmin * 128],
                                                                  start=False, stop=False), [], [("ps", pl)])
                    for g in range(gmin, 4):
                        B.pe(lambda e, g=g, kt=kt, pl=pl: e.matmul(ps[pl][:, g * 128:(g + 1) * 128], Mc[:, g, kt * 128:(kt + 1) * 128],
                                                                   nbident[:], start=False, stop=(g == 3)),
                             [("Mc", g)], [("ps", pl)])
                    B.act(lambda e, pl=pl, sl=sl: e.activation(out=pT[sl][:], in_=ps[pl][:], func=AF.Exp), [("ps", pl)], [("pT", sl)])
                    B.pe(lambda e, kt=kt, h=h, sl=sl, po=po, nk=nk: e.matmul(ps[po][0:65, :], V[:, kt, h, :], pT[sl][:],
                                                                             start=(kt == 0), stop=(kt == nk - 1)),
                         [("V", h), "Vone", ("pT", sl)], [("ps", po)])
                B.act(lambda e, po=po: e.activation(out=osb[:], in_=ps[po][0:65, :], func=AF.Copy), [("ps", po)], ["osb"])
                B.pe(lambda e: e.matmul(ps[2][0:64, :], ed[:], osb[:], start=True, stop=True), ["ed", "osb"], [("ps", 2)])
                B.dve(lambda e: e.reciprocal(out=rec[:], in_=ps[2][0:64, :]), [("ps", 2)], ["rec"])
                so = h % 2
                B.dve(lambda e, so=so: e.tensor_tensor(out=ost[so][:], in0=osb[0:64, :], in1=rec[:], op=ALU.mult),
                      ["osb", "rec"], [("ost", so)])
                B.dma("sp", OT[768 + h * 64:768 + (h + 1) * 64, qb * 512:(qb + 1) * 512], ost[so][:], [("ost", so)], [], ("ost", so))
        B.end_phase()

    def out_phase(l, xsrc):
        gi = l * 3 + 1
        Wgt = B.sbuf("Wgt", [128, NDC, 3 * D], BF16)
        Wbr = B.sbuf("Wbr", [128, NDC, D], BF16)
        Wo = B.sbuf("Wo", [128, NDC, D], BF16)
        xt = [B.sbuf(f"xt{i}", [128, NDC, TT], F32) for i in range(2)]
        ot = [B.sbuf(f"ot{i}", [128, NDC, TT], BF16) for i in range(2)]
        hT = B.sbuf("hT", [128, NDC, TT], BF16)
        sq = B.sbuf("sq", [128, NDC, TT], BF16)
        rstd = B.sbuf("rstd", [128, TT], F32)
        sg = [B.sbuf(f"sg{i}", [128, TT], F32) for i in range(2)]
        macc = B.sbuf("macc", [128, TT], F32)
        mt = B.sbuf("mt", [128, TT], F32)
        mrg = B.sbuf("mrg", [128, NDC, TT], BF16)
        for c in range(NDC):
            B.dma("pool", Wgt[:, c, :], w_in[l, c * 128:(c + 1) * 128, C_G:NIN], [], [("Wgt", c)], "w0")
            B.dma("pool", Wo[:, c, :], w_out[l, c * 128:(c + 1) * 128, :], [], [("Wo", c)], "w1")
        brch = [(0, 0), (0, 1), (0, 2), (1, 0), (1, 1), (1, 2), (2, 0), (2, 1)]
        for c, (br, j) in enumerate(brch):
            B.dma("pool", Wbr[:, c, :], w_br[br][l, j * 128:(j + 1) * 128, :], [], [("Wbr", c)], "w2")
        B.group([("Wgt", c) for c in range(NDC)], "w0")
        B.group([("Wo", c) for c in range(NDC)], "w1")
        B.group([("Wbr", c) for c in range(NDC)], "w2")

        def load(i):
            t0 = i * TT
            B.dma("sp", xt[i % 2][:], xsrc[:, t0:t0 + TT].rearrange("(c p) t -> p c t", p=128), [], [("xt", i % 2)], ("xt", i % 2))
            B.dma("sp", ot[i % 2][:], OT[:, t0:t0 + TT].rearrange("(c p) t -> p c t", p=128), [], [("ot", i % 2)], ("ot", i % 2))

        load(0)
        k = 0
        for i in range(NTT):
            if i + 1 < NTT:
                load(i + 1)
            X = xt[i % 2]
            xk = ("xt", i % 2)
            O = ot[i % 2]
            ok = ("ot", i % 2)
            rmsnorm_tile(X, xk, gi, hT, sq, rstd)
            brc = ((0, 1, 2), (3, 4, 5), (6, 7))
            for m in range(NDC):
                for br in range(3):
                    pg = 1 + (k % 2)
                    pbb = 3 + (k % 2)
                    sgi = k % 2
                    k += 1
                    col = br * D + m * 128
                    for c in range(NDC):
                        B.pe(lambda e, c=c, col=col, pg=pg: e.matmul(ps[pg][:], Wgt[:, c, col:col + 128], hT[:, c, :],
                                                                      start=(c == 0), stop=(c == NDC - 1)),
                             [("Wgt", c), ("hT", c)], [("ps", pg)])
                    cs_ = brc[br]
                    for ci, c in enumerate(cs_):
                        B.pe(lambda e, c=c, ci=ci, m=m, pbb=pbb, n_=len(cs_), O=O: e.matmul(ps[pbb][:], Wbr[:, c, m * 128:(m + 1) * 128], O[:, c, :],
                                                                                            start=(ci == 0), stop=(ci == n_ - 1)),
                             [("Wbr", c), ok], [("ps", pbb)])
                    B.act(lambda e, pg=pg, sgi=sgi, br=br, m=m: e.activation(out=sg[sgi][:], in_=ps[pg][:], func=AF.Sigmoid,
                                                                            bias=bgate[:, l, br, m:m + 1], scale=1.0),
                          [("ps", pg)], [("sg", sgi)])
                    if br == 0:
                        B.dve(lambda e, pbb=pbb, sgi=sgi: e.tensor_tensor(out=macc[:], in0=ps[pbb][:], in1=sg[sgi][:], op=ALU.mult),
                              [("ps", pbb), ("sg", sgi)], ["macc"])
                    else:
                        B.dve(lambda e, pbb=pbb, sgi=sgi: e.tensor_tensor(out=mt[:], in0=ps[pbb][:], in1=sg[sgi][:], op=ALU.mult),
                              [("ps", pbb), ("sg", sgi)], ["mt"])
                        if br == 1:
                            B.dve(lambda e: e.tensor_tensor(out=macc[:], in0=macc[:], in1=mt[:], op=ALU.add), ["macc", "mt"], ["macc"])
                        else:
                            B.dve(lambda e, m=m: e.tensor_tensor(out=mrg[:, m, :], in0=macc[:], in1=mt[:], op=ALU.add),
                                  ["macc", "mt"], [("mrg", m)])
            for e_ in range(NDC):
                py = 5 + (e_ % 2)
                for m in range(NDC):
                    B.pe(lambda e, e_=e_, m=m, py=py: e.matmul(ps[py][:], Wo[:, m, e_ * 128:(e_ + 1) * 128], mrg[:, m, :],
                                                               start=(m == 0), stop=(m == NDC - 1)),
                         [("Wo", m), ("mrg", m)], [("ps", py)])
                B.dve(lambda e, e_=e_, py=py, X=X: e.tensor_tensor(out=X[:, e_, :], in0=ps[py][:], in1=X[:, e_, :], op=ALU.add),
                      [("ps", py), xk], [xk])
            t0 = i * TT
            B.dma("sp", outT[:, t0:t0 + TT].rearrange("(c p) t -> p c t", p=128), X[:], [xk], [], ("xt", i % 2))
        B.end_phase()

    phase0()
    for l in range(DEPTH):
        if on(f"ffn1_{l}"):
            ffn_phase(l, 0, xT_in if l == 0 else outT, outT, l == 0)
        if on(f"proj_{l}"):
            proj_phase(l, outT)
        if on(f"fox_{l}"):
            fox_phase()
        if on(f"sb_{l}"):
            sb_phase()
        if on(f"dsa_{l}"):
            dsa_phase()
        if on(f"out_{l}"):
            out_phase(l, outT)
        if on(f"ffn2_{l}"):
            ffn_phase(l, 1, outT, outT, False)

    B.sc.close()
    for g in reversed(psg):
        g.__exit__(None, None, None)
    for g in reversed(B._glob):
        g.__exit__(None, None, None)
    return nc


_WNAMES = ("ffn1_norm", "ffn1_w_gate", "ffn1_w_up", "ffn1_w_down", "mix_norm", "w_in", "b_forget", "b_gates",
           "q_norm_fox", "k_norm_fox", "q_norm_sb", "k_norm_sb", "q_norm_dsa", "k_norm_dsa",
           "w_branch_fox", "w_branch_sb", "w_branch_dsa", "w_out",
           "ffn2_norm", "ffn2_w_gate", "ffn2_w_up", "ffn2_w_down")


def kernel(**inputs):
    n = 8
    phases = inputs.pop("_phases", ("all",))
    ncores = inputs.pop("_ncores", n)
    nc = build_program(phases)
    x = np.asarray(inputs["x"])
    pos = np.asarray(inputs["positions"]).astype(np.int32)
    shared = {k: np.ascontiguousarray(np.asarray(inputs[k], dtype=np.float32)) for k in _WNAMES}
    shared.update(_CONST)
    in_maps = []
    for b in range(ncores):
        m = dict(shared)
        m["xT"] = np.ascontiguousarray(x[b].T)
        m["pos"] = np.ascontiguousarray(pos[b:b + 1])
        in_maps.append(m)
    res = run_bass_kernel_spmd(nc, in_maps, core_ids=list(range(ncores)))
    if any(p.startswith("dbgOT") for p in phases):
        return res.results[0]["OT"]
    out = np.stack([np.ascontiguousarray(r["outT"].T) for r in res.results], axis=0)
    return out.astype(np.float32, copy=False)
```

```python
import numpy as np
import concourse.bass as bass
import concourse.mybir as mybir
from concourse.bass_utils import run_bass_kernel_spmd

F32 = mybir.dt.float32
BF16 = mybir.dt.bfloat16
I32 = mybir.dt.int32
AF = mybir.ActivationFunctionType
ALU = mybir.AluOpType
AX = mybir.AxisListType

D = 1024
S = 4096
DEPTH = 2
DFF = 2816
NFC = DFF // 128
NDC = D // 128
TT = 512
NTT = S // TT
NKT = S // 128
EPS = 1e-6
BIG = 30000.0
TOPK = 256
NBIS = 24

C_QF, C_KF, C_VF, C_FF = 0, 384, 768, 1152
C_QS, C_KS, C_VS = 1158, 1542, 1926
C_QC, C_KC, C_VC = 2310, 2566, 2822
C_QI, C_KI, C_WI = 3078, 3334, 3398
C_G = 3402
NIN = 6474


class Sched:
    ENG = ("pe", "act", "dve", "pool", "sp")

    def __init__(self, nc):
        self.nc = nc
        self._new_phase()
        self._reset()

    def _new_phase(self):
        self.known = {e: {} for e in self.ENG}
        self.chan_n = {}
        self.n = {e: 0 for e in self.ENG}
        self.sig_base = {e: 0 for e in self.ENG}
        self.sems = {}
        self.chan_sems = {}

    def _reset(self):
        self.ops = {e: [] for e in self.ENG}
        self.lastw = {}
        self.readers = {}
        self.signal = set()

    def add(self, eng, emit, reads=(), writes=(), dma=False, chan=None):
        deps = {}

        def need(d):
            for sk, o in d.items():
                if deps.get(sk, 0) < o:
                    deps[sk] = o

        for r in reads:
            need(self.lastw.get(r, {}))
        for w in writes:
            need(self.lastw.get(w, {}))
            need(self.readers.get(w, {}))
        if dma:
            self.chan_n[chan] = self.chan_n.get(chan, 0) + 1
            me = (("ch", chan), self.chan_n[chan])
        else:
            self.n[eng] += 1
            me = (eng, self.n[eng])
        waits = []
        kn = self.known[eng]
        for sk, o in deps.items():
            if sk == "pe" and eng == "pe" and not dma:
                continue
            if kn.get(sk, 0) >= o:
                continue
            kn[sk] = o
            waits.append((sk, o))
            if not isinstance(sk, tuple):
                self.signal.add((sk, o))
        for w in writes:
            self.lastw[w] = {me[0]: me[1]}
            self.readers[w] = {}
        for r in reads:
            rd = self.readers.setdefault(r, {})
            if rd.get(me[0], 0) < me[1]:
                rd[me[0]] = me[1]
        self.ops[eng].append(dict(emit=emit, waits=waits, me=me, dma=dma))
        return me

    def end_phase(self):
        nc = self.nc
        waits = []
        for chan, n in self.chan_n.items():
            sk = ("ch", chan)
            if self.known["sp"].get(sk, 0) < n:
                waits.append((sk, n))
                self.known["sp"][sk] = n
        self.ops["sp"].append(dict(emit=None, waits=waits, me=None, dma=False))
        self._pid = getattr(self, "_pid", 0) + 1
        snap = nc.snapshot_sems()
        for e in self.ENG:
            self.sems[e] = nc.alloc_semaphore(f"s{self._pid}_{e}")
        for i, ch in enumerate(self.chan_n):
            self.chan_sems[ch] = nc.alloc_semaphore(f"s{self._pid}_ch{i}")
        sigrank = {}
        cnt = {}
        for e in self.ENG:
            ords = sorted(o for (sk, o) in self.signal if sk == e)
            for i, o in enumerate(ords):
                sigrank[(e, o)] = self.sig_base[e] + i + 1
            cnt[e] = len(ords)

        def semval(sk, o):
            if isinstance(sk, tuple):
                return self.chan_sems[sk[1]], 16 * o
            return self.sems[sk], sigrank[(sk, o)]

        ops = self.ops
        signal = self.signal

        def run(eng_name):
            def body(eng):
                for op in ops[eng_name]:
                    for sk, o in op["waits"]:
                        s, v = semval(sk, o)
                        eng.wait_ge(s, v)
                    if op["emit"] is None:
                        continue
                    ins = op["emit"](eng)
                    me = op["me"]
                    if op["dma"]:
                        ins.then_inc(self.chan_sems[me[0][1]], 16)
                    elif me in signal:
                        ins.then_inc(self.sems[me[0]], 1)
            return body

        with nc.Block() as block:
            block.tensor(run("pe"))
            block.scalar(run("act"))
            block.vector(run("dve"))
            block.gpsimd(run("pool"))
            block.sync(run("sp"))
        nc.clear_and_free_semaphores(nc.allocated_since(snap))
        nc.all_engine_barrier()
        self._new_phase()
        self._reset()

    def close(self):
        pass


class Builder:
    def __init__(self):
        self.nc = bass.Bass("TRN2", target_bir_lowering=False)
        self.sc = Sched(self.nc)
        self._glob = []
        self._scope = []

    def din(self, name, shape, dt=F32):
        return self.nc.dram_tensor(name, list(shape), dt, kind="ExternalInput").ap()

    def dout(self, name, shape, dt=F32):
        return self.nc.dram_tensor(name, list(shape), dt, kind="ExternalOutput").ap()

    def dscr(self, name, shape, dt=F32):
        return self.nc.dram_tensor(name, list(shape), dt, kind="Internal").ap()

    def sbuf(self, name, shape, dt, glob=False):
        self._uid = getattr(self, "_uid", 0) + 1
        g = self.nc.sbuf_tensor(f"{name}_{self._uid}", list(shape), dt)
        t = g.__enter__()
        (self._glob if glob else self._scope).append(g)
        return t

    def end_phase(self):
        self.sc.end_phase()
        for g in reversed(self._scope):
            g.__exit__(None, None, None)
        self._scope = []

    def pe(self, emit, reads, writes):
        return self.sc.add("pe", emit, reads, writes)

    def act(self, emit, reads, writes):
        return self.sc.add("act", emit, reads, writes)

    def dve(self, emit, reads, writes):
        return self.sc.add("dve", emit, reads, writes)

    def pool(self, emit, reads, writes):
        return self.sc.add("pool", emit, reads, writes)

    def group(self, keys, chan):
        n = self.sc.chan_n[chan]
        for k in keys:
            self.sc.lastw[k] = {("ch", chan): n}

    def dma(self, q, out, in_, reads, writes, chan, slow=False):
        return self.sc.add(q, lambda e: e.dma_start(out=out, in_=in_, allow_slow_non_contiguous=slow),
                           reads, writes, dma=True, chan=chan)


def _consts():
    c = {}
    c["c_ones"] = np.ones((128, 128), np.float32)
    bo = np.zeros((128, 128), np.float32)
    bo[:64, :64] = 1
    bo[64:, 64:] = 1
    c["c_blockones"] = bo
    rm = np.zeros((128, 128), np.float32)
    for hh in (0, 64):
        for j in range(8):
            rm[hh + 8 + j, hh + j] = -1.0
            rm[hh + j, hh + 8 + j] = 1.0
    c["c_rmat"] = rm
    c["c_ident"] = np.eye(128, dtype=np.float32)
    j = np.arange(128)[:, None]
    s = np.arange(128)[None, :]
    c["c_negtri"] = -(j >= s).astype(np.float32)
    t = np.arange(512)[None, None, :]
    i4 = np.arange(4)[None, :, None]
    jj = np.arange(128)[:, None, None]
    kg = 128 * i4 + jj
    c["c_mbig_fox"] = (-BIG * (kg > t)).astype(np.float32)
    c["c_mbig_sb"] = (-BIG * (kg >= t)).astype(np.float32)
    c["c_m01_sb"] = (kg < t).astype(np.float32)
    es = np.zeros((128, 32, 64), np.float32)
    for kt in range(32):
        es[:, kt, kt] = 1
        es[:, kt, 32 + kt] = 1
    c["c_esel2"] = es
    ns = np.zeros((64, 32, 128), np.float32)
    for kt in range(32):
        for r in range(64):
            if (r % 32) > kt:
                ns[r, kt, :] = -1
    c["c_negsel"] = ns
    c["c_nbident"] = (-BIG * np.eye(128)).astype(np.float32)
    q = np.arange(128)[:, None]
    k = np.arange(128)[None, :]
    c["c_causal"] = (-1e30 * (k > q)).astype(np.float32)
    invf = np.zeros((1, 128), np.float32)
    half = 8
    f = (500000.0 ** (-np.arange(half, dtype=np.float32) * 2.0 / 16.0)).astype(np.float32)
    for hh in (0, 64):
        invf[0, hh:hh + 8] = f
        invf[0, hh + 8:hh + 16] = f
    c["c_invf"] = invf
    ed = np.zeros((65, 64), np.float32)
    ed[64, :] = 1
    c["c_edenom"] = ed
    return c


_CONST = _consts()


def build_program(phases=("all",)):
    B = Builder()
    nc = B.nc
    ALLP = "all" in phases

    def on(p):
        return ALLP or p in phases

    xT_in = B.din("xT", [D, S])
    pos_in = B.din("pos", [1, S], I32)
    ffn_w = {}
    for nm in ("ffn1", "ffn2"):
        ffn_w[nm] = dict(
            norm=B.din(nm + "_norm", [DEPTH, D]),
            wg=B.din(nm + "_w_gate", [DEPTH, D, DFF]),
            wu=B.din(nm + "_w_up", [DEPTH, D, DFF]),
            wd=B.din(nm + "_w_down", [DEPTH, DFF, D]),
        )
    mix_norm = B.din("mix_norm", [DEPTH, D])
    w_in = B.din("w_in", [DEPTH, D, NIN])
    b_forget = B.din("b_forget", [DEPTH, 6])
    b_gates = B.din("b_gates", [DEPTH, 3, D])
    hn_names = ("q_norm_fox", "k_norm_fox", "q_norm_sb", "k_norm_sb", "q_norm_dsa", "k_norm_dsa")
    hn = {n: B.din(n, [DEPTH, 64]) for n in hn_names}
    w_br = [B.din("w_branch_fox", [DEPTH, 384, D]), B.din("w_branch_sb", [DEPTH, 384, D]),
            B.din("w_branch_dsa", [DEPTH, 256, D])]
    w_out = B.din("w_out", [DEPTH, D, D])
    cd = {k: B.din(k, list(v.shape)) for k, v in _CONST.items()}
    outT = B.dout("outT", [D, S])

    QfA = B.dscr("QfA", [6, 68, S], BF16)
    KfA = B.dscr("KfA", [6, 68, S], BF16)
    QsT = B.dscr("QsT", [6, 64, S], BF16)
    KsT = B.dscr("KsT", [6, 64, S], BF16)
    QcT = B.dscr("QcT", [4, 64, S], BF16)
    KcT = B.dscr("KcT", [4, 64, S], BF16)
    QiT = B.dscr("QiT", [4, 64, S], BF16)
    KiT = B.dscr("KiT", [64, S], BF16)
    Vtok = B.dscr("Vtok", [S, 1024], BF16)
    WiD = B.dscr("WiD", [S, 4], F32)
    NLF = B.dscr("NLF", [6, S], F32)
    CosD = B.dscr("CosD", [128, S], F32)
    SinD = B.dscr("SinD", [128, S], F32)
    dbg = [p for p in phases if p.startswith("dbgOT")]
    if dbg:
        OT = B.dout("OT", [D, S], BF16)
    else:
        OT = B.dscr("OT", [D, S], BF16)

    ones = B.sbuf("ones", [128, 128], BF16, glob=True)
    blockones = B.sbuf("blockones", [128, 128], BF16, glob=True)
    rmat = B.sbuf("rmat", [128, 128], BF16, glob=True)
    ident = B.sbuf("ident", [128, 128], BF16, glob=True)
    negtri = B.sbuf("negtri", [128, 128], BF16, glob=True)
    nbident = B.sbuf("nbident", [128, 128], BF16, glob=True)
    mbz = B.sbuf("mbz", [128, 512], BF16, glob=True)
    epsb = B.sbuf("epsb", [128, 1], F32, glob=True)
    oneb = B.sbuf("oneb", [128, 1], F32, glob=True)
    negpi = B.sbuf("negpi", [128, 1], F32, glob=True)
    gvec = B.sbuf("gvec", [128, 6, NDC], F32, glob=True)
    hg = B.sbuf("hg", [128, DEPTH, 6], F32, glob=True)
    negb = B.sbuf("negb", [6, DEPTH], F32, glob=True)
    bgate = B.sbuf("bgate", [128, DEPTH, 3, NDC], F32, glob=True)

    psg = [nc.psum_tensor(f"ps{i}", [128, 512], F32) for i in range(8)]
    ps = [g.__enter__() for g in psg]

    def phase0():
        B.dma("pool", ones[:], cd["c_ones"][:, :], [], ["c"], "c0")
        B.dma("pool", blockones[:], cd["c_blockones"][:, :], [], ["c"], "c0")
        B.dma("pool", rmat[:], cd["c_rmat"][:, :], [], ["c"], "c0")
        B.dma("pool", ident[:], cd["c_ident"][:, :], [], ["c"], "c0")
        B.dma("pool", negtri[:], cd["c_negtri"][:, :], [], ["c"], "c0")
        B.dma("pool", nbident[:], cd["c_nbident"][:, :], [], ["c"], "c0")
        B.dve(lambda e: e.memset(epsb[:], EPS), [], ["epsb"])
        B.dve(lambda e: e.memset(oneb[:], 1.0), [], ["oneb"])
        B.dve(lambda e: e.memset(negpi[:], -float(np.pi)), [], ["negpi"])
        B.dve(lambda e: e.memset(mbz[:], -BIG), [], ["mbz"])
        for l in range(DEPTH):
            for wi, src in enumerate((ffn_w["ffn1"]["norm"], mix_norm, ffn_w["ffn2"]["norm"])):
                B.dma("sp", gvec[:, l * 3 + wi, :], src[l].rearrange("(c p) -> p c", p=128),
                      [], [("gvec", l, wi)], "c_gvec", slow=True)
            for ni, n in enumerate(hn_names):
                for hh in (0, 64):
                    B.dma("sp", hg[hh:hh + 64, l, ni:ni + 1], hn[n][l].rearrange("(d o) -> d o", o=1),
                          [], [("hg", l, ni, hh)], "c_hg", slow=True)
            for br in range(3):
                B.dma("sp", bgate[:, l, br, :], b_gates[l, br].rearrange("(c p) -> p c", p=128),
                      [], [("bgate", l, br)], "c_bgate", slow=True)
        B.group(["hg"], "c_hg")
        B.dma("sp", negb[:], b_forget.rearrange("l h -> h l"), [], ["negb"], "c_negb", slow=True)
        B.dve(lambda e: e.tensor_scalar(out=negb[:], in0=negb[:], scalar1=-1.0, scalar2=None, op0=ALU.mult),
              ["negb"], ["negb"])
        for ni in (0, 2, 4):
            B.dve(lambda e, ni=ni: e.tensor_scalar(out=hg[:, :, ni:ni + 1], in0=hg[:, :, ni:ni + 1], scalar1=0.125,
                                                   scalar2=None, op0=ALU.mult), ["hg"], ["hg"])
        posi = B.sbuf("posi", [1, S], I32)
        posf = B.sbuf("posf", [1, S], F32)
        invf = B.sbuf("invf", [1, 128], F32)
        ang = B.sbuf("ang", [128, 512], F32)
        angi = B.sbuf("angi", [128, 512], I32)
        angf = B.sbuf("angf", [128, 512], F32)
        tb = [B.sbuf(f"tb{i}", [128, 512], F32) for i in range(2)]
        B.dma("sp", posi[:], pos_in[:, :], [], ["posi"], "c2")
        B.dma("sp", invf[:], cd["c_invf"][:, :], [], ["invf"], "c2")
        B.dve(lambda e: e.tensor_copy(out=posf[:], in_=posi[:]), ["posi"], ["posf"])
        for i in range(NTT):
            t0 = i * TT
            B.pe(lambda e, t0=t0: e.matmul(ps[0][:], invf[:], posf[:, t0:t0 + TT], start=True, stop=True),
                 ["invf", "posf"], [("ps", 0)])
            for k, (shift, dst) in enumerate(((0.0, SinD), (0.25, CosD))):
                B.dve(lambda e, shift=shift: e.tensor_scalar(out=ang[:], in0=ps[0][:], scalar1=float(1.0 / (2 * np.pi)),
                                                             scalar2=float(shift), op0=ALU.mult, op1=ALU.add),
                      [("ps", 0)], ["ang"])
                B.dve(lambda e: e.tensor_copy(out=angi[:], in_=ang[:]), ["ang"], ["angi"])
                B.dve(lambda e: e.tensor_copy(out=angf[:], in_=angi[:]), ["angi"], ["angf"])
                B.dve(lambda e: e.tensor_tensor(out=ang[:], in0=ang[:], in1=angf[:], op=ALU.subtract), ["ang", "angf"], ["ang"])
                B.dve(lambda e: e.tensor_scalar(out=angf[:], in0=ang[:], scalar1=0.5, scalar2=None, op0=ALU.is_gt), ["ang"], ["angf"])
                B.dve(lambda e: e.tensor_tensor(out=ang[:], in0=ang[:], in1=angf[:], op=ALU.subtract), ["ang", "angf"], ["ang"])
                B.act(lambda e, k=k: e.activation(out=tb[k][:], in_=ang[:], func=AF.Sin, scale=float(2 * np.pi)),
                      ["ang"], [("tb", k)])
                B.dma("sp", dst[:, t0:t0 + TT], tb[k][:], [("tb", k)], [], ("tb", k))
        B.end_phase()

    def rmsnorm_tile(X, xk, gi, hT, sq, rstd):
        for c in range(NDC):
            B.dve(lambda e, c=c: e.tensor_tensor(out=sq[:, c, :], in0=X[:, c, :], in1=X[:, c, :], op=ALU.mult),
                  [xk], [("sq", c)])
        for c in range(NDC):
            B.pe(lambda e, c=c: e.matmul(ps[0][:], ones[:], sq[:, c, :], start=(c == 0), stop=(c == NDC - 1)),
                 [("sq", c)], [("ps", 0)])
        B.act(lambda e: e.activation(out=rstd[:], in_=ps[0][:], func=AF.Sqrt, scale=1.0 / D, bias=epsb[:, 0:1]),
              [("ps", 0)], ["rstd"])
        B.dve(lambda e: e.reciprocal(out=rstd[:], in_=rstd[:]), ["rstd"], ["rstd"])
        for c in range(NDC):
            B.dve(lambda e, c=c: e.scalar_tensor_tensor(out=hT[:, c, :], in0=X[:, c, :], scalar=gvec[:, gi, c:c + 1],
                                                        in1=rstd[:], op0=ALU.mult, op1=ALU.mult),
                  [xk, "rstd"], [("hT", c)])

    def ffn_phase(l, wi, src, dst, first):
        nm = ("ffn1", "ffn2")[wi]
        w = ffn_w[nm]
        gi = l * 3 + (0, 2)[wi]
        Wg = B.sbuf("Wg", [128, NDC, DFF], BF16)
        Wu = B.sbuf("Wu", [128, NDC, DFF], BF16)
        Wd = B.sbuf("Wd", [128, NFC, D], BF16)
        xt = [B.sbuf(f"xt{i}", [128, NDC, TT], F32) for i in range(2)]
        hT = B.sbuf("hT", [128, NDC, TT], BF16)
        actT = B.sbuf("actT", [128, NFC, TT], BF16)
        sq = actT
        rstd = B.sbuf("rstd", [128, TT], F32)
        sil = [B.sbuf(f"sil{i}", [128, TT], F32) for i in range(2)]
        for c in range(NDC):
            B.dma("pool", Wg[:, c, :], w["wg"][l, c * 128:(c + 1) * 128, :], [], [("Wg", c)], "w0")
            B.dma("pool", Wu[:, c, :], w["wu"][l, c * 128:(c + 1) * 128, :], [], [("Wu", c)], "w1")
        for j in range(NFC):
            B.dma("pool", Wd[:, j, :], w["wd"][l, j * 128:(j + 1) * 128, :], [], [("Wd", j)], "w2")
        B.group([("Wg", c) for c in range(NDC)], "w0")
        B.group([("Wu", c) for c in range(NDC)], "w1")
        B.group([("Wd", j) for j in range(NFC)], "w2")

        def load_x(i):
            t0 = i * TT
            B.dma("sp", xt[i % 2][:], src[:, t0:t0 + TT].rearrange("(c p) t -> p c t", p=128),
                  [], [("xt", i % 2)], ("xt", i % 2))

        load_x(0)
        for i in range(NTT):
            if i + 1 < NTT:
                load_x(i + 1)
            X = xt[i % 2]
            xk = ("xt", i % 2)
            rmsnorm_tile(X, xk, gi, hT, sq, rstd)
            for j in range(NFC):
                pa = 1 + (j % 2)
                pu = 3 + (j % 2)
                for c in range(NDC):
                    B.pe(lambda e, c=c, j=j, pa=pa: e.matmul(ps[pa][:], Wg[:, c, j * 128:(j + 1) * 128], hT[:, c, :],
                                                             start=(c == 0), stop=(c == NDC - 1)),
                         [("Wg", c), ("hT", c)], [("ps", pa)])
                for c in range(NDC):
                    B.pe(lambda e, c=c, j=j, pu=pu: e.matmul(ps[pu][:], Wu[:, c, j * 128:(j + 1) * 128], hT[:, c, :],
                                                             start=(c == 0), stop=(c == NDC - 1)),
                         [("Wu", c), ("hT", c)], [("ps", pu)])
                B.act(lambda e, j=j, pa=pa: e.activation(out=sil[j % 2][:], in_=ps[pa][:], func=AF.Silu),
                      [("ps", pa)], [("sil", j % 2)])
                B.dve(lambda e, j=j, pu=pu: e.tensor_tensor(out=actT[:, j, :], in0=ps[pu][:], in1=sil[j % 2][:], op=ALU.mult),
                      [("ps", pu), ("sil", j % 2)], [("sq", j)])
            for m in range(NDC):
                py = 5 + (m % 2)
                for j in range(NFC):
                    B.pe(lambda e, m=m, j=j, py=py: e.matmul(ps[py][:], Wd[:, j, m * 128:(m + 1) * 128], actT[:, j, :],
                                                             start=(j == 0), stop=(j == NFC - 1)),
                         [("Wd", j), ("sq", j)], [("ps", py)])
                B.dve(lambda e, m=m, py=py, X=X: e.scalar_tensor_tensor(out=X[:, m, :], in0=ps[py][:], scalar=0.5, in1=X[:, m, :],
                                                                       op0=ALU.mult, op1=ALU.add),
                      [("ps", py), xk], [xk])
            t0 = i * TT
            B.dma("sp", dst[:, t0:t0 + TT].rearrange("(c p) t -> p c t", p=128), X[:],
                  [xk], [], ("xt", i % 2))
        B.end_phase()

    def proj_phase(l, src):
        gi = l * 3 + 1
        NW = C_G
        Win = B.sbuf("Win", [128, NDC, NW], BF16)
        xt = [B.sbuf(f"xt{i}", [128, NDC, TT], F32) for i in range(2)]
        hT = B.sbuf("hT", [128, NDC, TT], BF16)
        sq = B.sbuf("sq", [128, NDC, TT], BF16)
        rstd = B.sbuf("rstd", [128, TT], F32)
        q32_ = [B.sbuf("q32%d" % i_, [128, TT], F32) for i_ in range(2)]
        qsq_ = [B.sbuf("qsq%d" % i_, [128, TT], BF16) for i_ in range(2)]
        qr_ = [B.sbuf("qr%d" % i_, [128, TT], F32) for i_ in range(2)]
        qn32_ = [B.sbuf("qn32%d" % i_, [128, TT], F32) for i_ in range(2)]
        qnt_ = [B.sbuf("qnt%d" % i_, [128, TT], BF16) for i_ in range(2)]
        t1_ = [B.sbuf("t1%d" % i_, [128, TT], F32) for i_ in range(2)]
        t2_ = [B.sbuf("t2%d" % i_, [128, TT], F32) for i_ in range(2)]
        qnb = [B.sbuf(f"qnb{i}", [128, TT], BF16) for i in range(2)]
        cosT = B.sbuf("cosT", [128, TT], F32)
        sinT = B.sbuf("sinT", [128, TT], F32)
        vst = B.sbuf("vst", [128, 4, 1024], BF16)
        wist = B.sbuf("wist", [128, 4, 4], F32)
        lfe = B.sbuf("lfe", [6, TT], F32)
        nlft = B.sbuf("nlft", [6, TT], F32)

        for c in range(NDC):
            B.dma("pool", Win[:, c, :], w_in[l, c * 128:(c + 1) * 128, 0:NW], [], [("Win", c)], "w0")
        B.group([("Win", c) for c in range(NDC)], "w0")

        def load_x(i):
            t0 = i * TT
            B.dma("sp", xt[i % 2][:], src[:, t0:t0 + TT].rearrange("(c p) t -> p c t", p=128),
                  [], [("xt", i % 2)], ("xt", i % 2))

        pairs = []
        for p in range(3):
            pairs.append((C_QF + 128 * p, 128, 0, False, ("A", QfA, 2 * p)))
            pairs.append((C_KF + 128 * p, 128, 1, False, ("A", KfA, 2 * p)))
            pairs.append((C_QS + 128 * p, 128, 2, False, ("T", QsT, 2 * p)))
            pairs.append((C_KS + 128 * p, 128, 3, False, ("T", KsT, 2 * p)))
        for p in range(2):
            pairs.append((C_QC + 128 * p, 128, 4, True, ("T", QcT, 2 * p)))
            pairs.append((C_KC + 128 * p, 128, 5, True, ("T", KcT, 2 * p)))
            pairs.append((C_QI + 128 * p, 128, None, True, ("T", QiT, 2 * p)))
        pairs.append((C_KI, 64, None, True, ("K", KiT, 0)))

        load_x(0)
        for i in range(NTT):
            t0 = i * TT
            if i + 1 < NTT:
                load_x(i + 1)
            X = xt[i % 2]
            xk = ("xt", i % 2)
            B.dma("sp", cosT[:], CosD[:, t0:t0 + TT], [], ["cosT"], "cosT")
            B.dma("sp", sinT[:], SinD[:, t0:t0 + TT], [], ["sinT"], "sinT")
            rmsnorm_tile(X, xk, gi, hT, sq, rstd)
            def proj_mm(pi):
                col0, M = pairs[pi][0], pairs[pi][1]
                pb = 1 + (pi % 2)
                for c in range(NDC):
                    B.pe(lambda e, c=c, col0=col0, M=M, pb=pb: e.matmul(ps[pb][0:M, :], Win[:, c, col0:col0 + M], hT[:, c, :],
                                                                         start=(c == 0), stop=(c == NDC - 1)),
                         [("Win", c), ("hT", c)], [("ps", pb)])

            proj_mm(0)
            def pair_body(pi, col0, M, gidx, rope, dstd):
                pb = 1 + (pi % 2)
                slot = pi % 2
                if pi + 1 < len(pairs):
                    proj_mm(pi + 1)
                q32, qsq, qr, qn32, qnt, t1, t2 = (q32_[slot], qsq_[slot], qr_[slot], qn32_[slot], qnt_[slot], t1_[slot], t2_[slot])
                final = qnb[slot]
                fk = ("qnb", slot)
                if gidx is not None:
                    B.act(lambda e, pb=pb: e.activation(out=q32[:], in_=ps[pb][:], func=AF.Copy), [("ps", pb)], [("q32", slot)])
                    B.dve(lambda e: e.tensor_tensor(out=qsq[:], in0=q32[:], in1=q32[:], op=ALU.mult), [("q32", slot)], [("qsq", slot)])
                    B.pe(lambda e: e.matmul(ps[3][:], blockones[:], qsq[:], start=True, stop=True), [("qsq", slot)], [("ps", 3)])
                    B.act(lambda e: e.activation(out=qr[:], in_=ps[3][:], func=AF.Sqrt, scale=1.0 / 64, bias=epsb[:, 0:1]),
                          [("ps", 3)], [("qr", slot)])
                    B.dve(lambda e: e.reciprocal(out=qr[:], in_=qr[:]), [("qr", slot)], [("qr", slot)])
                    tgt = qn32 if rope else final
                    B.dve(lambda e, tgt=tgt, gidx=gidx: e.scalar_tensor_tensor(out=tgt[:], in0=q32[:], scalar=hg[:, l, gidx:gidx + 1],
                                                                              in1=qr[:], op0=ALU.mult, op1=ALU.mult),
                          [("q32", slot), ("qr", slot)], [("qn32", slot) if rope else fk])
                else:
                    B.act(lambda e, pb=pb, M=M: e.activation(out=qn32[0:M, :], in_=ps[pb][0:M, :], func=AF.Copy),
                          [("ps", pb)], [("qn32", slot)])
                if rope:
                    B.dve(lambda e, M=M: e.tensor_copy(out=qnt[0:M, :], in_=qn32[0:M, :]), [("qn32", slot)], [("qnt", slot)])
                    B.pe(lambda e, M=M: e.matmul(ps[4][0:M, :], rmat[0:M, 0:M], qnt[0:M, :], start=True, stop=True),
                         [("qnt", slot)], [("ps", 4)])
                    B.dve(lambda e, M=M: e.tensor_tensor(out=t1[0:M, :], in0=qn32[0:M, :], in1=cosT[0:M, :], op=ALU.mult),
                          [("qn32", slot), "cosT"], [("t1", slot)])
                    B.dve(lambda e, M=M: e.tensor_tensor(out=t2[0:M, :], in0=ps[4][0:M, :], in1=sinT[0:M, :], op=ALU.mult),
                          [("ps", 4), "sinT"], [("t2", slot)])
                    B.dve(lambda e, M=M, final=final: e.tensor_tensor(out=final[0:M, :], in0=t1[0:M, :], in1=t2[0:M, :], op=ALU.add),
                          [("t1", slot), ("t2", slot)], [fk])
                kind, dt_, h0 = dstd
                if kind == "A":
                    for hh in range(2):
                        B.dma("sp", dt_[h0 + hh, 0:64, t0:t0 + TT], final[hh * 64:(hh + 1) * 64, :], [fk], [], fk)
                elif kind == "T":
                    B.dma("sp", dt_[h0:h0 + 2, :, t0:t0 + TT].rearrange("h d t -> (h d) t"), final[:], [fk], [], fk)
                else:
                    B.dma("sp", dt_[:, t0:t0 + TT], final[0:64, :], [fk], [], fk)
            for pi_, pr_ in enumerate(pairs):
                pair_body(pi_, *pr_)
            k = 0
            for sub in range(4):
                for (col, n, d0) in ((C_VF, 384, 0), (C_VS, 384, 384), (C_VC, 256, 768)):
                    pv = 5 + (k % 2)
                    k += 1
                    for c in range(NDC):
                        B.pe(lambda e, c=c, sub=sub, col=col, n=n, pv=pv: e.matmul(
                            ps[pv][:, 0:n], hT[:, c, sub * 128:(sub + 1) * 128], Win[:, c, col:col + n],
                            start=(c == 0), stop=(c == NDC - 1)), [("Win", c), ("hT", c)], [("ps", pv)])
                    B.act(lambda e, sub=sub, n=n, d0=d0, pv=pv: e.activation(out=vst[:, sub, d0:d0 + n], in_=ps[pv][:, 0:n], func=AF.Copy),
                          [("ps", pv)], ["vst"])
                for c in range(NDC):
                    B.pe(lambda e, c=c, sub=sub: e.matmul(ps[7][:, 0:4], hT[:, c, sub * 128:(sub + 1) * 128], Win[:, c, C_WI:C_WI + 4],
                                                          start=(c == 0), stop=(c == NDC - 1)), [("Win", c), ("hT", c)], [("ps", 7)])
                B.dve(lambda e, sub=sub: e.tensor_scalar(out=wist[:, sub, :], in0=ps[7][:, 0:4], scalar1=1.0 / 16, scalar2=None, op0=ALU.mult),
                      [("ps", 7)], ["wist"])
            B.dma("sp", Vtok[t0:t0 + TT, :].rearrange("(s p) n -> p s n", p=128), vst[:], ["vst"], [], "vst")
            B.dma("sp", WiD[t0:t0 + TT, :].rearrange("(s p) n -> p s n", p=128), wist[:], ["wist"], [], "wist", slow=True)
            for c in range(NDC):
                B.pe(lambda e, c=c: e.matmul(ps[0][0:6, :], Win[:, c, C_FF:C_FF + 6], hT[:, c, :], start=(c == 0), stop=(c == NDC - 1)),
                     [("Win", c), ("hT", c)], [("ps", 0)])
            B.act(lambda e: e.activation(out=lfe[:], in_=ps[0][0:6, :], func=AF.Exp, scale=-1.0, bias=negb[:, l:l + 1]),
                  [("ps", 0), "negb"], ["lfe"])
            B.act(lambda e: e.activation(out=nlft[:], in_=lfe[:], func=AF.Ln, bias=oneb[0:6, 0:1], scale=1.0),
                  ["lfe"], ["nlft"])
            B.dma("sp", NLF[:, t0:t0 + TT], nlft[:], ["nlft"], [], "nlft")
        B.end_phase()
        nlf = B.sbuf("nlf", [6, S], F32)
        ncum = B.sbuf("ncum", [6, S], F32)
        one6 = B.sbuf("one6", [6, S], F32)
        hi6 = B.sbuf("hi6", [6, S], BF16)
        lo6 = B.sbuf("lo6", [6, S], BF16)
        nhi6 = B.sbuf("nhi6", [6, S], BF16)
        nlo6 = B.sbuf("nlo6", [6, S], BF16)
        ob6 = B.sbuf("ob6", [6, S], BF16)
        B.dma("sp", nlf[:], NLF[:, :], [], ["nlf"], "nlf")
        B.dve(lambda e: e.memset(one6[:], 1.0), [], ["one6"])
        B.dve(lambda e: e.memset(ob6[:], 1.0), [], ["ob6"])
        B.dve(lambda e: e.tensor_tensor_scan(out=ncum[:], data0=one6[:], data1=nlf[:], initial=0.0, op0=ALU.mult, op1=ALU.add),
              ["one6", "nlf"], ["ncum"])
        B.dve(lambda e: e.tensor_copy(out=hi6[:], in_=ncum[:]), ["ncum"], ["hi6"])
        B.dve(lambda e: e.tensor_tensor(out=lo6[:], in0=ncum[:], in1=hi6[:], op=ALU.subtract), ["ncum", "hi6"], ["lo6"])
        B.dve(lambda e: e.tensor_scalar(out=nhi6[:], in0=hi6[:], scalar1=-1.0, scalar2=None, op0=ALU.mult), ["hi6"], ["nhi6"])
        B.dve(lambda e: e.tensor_scalar(out=nlo6[:], in0=lo6[:], scalar1=-1.0, scalar2=None, op0=ALU.mult), ["lo6"], ["nlo6"])
        for row, t_, k_ in ((64, nhi6, "nhi6"), (65, nlo6, "nlo6"), (66, ob6, "ob6"), (67, ob6, "ob6")):
            B.dma("sp", QfA[:, row, :], t_[:], [k_], [], "aug")
        for row, t_, k_ in ((64, ob6, "ob6"), (65, ob6, "ob6"), (66, hi6, "hi6"), (67, lo6, "lo6")):
            B.dma("sp", KfA[:, row, :], t_[:], [k_], [], "aug")
        B.end_phase()

    def fox_phase():
        Ka = [B.sbuf(f"Ka{i}", [68, S], BF16) for i in range(2)]
        Qa = [B.sbuf(f"Qa{i}", [68, S], BF16) for i in range(2)]
        V = [B.sbuf(f"V{i}", [128, NKT, 65], BF16) for i in range(2)]
        mb = B.sbuf("mb", [128, 4, 512], BF16)
        ed = B.sbuf("ed", [65, 64], F32)
        pT = [B.sbuf(f"pT{i}", [128, 512], BF16) for i in range(3)]
        osb = B.sbuf("osb", [65, 512], F32)
        rec = B.sbuf("rec", [64, 512], F32)
        ost = [B.sbuf(f"ost{i}", [64, 512], BF16) for i in range(2)]
        B.dma("pool", mb[:], cd["c_mbig_fox"][:, :, :], [], ["mb"], "c0")
        B.dma("sp", ed[:], cd["c_edenom"][:, :], [], ["ed"], "c2")
        for i in range(2):
            B.dve(lambda e, i=i: e.memset(V[i][:, :, 64:65], 1.0), [], [("Vone", i)])

        def load_head(h):
            b = h % 2
            B.dma("sp", Ka[b][:], KfA[h, :, :], [], [("Ka", b)], ("Ka", b))
            B.dma("sp", Qa[b][:], QfA[h, :, :], [], [("Qa", b)], ("Qa", b))
            B.dma("sp", V[b][:, :, 0:64], Vtok[:, h * 64:(h + 1) * 64].rearrange("(kt p) d -> p kt d", p=128),
                  [], [("V", b)], ("V", b))

        load_head(0)
        it = 0
        for h in range(6):
            if h + 1 < 6:
                load_head(h + 1)
            b = h % 2
            for qb in range(8):
                po = 3 + (qb % 2)
                nk = 4 * (qb + 1)
                pbs = {}

                def fox_qk(kt):
                    nonlocal it
                    pb = it % 3
                    it += 1
                    pbs[kt] = pb
                    diag = kt >= 4 * qb
                    B.pe(lambda e, kt=kt, qb=qb, pb=pb, b=b, diag=diag: e.matmul(
                        ps[pb][:], Ka[b][:, kt * 128:(kt + 1) * 128], Qa[b][:, qb * 512:(qb + 1) * 512],
                        start=True, stop=not diag), [("Ka", b), ("Qa", b)], [("ps", pb)])
                    if diag:
                        B.pe(lambda e, kt=kt, qb=qb, pb=pb: e.matmul(ps[pb][:], ident[:], mb[:, kt - 4 * qb, :],
                                                                     start=False, stop=True), ["mb"], [("ps", pb)])

                def fox_rest(kt):
                    pb = pbs[kt]
                    B.act(lambda e, pb=pb: e.activation(out=pT[pb][:], in_=ps[pb][:], func=AF.Exp),
                          [("ps", pb)], [("pT", pb)])
                    B.pe(lambda e, kt=kt, pb=pb, po=po, b=b, nk=nk: e.matmul(
                        ps[po][0:65, :], V[b][:, kt, :], pT[pb][:], start=(kt == 0), stop=(kt == nk - 1)),
                        [("V", b), ("Vone", b), ("pT", pb)], [("ps", po)])

                fox_qk(0)
                for kt in range(nk):
                    if kt + 1 < nk:
                        fox_qk(kt + 1)
                    fox_rest(kt)
                B.act(lambda e, po=po: e.activation(out=osb[:], in_=ps[po][0:65, :], func=AF.Copy), [("ps", po)], ["osb"])
                B.pe(lambda e: e.matmul(ps[5][0:64, :], ed[:], osb[:], start=True, stop=True), ["ed", "osb"], [("ps", 5)])
                B.dve(lambda e: e.reciprocal(out=rec[:], in_=ps[5][0:64, :]), [("ps", 5)], ["rec"])
                so = qb % 2
                B.dve(lambda e, so=so: e.tensor_tensor(out=ost[so][:], in0=osb[0:64, :], in1=rec[:], op=ALU.mult),
                      ["osb", "rec"], [("ost", so)])
                B.dma("sp", OT[h * 64:(h + 1) * 64, qb * 512:(qb + 1) * 512], ost[so][:], [("ost", so)], [], ("ost", so))
        B.end_phase()

    def sb_phase():
        Kt = [B.sbuf(f"Kt{i}", [64, S], BF16) for i in range(2)]
        Qt = [B.sbuf(f"Qt{i}", [64, S], BF16) for i in range(2)]
        V = [B.sbuf(f"V{i}", [128, NKT, 64], BF16) for i in range(2)]
        mb = B.sbuf("mb", [128, 4, 512], BF16)
        m01 = B.sbuf("m01", [128, 4, 512], BF16)
        esel = B.sbuf("esel", [128, 32, 64], BF16)
        nsel = B.sbuf("nsel", [64, 32, 128], BF16)
        spT = B.sbuf("spT", [128, NKT, 512], BF16)
        e32 = [B.sbuf(f"e32{i}", [128, 512], F32) for i in range(2)]
        csh = B.sbuf("csh", [64, 512], BF16)
        hi2 = B.sbuf("hi2", [64, 512], BF16)
        aT = [B.sbuf(f"aT{i}", [128, 512], BF16) for i in range(3)]
        ost = [B.sbuf(f"ost{i}", [64, 512], BF16) for i in range(2)]
        B.dma("pool", mb[:], cd["c_mbig_sb"][:, :, :], [], ["mb"], "c0")
        B.dma("pool", m01[:], cd["c_m01_sb"][:, :, :], [], ["m01"], "c0")
        B.dma("pool", esel[:], cd["c_esel2"][:, :, :], [], ["esel"], "c0")
        B.dma("pool", nsel[:], cd["c_negsel"][:, :, :], [], ["nsel"], "c0")
        B.group(["mb", "m01", "esel", "nsel"], "c0")

        def load_head(h):
            b = h % 2
            B.dma("sp", Kt[b][:], KsT[h, :, :], [], [("Kt", b)], ("Kt", b))
            B.dma("sp", Qt[b][:], QsT[h, :, :], [], [("Qt", b)], ("Qt", b))
            B.dma("sp", V[b][:], Vtok[:, 384 + h * 64:384 + (h + 1) * 64].rearrange("(kt p) d -> p kt d", p=128),
                  [], [("V", b)], ("V", b))

        load_head(0)
        it = 0
        it2 = 0
        for h in range(6):
            if h + 1 < 6:
                load_head(h + 1)
            b = h % 2
            for qb in range(8):
                nk = 4 * (qb + 1)
                qs = slice(qb * 512, (qb + 1) * 512)
                pb1 = {}

                def sb_qk1(kt):
                    nonlocal it
                    pb = it % 2
                    it += 1
                    pb1[kt] = pb
                    B.pe(lambda e, kt=kt, qs=qs, pb=pb, b=b: e.matmul(ps[pb][:], Kt[b][:, kt * 128:(kt + 1) * 128], Qt[b][:, qs],
                                                                      start=True, stop=True), [("Kt", b), ("Qt", b)], [("ps", pb)])

                def sb_rest1(kt):
                    pb = pb1[kt]
                    B.act(lambda e, pb=pb: e.activation(out=e32[pb][:], in_=ps[pb][:], func=AF.Exp), [("ps", pb)], [("e32", pb)])
                    B.act(lambda e, pb=pb, kt=kt: e.activation(out=spT[:, kt, :], in_=e32[pb][:], func=AF.Ln, bias=oneb[:, 0:1], scale=1.0),
                          [("e32", pb)], [("sp", kt)])
                    if kt >= 4 * qb:
                        B.dve(lambda e, kt=kt, qb=qb: e.tensor_tensor(out=spT[:, kt, :], in0=spT[:, kt, :], in1=m01[:, kt - 4 * qb, :], op=ALU.mult),
                              [("sp", kt), "m01"], [("sp", kt)])
                    B.pe(lambda e, kt=kt, nk=nk: e.matmul(ps[2][0:64, :], esel[:, kt, :], spT[:, kt, :], start=(kt == 0), stop=(kt == nk - 1)),
                         ["esel", ("sp", kt)], [("ps", 2)])

                sb_qk1(0)
                for kt in range(nk):
                    if kt + 1 < nk:
                        sb_qk1(kt + 1)
                    sb_rest1(kt)
                B.dve(lambda e: e.tensor_copy(out=csh[0:32, :], in_=ps[2][0:32, :]), [("ps", 2)], ["csh0"])
                B.dve(lambda e: e.tensor_copy(out=hi2[32:64, :], in_=ps[2][32:64, :]), [("ps", 2)], ["hi2"])
                B.dve(lambda e: e.tensor_tensor(out=csh[32:64, :], in0=ps[2][32:64, :], in1=hi2[32:64, :], op=ALU.subtract),
                      [("ps", 2), "hi2"], ["csh1"])
                po = 6 + (qb % 2)
                pl2 = {}

                def sb_lg(kt):
                    nonlocal it2
                    pl = 3 + (it2 % 3)
                    sl = it2 % 3
                    it2 += 1
                    pl2[kt] = (pl, sl)
                    diag = kt >= 4 * qb
                    B.pe(lambda e, kt=kt, qs=qs, pl=pl, b=b: e.matmul(ps[pl][:], Kt[b][:, kt * 128:(kt + 1) * 128], Qt[b][:, qs],
                                                                      start=True, stop=False), [("Kt", b), ("Qt", b)], [("ps", pl)])
                    B.pe(lambda e, kt=kt, pl=pl: e.matmul(ps[pl][:], negtri[:], spT[:, kt, :], start=False, stop=False),
                         [("sp", kt)], [("ps", pl)])
                    B.pe(lambda e, kt=kt, pl=pl, diag=diag: e.matmul(ps[pl][:], nsel[:, kt, :], csh[:], start=False, stop=not diag),
                         ["nsel", "csh0", "csh1"], [("ps", pl)])
                    if diag:
                        B.pe(lambda e, kt=kt, qb=qb, pl=pl: e.matmul(ps[pl][:], ident[:], mb[:, kt - 4 * qb, :], start=False, stop=True),
                             ["mb"], [("ps", pl)])

                def sb_rest2(kt):
                    pl, sl = pl2[kt]
                    B.act(lambda e, pl=pl, sl=sl: e.activation(out=aT[sl][:], in_=ps[pl][:], func=AF.Exp), [("ps", pl)], [("aT", sl)])
                    B.pe(lambda e, kt=kt, sl=sl, po=po, b=b, nk=nk: e.matmul(ps[po][0:64, :], V[b][:, kt, :], aT[sl][:],
                                                                             start=(kt == 0), stop=(kt == nk - 1)),
                         [("V", b), ("aT", sl)], [("ps", po)])

                sb_lg(0)
                for kt in range(nk):
                    if kt + 1 < nk:
                        sb_lg(kt + 1)
                    sb_rest2(kt)
                so = qb % 2
                B.act(lambda e, po=po, so=so: e.activation(out=ost[so][:], in_=ps[po][0:64, :], func=AF.Copy), [("ps", po)], [("ost", so)])
                B.dma("sp", OT[384 + h * 64:384 + (h + 1) * 64, qs], ost[so][:], [("ost", so)], [], ("ost", so))
        B.end_phase()

    def dsa_phase():
        Kc = B.sbuf("Kc", [64, 4, S], BF16)
        Ki = B.sbuf("Ki", [64, S], BF16)
        V = B.sbuf("V", [128, NKT, 4, 65], BF16)
        Qc = [B.sbuf(f"Qc{i}", [64, 4, 512], BF16) for i in range(2)]
        Qi = [B.sbuf(f"Qi{i}", [64, 4, 512], BF16) for i in range(2)]
        wi = [B.sbuf(f"wi{i}", [128, 4, 4], F32) for i in range(2)]
        score = B.sbuf("score", [128, 4, S], F32)
        Mc = B.sbuf("Mc", [128, 4, S], BF16)
        junk = B.sbuf("junk", [128, S], BF16)
        rl = [B.sbuf(f"rl{i}", [128, 512], F32) for i in range(2)]
        cz = B.sbuf("cz", [128, 128], F32)
        ed = B.sbuf("ed", [65, 64], F32)
        mx = B.sbuf("mx", [128, 4], F32)
        mn = B.sbuf("mn", [128, 4], F32)
        w0 = B.sbuf("w0", [128, 4], F32)
        lo = B.sbuf("lo", [128, 4], F32)
        mid = B.sbuf("mid", [128, 4], F32)
        cnt = B.sbuf("cnt", [128, 4], F32)
        prd = B.sbuf("prd", [128, 4], F32)
        pT = [B.sbuf(f"pT{i}", [128, 512], BF16) for i in range(3)]
        osb = B.sbuf("osb", [65, 512], F32)
        rec = B.sbuf("rec", [64, 512], F32)
        ost = [B.sbuf(f"ost{i}", [64, 512], BF16) for i in range(2)]
        B.dma("sp", cz[:], cd["c_causal"][:, :], [], ["cz"], "c2")
        B.dma("sp", ed[:], cd["c_edenom"][:, :], [], ["ed"], "c2")
        B.group(["cz", "ed"], "c2")
        for h in range(4):
            B.dma("sp", Kc[:, h, :], KcT[h, :, :], [], [("Kc", h)], "Kc")
        B.group([("Kc", h) for h in range(4)], "Kc")
        B.dma("sp", Ki[:], KiT[:, :], [], ["Ki"], "Ki")
        B.dve(lambda e: e.memset(V[:, :, :, 64:65], 1.0), [], ["Vone"])
        for h in range(4):
            B.dma("sp", V[:, :, h, 0:64], Vtok[:, 768 + h * 64:768 + (h + 1) * 64].rearrange("(kt p) d -> p kt d", p=128),
                  [], [("V", h)], "V")
        B.group([("V", h) for h in range(4)], "V")

        def load_q(qb):
            b = qb % 2
            qs = slice(qb * 512, (qb + 1) * 512)
            for h in range(4):
                B.dma("sp", Qc[b][:, h, :], QcT[h, :, qs], [], [("Qc", b, h)], ("Qc", b))
                B.dma("sp", Qi[b][:, h, :], QiT[h, :, qs], [], [("Qi", b, h)], ("Qi", b))
            B.group([("Qc", b, h) for h in range(4)], ("Qc", b))
            B.group([("Qi", b, h) for h in range(4)], ("Qi", b))
            B.dma("sp", wi[b][:], WiD[qs, :].rearrange("(s p) n -> p s n", p=128), [], [("wi", b)], ("wi", b), slow=True)

        load_q(0)
        it = 0
        ir = 0
        for qb in range(8):
            if qb + 1 < 8:
                load_q(qb + 1)
            b = qb % 2
            for g in range(4):
                qt = qb * 4 + g
                kmax = (qt + 1) * 128
                nch = (kmax + 511) // 512
                for kc in range(nch):
                    wdt = min(512, kmax - kc * 512)
                    ks = slice(kc * 512, kc * 512 + wdt)
                    for h in range(4):
                        pb = it % 2
                        it += 1
                        B.pe(lambda e, g=g, h=h, ks=ks, wdt=wdt, pb=pb, b=b: e.matmul(
                            ps[pb][:, 0:wdt], Qi[b][:, h, g * 128:(g + 1) * 128], Ki[:, ks], start=True, stop=True),
                            [("Qi", b, h), "Ki"], [("ps", pb)])
                        r = ir % 2
                        ir += 1
                        B.act(lambda e, pb=pb, wdt=wdt, r=r: e.activation(out=rl[r][:, 0:wdt], in_=ps[pb][:, 0:wdt], func=AF.Relu),
                              [("ps", pb)], [("rl", r)])
                        if h == 0:
                            B.dve(lambda e, g=g, ks=ks, wdt=wdt, r=r, b=b: e.tensor_scalar(
                                out=score[:, g, ks], in0=rl[r][:, 0:wdt], scalar1=wi[b][:, g, 0:1], scalar2=None, op0=ALU.mult),
                                [("rl", r), ("wi", b)], [("score", g)])
                        else:
                            B.dve(lambda e, g=g, h=h, ks=ks, wdt=wdt, r=r, b=b: e.scalar_tensor_tensor(
                                out=score[:, g, ks], in0=rl[r][:, 0:wdt], scalar=wi[b][:, g, h:h + 1], in1=score[:, g, ks],
                                op0=ALU.mult, op1=ALU.add), [("rl", r), ("wi", b), ("score", g)], [("score", g)])
                B.dve(lambda e, g=g, kmax=kmax: e.tensor_reduce(out=mx[:, g:g + 1], in_=score[:, g, 0:kmax], axis=AX.X, op=ALU.max),
                      [("score", g)], ["mx"])
                B.dve(lambda e, g=g, kmax=kmax: e.tensor_reduce(out=mn[:, g:g + 1], in_=score[:, g, 0:kmax], axis=AX.X, op=ALU.min),
                      [("score", g)], ["mn"])
                B.dve(lambda e, g=g, kmax=kmax: e.tensor_tensor(out=score[:, g, kmax - 128:kmax], in0=score[:, g, kmax - 128:kmax],
                                                                in1=cz[:], op=ALU.add), [("score", g), "cz"], [("score", g)])
            B.dve(lambda e: e.tensor_tensor(out=w0[:], in0=mx[:], in1=mn[:], op=ALU.subtract), ["mx", "mn"], ["w0"])
            B.dve(lambda e: e.tensor_scalar(out=w0[:], in0=w0[:], scalar1=1.0001, scalar2=1e-6, op0=ALU.mult, op1=ALU.add), ["w0"], ["w0"])
            B.dve(lambda e: e.tensor_copy(out=lo[:], in_=mn[:]), ["mn"], ["lo"])
            for s_ in range(NBIS):
                hf = 0.5 ** (s_ + 1)
                B.dve(lambda e, hf=hf: e.scalar_tensor_tensor(out=mid[:], in0=w0[:], scalar=hf, in1=lo[:], op0=ALU.mult, op1=ALU.add),
                      ["w0", "lo"], ["mid"])
                B.dve(lambda e: e.memset(cnt[:], 0.0), [], ["cnt"])
                for g in range(4):
                    kmax = (qb * 4 + g + 1) * 128
                    B.dve(lambda e, g=g, kmax=kmax: e.tensor_scalar(out=junk[:, 0:kmax], in0=score[:, g, 0:kmax], scalar1=mid[:, g:g + 1],
                                                                    scalar2=0.0, op0=ALU.is_ge, op1=ALU.add, accum_out=cnt[:, g:g + 1]),
                          [("score", g), "mid", "cnt"], ["junk", "cnt"])
                B.dve(lambda e: e.tensor_scalar(out=prd[:], in0=cnt[:], scalar1=float(TOPK), scalar2=None, op0=ALU.is_ge), ["cnt"], ["prd"])
                B.dve(lambda e: e.tensor_tensor(out=prd[:], in0=prd[:], in1=w0[:], op=ALU.mult), ["prd", "w0"], ["prd"])
                B.dve(lambda e, hf=hf: e.scalar_tensor_tensor(out=lo[:], in0=prd[:], scalar=hf, in1=lo[:], op0=ALU.mult, op1=ALU.add),
                      ["prd", "lo"], ["lo"])
            for g in range(4):
                kmax = (qb * 4 + g + 1) * 128
                B.dve(lambda e, g=g, kmax=kmax: e.tensor_scalar(out=Mc[:, g, 0:kmax], in0=score[:, g, 0:kmax], scalar1=lo[:, g:g + 1],
                                                                scalar2=None, op0=ALU.is_lt), [("score", g), "lo"], [("Mc", g)])
            nk = 4 * (qb + 1)
            for h in range(4):
                po = 6 + (h % 2)
                pld = {}

                def dsa_lg(kt):
                    nonlocal it
                    pl = 3 + (it % 3)
                    sl = it % 3
                    it += 1
                    pld[kt] = (pl, sl)
                    gmin = max(0, kt - 4 * qb)
                    B.pe(lambda e, kt=kt, h=h, pl=pl, b=b: e.matmul(ps[pl][:], Kc[:, h, kt * 128:(kt + 1) * 128], Qc[b][:, h, :],
                                                                    start=True, stop=False), [("Kc", h), ("Qc", b, h)], [("ps", pl)])
                    if gmin > 0:
                        B.pe(lambda e, pl=pl, gmin=gmin: e.matmul(ps[pl][:, 0:gmin * 128], ident[:], mbz[:, 0:gmin * 128],
                                                                  start=False, stop=False), [], [("ps", pl)])
                    for g in range(gmin, 4):
                        B.pe(lambda e, g=g, kt=kt, pl=pl: e.matmul(ps[pl][:, g * 128:(g + 1) * 128], Mc[:, g, kt * 128:(kt + 1) * 128],
                                                                   nbident[:], start=False, stop=(g == 3)),
                             [("Mc", g)], [("ps", pl)])

                def dsa_rest(kt):
                    pl, sl = pld[kt]
                    B.act(lambda e, pl=pl, sl=sl: e.activation(out=pT[sl][:], in_=ps[pl][:], func=AF.Exp), [("ps", pl)], [("pT", sl)])
                    B.pe(lambda e, kt=kt, h=h, sl=sl, po=po, nk=nk: e.matmul(ps[po][0:65, :], V[:, kt, h, :], pT[sl][:],
                                                                             start=(kt == 0), stop=(kt == nk - 1)),
                         [("V", h), "Vone", ("pT", sl)], [("ps", po)])

                dsa_lg(0)
                for kt in range(nk):
                    if kt + 1 < nk:
                        dsa_lg(kt + 1)
                    dsa_rest(kt)
                B.act(lambda e, po=po: e.activation(out=osb[:], in_=ps[po][0:65, :], func=AF.Copy), [("ps", po)], ["osb"])
                B.pe(lambda e: e.matmul(ps[2][0:64, :], ed[:], osb[:], start=True, stop=True), ["ed", "osb"], [("ps", 2)])
                B.dve(lambda e: e.reciprocal(out=rec[:], in_=ps[2][0:64, :]), [("ps", 2)], ["rec"])
                so = h % 2
                B.dve(lambda e, so=so: e.tensor_tensor(out=ost[so][:], in0=osb[0:64, :], in1=rec[:], op=ALU.mult),
                      ["osb", "rec"], [("ost", so)])
                B.dma("sp", OT[768 + h * 64:768 + (h + 1) * 64, qb * 512:(qb + 1) * 512], ost[so][:], [("ost", so)], [], ("ost", so))
        B.end_phase()

    def out_phase(l, xsrc):
        gi = l * 3 + 1
        Wgt = B.sbuf("Wgt", [128, NDC, 3 * D], BF16)
        Wbr = B.sbuf("Wbr", [128, NDC, D], BF16)
        Wo = B.sbuf("Wo", [128, NDC, D], BF16)
        xt = [B.sbuf(f"xt{i}", [128, NDC, TT], F32) for i in range(2)]
        ot = [B.sbuf(f"ot{i}", [128, NDC, TT], BF16) for i in range(2)]
        hT = B.sbuf("hT", [128, NDC, TT], BF16)
        sq = B.sbuf("sq", [128, NDC, TT], BF16)
        rstd = B.sbuf("rstd", [128, TT], F32)
        sg = [B.sbuf(f"sg{i}", [128, TT], F32) for i in range(2)]
        macc = B.sbuf("macc", [128, TT], F32)
        mt = B.sbuf("mt", [128, TT], F32)
        mrg = B.sbuf("mrg", [128, NDC, TT], BF16)
        for c in range(NDC):
            B.dma("pool", Wgt[:, c, :], w_in[l, c * 128:(c + 1) * 128, C_G:NIN], [], [("Wgt", c)], "w0")
            B.dma("pool", Wo[:, c, :], w_out[l, c * 128:(c + 1) * 128, :], [], [("Wo", c)], "w1")
        brch = [(0, 0), (0, 1), (0, 2), (1, 0), (1, 1), (1, 2), (2, 0), (2, 1)]
        for c, (br, j) in enumerate(brch):
            B.dma("pool", Wbr[:, c, :], w_br[br][l, j * 128:(j + 1) * 128, :], [], [("Wbr", c)], "w2")
        B.group([("Wgt", c) for c in range(NDC)], "w0")
        B.group([("Wo", c) for c in range(NDC)], "w1")
        B.group([("Wbr", c) for c in range(NDC)], "w2")

        def load(i):
            t0 = i * TT
            B.dma("sp", xt[i % 2][:], xsrc[:, t0:t0 + TT].rearrange("(c p) t -> p c t", p=128), [], [("xt", i % 2)], ("xt", i % 2))
            B.dma("sp", ot[i % 2][:], OT[:, t0:t0 + TT].rearrange("(c p) t -> p c t", p=128), [], [("ot", i % 2)], ("ot", i % 2))

        load(0)
        k = 0
        for i in range(NTT):
            if i + 1 < NTT:
                load(i + 1)
            X = xt[i % 2]
            xk = ("xt", i % 2)
            O = ot[i % 2]
            ok = ("ot", i % 2)
            rmsnorm_tile(X, xk, gi, hT, sq, rstd)
            brc = ((0, 1, 2), (3, 4, 5), (6, 7))
            for m in range(NDC):
                for br in range(3):
                    pg = 1 + (k % 2)
                    pbb = 3 + (k % 2)
                    sgi = k % 2
                    k += 1
                    col = br * D + m * 128
                    for c in range(NDC):
                        B.pe(lambda e, c=c, col=col, pg=pg: e.matmul(ps[pg][:], Wgt[:, c, col:col + 128], hT[:, c, :],
                                                                      start=(c == 0), stop=(c == NDC - 1)),
                             [("Wgt", c), ("hT", c)], [("ps", pg)])
                    cs_ = brc[br]
                    for ci, c in enumerate(cs_):
                        B.pe(lambda e, c=c, ci=ci, m=m, pbb=pbb, n_=len(cs_), O=O: e.matmul(ps[pbb][:], Wbr[:, c, m * 128:(m + 1) * 128], O[:, c, :],
                                                                                            start=(ci == 0), stop=(ci == n_ - 1)),
                             [("Wbr", c), ok], [("ps", pbb)])
                    B.act(lambda e, pg=pg, sgi=sgi, br=br, m=m: e.activation(out=sg[sgi][:], in_=ps[pg][:], func=AF.Sigmoid,
                                                                            bias=bgate[:, l, br, m:m + 1], scale=1.0),
                          [("ps", pg)], [("sg", sgi)])
                    if br == 0:
                        B.dve(lambda e, pbb=pbb, sgi=sgi: e.tensor_tensor(out=macc[:], in0=ps[pbb][:], in1=sg[sgi][:], op=ALU.mult),
                              [("ps", pbb), ("sg", sgi)], ["macc"])
                    else:
                        B.dve(lambda e, pbb=pbb, sgi=sgi: e.tensor_tensor(out=mt[:], in0=ps[pbb][:], in1=sg[sgi][:], op=ALU.mult),
                              [("ps", pbb), ("sg", sgi)], ["mt"])
                        if br == 1:
                            B.dve(lambda e: e.tensor_tensor(out=macc[:], in0=macc[:], in1=mt[:], op=ALU.add), ["macc", "mt"], ["macc"])
                        else:
                            B.dve(lambda e, m=m: e.tensor_tensor(out=mrg[:, m, :], in0=macc[:], in1=mt[:], op=ALU.add),
                                  ["macc", "mt"], [("mrg", m)])
            for e_ in range(NDC):
                py = 5 + (e_ % 2)
                for m in range(NDC):
                    B.pe(lambda e, e_=e_, m=m, py=py: e.matmul(ps[py][:], Wo[:, m, e_ * 128:(e_ + 1) * 128], mrg[:, m, :],
                                                               start=(m == 0), stop=(m == NDC - 1)),
                         [("Wo", m), ("mrg", m)], [("ps", py)])
                B.dve(lambda e, e_=e_, py=py, X=X: e.tensor_tensor(out=X[:, e_, :], in0=ps[py][:], in1=X[:, e_, :], op=ALU.add),
                      [("ps", py), xk], [xk])
            t0 = i * TT
            B.dma("sp", outT[:, t0:t0 + TT].rearrange("(c p) t -> p c t", p=128), X[:], [xk], [], ("xt", i % 2))
        B.end_phase()

    phase0()
    for l in range(DEPTH):
        if on(f"ffn1_{l}"):
            ffn_phase(l, 0, xT_in if l == 0 else outT, outT, l == 0)
        if on(f"proj_{l}"):
            proj_phase(l, outT)
        if on(f"fox_{l}"):
            fox_phase()
        if on(f"sb_{l}"):
            sb_phase()
        if on(f"dsa_{l}"):
            dsa_phase()
        if on(f"out_{l}"):
            out_phase(l, outT)
        if on(f"ffn2_{l}"):
            ffn_phase(l, 1, outT, outT, False)

    B.sc.close()
    for g in reversed(psg):
        g.__exit__(None, None, None)
    for g in reversed(B._glob):
        g.__exit__(None, None, None)
    return nc


_WNAMES = ("ffn1_norm", "ffn1_w_gate", "ffn1_w_up", "ffn1_w_down", "mix_norm", "w_in", "b_forget", "b_gates",
           "q_norm_fox", "k_norm_fox", "q_norm_sb", "k_norm_sb", "q_norm_dsa", "k_norm_dsa",
           "w_branch_fox", "w_branch_sb", "w_branch_dsa", "w_out",
           "ffn2_norm", "ffn2_w_gate", "ffn2_w_up", "ffn2_w_down")


def kernel(**inputs):
    n = 8
    phases = inputs.pop("_phases", ("all",))
    ncores = inputs.pop("_ncores", n)
    nc = build_program(phases)
    x = np.asarray(inputs["x"])
    pos = np.asarray(inputs["positions"]).astype(np.int32)
    shared = {k: np.ascontiguousarray(np.asarray(inputs[k], dtype=np.float32)) for k in _WNAMES}
    shared.update(_CONST)
    in_maps = []
    for b in range(ncores):
        m = dict(shared)
        m["xT"] = np.ascontiguousarray(x[b].T)
        m["pos"] = np.ascontiguousarray(pos[b:b + 1])
        in_maps.append(m)
    res = run_bass_kernel_spmd(nc, in_maps, core_ids=list(range(ncores)))
    if any(p.startswith("dbgOT") for p in phases):
        return res.results[0]["OT"]
    out = np.stack([np.ascontiguousarray(r["outT"].T) for r in res.results], axis=0)
    return out.astype(np.float32, copy=False)
```

```python
import numpy as np
import concourse.bass as bass
import concourse.mybir as mybir
from concourse.bass_utils import run_bass_kernel_spmd

F32 = mybir.dt.float32
BF16 = mybir.dt.bfloat16
I32 = mybir.dt.int32
AF = mybir.ActivationFunctionType
ALU = mybir.AluOpType
AX = mybir.AxisListType

D = 1024
S = 4096
DEPTH = 2
DFF = 2816
NFC = DFF // 128
NDC = D // 128
TT = 512
NTT = S // TT
NKT = S // 128
EPS = 1e-6
BIG = 30000.0
TOPK = 256
NBIS = 24

C_QF, C_KF, C_VF, C_FF = 0, 384, 768, 1152
C_QS, C_KS, C_VS = 1158, 1542, 1926
C_QC, C_KC, C_VC = 2310, 2566, 2822
C_QI, C_KI, C_WI = 3078, 3334, 3398
C_G = 3402
NIN = 6474


class Sched:
    ENG = ("pe", "act", "dve", "pool", "sp")

    def __init__(self, nc):
        self.nc = nc
        self._new_phase()
        self._reset()

    def _new_phase(self):
        self.known = {e: {} for e in self.ENG}
        self.chan_n = {}
        self.n = {e: 0 for e in self.ENG}
        self.sig_base = {e: 0 for e in self.ENG}
        self.sems = {}
        self.chan_sems = {}

    def _reset(self):
        self.ops = {e: [] for e in self.ENG}
        self.lastw = {}
        self.readers = {}
        self.signal = set()

    def add(self, eng, emit, reads=(), writes=(), dma=False, chan=None):
        deps = {}

        def need(d):
            for sk, o in d.items():
                if deps.get(sk, 0) < o:
                    deps[sk] = o

        for r in reads:
            need(self.lastw.get(r, {}))
        for w in writes:
            need(self.lastw.get(w, {}))
            need(self.readers.get(w, {}))
        if dma:
            self.chan_n[chan] = self.chan_n.get(chan, 0) + 1
            me = (("ch", chan), self.chan_n[chan])
        else:
            self.n[eng] += 1
            me = (eng, self.n[eng])
        waits = []
        kn = self.known[eng]
        for sk, o in deps.items():
            if sk == "pe" and eng == "pe" and not dma:
                continue
            if kn.get(sk, 0) >= o:
                continue
            kn[sk] = o
            waits.append((sk, o))
            if not isinstance(sk, tuple):
                self.signal.add((sk, o))
        for w in writes:
            self.lastw[w] = {me[0]: me[1]}
            self.readers[w] = {}
        for r in reads:
            rd = self.readers.setdefault(r, {})
            if rd.get(me[0], 0) < me[1]:
                rd[me[0]] = me[1]
        self.ops[eng].append(dict(emit=emit, waits=waits, me=me, dma=dma))
        return me

    def end_phase(self):
        nc = self.nc
        waits = []
        for chan, n in self.chan_n.items():
            sk = ("ch", chan)
            if self.known["sp"].get(sk, 0) < n:
                waits.append((sk, n))
                self.known["sp"][sk] = n
        self.ops["sp"].append(dict(emit=None, waits=waits, me=None, dma=False))
        self._pid = getattr(self, "_pid", 0) + 1
        snap = nc.snapshot_sems()
        for e in self.ENG:
            self.sems[e] = nc.alloc_semaphore(f"s{self._pid}_{e}")
        for i, ch in enumerate(self.chan_n):
            self.chan_sems[ch] = nc.alloc_semaphore(f"s{self._pid}_ch{i}")
        sigrank = {}
        cnt = {}
        for e in self.ENG:
            ords = sorted(o for (sk, o) in self.signal if sk == e)
            for i, o in enumerate(ords):
                sigrank[(e, o)] = self.sig_base[e] + i + 1
            cnt[e] = len(ords)

        def semval(sk, o):
            if isinstance(sk, tuple):
                return self.chan_sems[sk[1]], 16 * o
            return self.sems[sk], sigrank[(sk, o)]

        ops = self.ops
        signal = self.signal
        import os
        if os.environ.get("KDBG"):
            print("PHASE", self._pid, {e: (len(ops[e]), cnt[e]) for e in self.ENG}, "chan max", max([16 * n for n in self.chan_n.values()] + [0]),
                  "nchan", len(self.chan_n), flush=True)

        def run(eng_name):
            def body(eng):
                for op in ops[eng_name]:
                    for sk, o in op["waits"]:
                        s, v = semval(sk, o)
                        eng.wait_ge(s, v)
                    if op["emit"] is None:
                        continue
                    ins = op["emit"](eng)
                    me = op["me"]
                    if op["dma"]:
                        ins.then_inc(self.chan_sems[me[0][1]], 16)
                    elif me in signal:
                        ins.then_inc(self.sems[me[0]], 1)
            return body

        with nc.Block() as block:
            block.tensor(run("pe"))
            block.scalar(run("act"))
            block.vector(run("dve"))
            block.gpsimd(run("pool"))
            block.sync(run("sp"))
        nc.clear_and_free_semaphores(nc.allocated_since(snap))
        nc.all_engine_barrier()
        self._new_phase()
        self._reset()

    def close(self):
        pass


class Builder:
    def __init__(self):
        self.nc = bass.Bass("TRN2", target_bir_lowering=False)
        self.sc = Sched(self.nc)
        self._glob = []
        self._scope = []

    def din(self, name, shape, dt=F32):
        return self.nc.dram_tensor(name, list(shape), dt, kind="ExternalInput").ap()

    def dout(self, name, shape, dt=F32):
        return self.nc.dram_tensor(name, list(shape), dt, kind="ExternalOutput").ap()

    def dscr(self, name, shape, dt=F32):
        return self.nc.dram_tensor(name, list(shape), dt, kind="Internal").ap()

    def sbuf(self, name, shape, dt, glob=False):
        self._uid = getattr(self, "_uid", 0) + 1
        g = self.nc.sbuf_tensor(f"{name}_{self._uid}", list(shape), dt)
        t = g.__enter__()
        (self._glob if glob else self._scope).append(g)
        return t

    def end_phase(self):
        self.sc.end_phase()
        for g in reversed(self._scope):
            g.__exit__(None, None, None)
        self._scope = []

    def pe(self, emit, reads, writes):
        return self.sc.add("pe", emit, reads, writes)

    def act(self, emit, reads, writes):
        return self.sc.add("act", emit, reads, writes)

    def dve(self, emit, reads, writes):
        return self.sc.add("dve", emit, reads, writes)

    def pool(self, emit, reads, writes):
        return self.sc.add("pool", emit, reads, writes)

    def group(self, keys, chan):
        n = self.sc.chan_n[chan]
        for k in keys:
            self.sc.lastw[k] = {("ch", chan): n}

    def dma(self, q, out, in_, reads, writes, chan, slow=False):
        return self.sc.add(q, lambda e: e.dma_start(out=out, in_=in_, allow_slow_non_contiguous=slow),
                           reads, writes, dma=True, chan=chan)


def _consts():
    c = {}
    c["c_ones"] = np.ones((128, 128), np.float32)
    bo = np.zeros((128, 128), np.float32)
    bo[:64, :64] = 1
    bo[64:, 64:] = 1
    c["c_blockones"] = bo
    rm = np.zeros((128, 128), np.float32)
    for hh in (0, 64):
        for j in range(8):
            rm[hh + 8 + j, hh + j] = -1.0
            rm[hh + j, hh + 8 + j] = 1.0
    c["c_rmat"] = rm
    c["c_ident"] = np.eye(128, dtype=np.float32)
    j = np.arange(128)[:, None]
    s = np.arange(128)[None, :]
    c["c_negtri"] = -(j >= s).astype(np.float32)
    t = np.arange(512)[None, None, :]
    i4 = np.arange(4)[None, :, None]
    jj = np.arange(128)[:, None, None]
    kg = 128 * i4 + jj
    c["c_mbig_fox"] = (-BIG * (kg > t)).astype(np.float32)
    c["c_mbig_sb"] = (-BIG * (kg >= t)).astype(np.float32)
    c["c_m01_sb"] = (kg < t).astype(np.float32)
    es = np.zeros((128, 32, 64), np.float32)
    for kt in range(32):
        es[:, kt, kt] = 1
        es[:, kt, 32 + kt] = 1
    c["c_esel2"] = es
    ns = np.zeros((64, 32, 128), np.float32)
    for kt in range(32):
        for r in range(64):
            if (r % 32) > kt:
                ns[r, kt, :] = -1
    c["c_negsel"] = ns
    c["c_nbident"] = (-BIG * np.eye(128)).astype(np.float32)
    q = np.arange(128)[:, None]
    k = np.arange(128)[None, :]
    c["c_causal"] = (-1e30 * (k > q)).astype(np.float32)
    invf = np.zeros((1, 128), np.float32)
    half = 8
    f = (500000.0 ** (-np.arange(half, dtype=np.float32) * 2.0 / 16.0)).astype(np.float32)
    for hh in (0, 64):
        invf[0, hh:hh + 8] = f
        invf[0, hh + 8:hh + 16] = f
    c["c_invf"] = invf
    ed = np.zeros((65, 64), np.float32)
    ed[64, :] = 1
    c["c_edenom"] = ed
    return c


_CONST = _consts()


def build_program(phases=("all",)):
    B = Builder()
    nc = B.nc
    ALLP = "all" in phases

    def on(p):
        return ALLP or p in phases

    xT_in = B.din("xT", [D, S])
    pos_in = B.din("pos", [1, S], I32)
    ffn_w = {}
    for nm in ("ffn1", "ffn2"):
        ffn_w[nm] = dict(
            norm=B.din(nm + "_norm", [DEPTH, D]),
            wg=B.din(nm + "_w_gate", [DEPTH, D, DFF]),
            wu=B.din(nm + "_w_up", [DEPTH, D, DFF]),
            wd=B.din(nm + "_w_down", [DEPTH, DFF, D]),
        )
    mix_norm = B.din("mix_norm", [DEPTH, D])
    w_in = B.din("w_in", [DEPTH, D, NIN])
    b_forget = B.din("b_forget", [DEPTH, 6])
    b_gates = B.din("b_gates", [DEPTH, 3, D])
    hn_names = ("q_norm_fox", "k_norm_fox", "q_norm_sb", "k_norm_sb", "q_norm_dsa", "k_norm_dsa")
    hn = {n: B.din(n, [DEPTH, 64]) for n in hn_names}
    w_br = [B.din("w_branch_fox", [DEPTH, 384, D]), B.din("w_branch_sb", [DEPTH, 384, D]),
            B.din("w_branch_dsa", [DEPTH, 256, D])]
    w_out = B.din("w_out", [DEPTH, D, D])
    cd = {k: B.din(k, list(v.shape)) for k, v in _CONST.items()}
    outT = B.dout("outT", [D, S])

    QfA = B.dscr("QfA", [6, 68, S], BF16)
    KfA = B.dscr("KfA", [6, 68, S], BF16)
    QsT = B.dscr("QsT", [6, 64, S], BF16)
    KsT = B.dscr("KsT", [6, 64, S], BF16)
    QcT = B.dscr("QcT", [4, 64, S], BF16)
    KcT = B.dscr("KcT", [4, 64, S], BF16)
    QiT = B.dscr("QiT", [4, 64, S], BF16)
    KiT = B.dscr("KiT", [64, S], BF16)
    Vtok = B.dscr("Vtok", [S, 1024], BF16)
    WiD = B.dscr("WiD", [S, 4], F32)
    NLF = B.dscr("NLF", [6, S], F32)
    CosD = B.dscr("CosD", [128, S], F32)
    SinD = B.dscr("SinD", [128, S], F32)
    dbg = [p for p in phases if p.startswith("dbgOT")]
    if dbg:
        OT = B.dout("OT", [D, S], BF16)
    else:
        OT = B.dscr("OT", [D, S], BF16)

    ones = B.sbuf("ones", [128, 128], BF16, glob=True)
    blockones = B.sbuf("blockones", [128, 128], BF16, glob=True)
    rmat = B.sbuf("rmat", [128, 128], BF16, glob=True)
    ident = B.sbuf("ident", [128, 128], BF16, glob=True)
    negtri = B.sbuf("negtri", [128, 128], BF16, glob=True)
    nbident = B.sbuf("nbident", [128, 128], BF16, glob=True)
    mbz = B.sbuf("mbz", [128, 512], BF16, glob=True)
    epsb = B.sbuf("epsb", [128, 1], F32, glob=True)
    oneb = B.sbuf("oneb", [128, 1], F32, glob=True)
    negpi = B.sbuf("negpi", [128, 1], F32, glob=True)
    gvec = B.sbuf("gvec", [128, 6, NDC], F32, glob=True)
    hg = B.sbuf("hg", [128, DEPTH, 6], F32, glob=True)
    negb = B.sbuf("negb", [6, DEPTH], F32, glob=True)
    bgate = B.sbuf("bgate", [128, DEPTH, 3, NDC], F32, glob=True)

    psg = [nc.psum_tensor(f"ps{i}", [128, 512], F32) for i in range(8)]
    ps = [g.__enter__() for g in psg]

    def phase0():
        B.dma("pool", ones[:], cd["c_ones"][:, :], [], ["c"], "c0")
        B.dma("pool", blockones[:], cd["c_blockones"][:, :], [], ["c"], "c0")
        B.dma("pool", rmat[:], cd["c_rmat"][:, :], [], ["c"], "c0")
        B.dma("pool", ident[:], cd["c_ident"][:, :], [], ["c"], "c0")
        B.dma("pool", negtri[:], cd["c_negtri"][:, :], [], ["c"], "c0")
        B.dma("pool", nbident[:], cd["c_nbident"][:, :], [], ["c"], "c0")
        B.dve(lambda e: e.memset(epsb[:], EPS), [], ["epsb"])
        B.dve(lambda e: e.memset(oneb[:], 1.0), [], ["oneb"])
        B.dve(lambda e: e.memset(negpi[:], -float(np.pi)), [], ["negpi"])
        B.dve(lambda e: e.memset(mbz[:], -BIG), [], ["mbz"])
        for l in range(DEPTH):
            for wi, src in enumerate((ffn_w["ffn1"]["norm"], mix_norm, ffn_w["ffn2"]["norm"])):
                B.dma("sp", gvec[:, l * 3 + wi, :], src[l].rearrange("(c p) -> p c", p=128),
                      [], [("gvec", l, wi)], "c_gvec", slow=True)
            for ni, n in enumerate(hn_names):
                for hh in (0, 64):
                    B.dma("sp", hg[hh:hh + 64, l, ni:ni + 1], hn[n][l].rearrange("(d o) -> d o", o=1),
                          [], [("hg", l, ni, hh)], "c_hg", slow=True)
            for br in range(3):
                B.dma("sp", bgate[:, l, br, :], b_gates[l, br].rearrange("(c p) -> p c", p=128),
                      [], [("bgate", l, br)], "c_bgate", slow=True)
        B.group(["hg"], "c_hg")
        B.dma("sp", negb[:], b_forget.rearrange("l h -> h l"), [], ["negb"], "c_negb", slow=True)
        B.dve(lambda e: e.tensor_scalar(out=negb[:], in0=negb[:], scalar1=-1.0, scalar2=None, op0=ALU.mult),
              ["negb"], ["negb"])
        for ni in (0, 2, 4):
            B.dve(lambda e, ni=ni: e.tensor_scalar(out=hg[:, :, ni:ni + 1], in0=hg[:, :, ni:ni + 1], scalar1=0.125,
                                                   scalar2=None, op0=ALU.mult), ["hg"], ["hg"])
        posi = B.sbuf("posi", [1, S], I32)
        posf = B.sbuf("posf", [1, S], F32)
        invf = B.sbuf("invf", [1, 128], F32)
        ang = B.sbuf("ang", [128, 512], F32)
        angi = B.sbuf("angi", [128, 512], I32)
        angf = B.sbuf("angf", [128, 512], F32)
        tb = [B.sbuf(f"tb{i}", [128, 512], F32) for i in range(2)]
        B.dma("sp", posi[:], pos_in[:, :], [], ["posi"], "c2")
        B.dma("sp", invf[:], cd["c_invf"][:, :], [], ["invf"], "c2")
        B.dve(lambda e: e.tensor_copy(out=posf[:], in_=posi[:]), ["posi"], ["posf"])
        for i in range(NTT):
            t0 = i * TT
            B.pe(lambda e, t0=t0: e.matmul(ps[0][:], invf[:], posf[:, t0:t0 + TT], start=True, stop=True),
                 ["invf", "posf"], [("ps", 0)])
            for k, (shift, dst) in enumerate(((0.0, SinD), (0.25, CosD))):
                B.dve(lambda e, shift=shift: e.tensor_scalar(out=ang[:], in0=ps[0][:], scalar1=float(1.0 / (2 * np.pi)),
                                                             scalar2=float(shift), op0=ALU.mult, op1=ALU.add),
                      [("ps", 0)], ["ang"])
                B.dve(lambda e: e.tensor_copy(out=angi[:], in_=ang[:]), ["ang"], ["angi"])
                B.dve(lambda e: e.tensor_copy(out=angf[:], in_=angi[:]), ["angi"], ["angf"])
                B.dve(lambda e: e.tensor_tensor(out=ang[:], in0=ang[:], in1=angf[:], op=ALU.subtract), ["ang", "angf"], ["ang"])
                B.dve(lambda e: e.tensor_scalar(out=angf[:], in0=ang[:], scalar1=0.5, scalar2=None, op0=ALU.is_gt), ["ang"], ["angf"])
                B.dve(lambda e: e.tensor_tensor(out=ang[:], in0=ang[:], in1=angf[:], op=ALU.subtract), ["ang", "angf"], ["ang"])
                B.act(lambda e, k=k: e.activation(out=tb[k][:], in_=ang[:], func=AF.Sin, scale=float(2 * np.pi)),
                      ["ang"], [("tb", k)])
                B.dma("sp", dst[:, t0:t0 + TT], tb[k][:], [("tb", k)], [], ("tb", k))
        B.end_phase()

    def rmsnorm_tile(X, xk, gi, hT, sq, rstd):
        for c in range(NDC):
            B.dve(lambda e, c=c: e.tensor_tensor(out=sq[:, c, :], in0=X[:, c, :], in1=X[:, c, :], op=ALU.mult),
                  [xk], [("sq", c)])
        for c in range(NDC):
            B.pe(lambda e, c=c: e.matmul(ps[0][:], ones[:], sq[:, c, :], start=(c == 0), stop=(c == NDC - 1)),
                 [("sq", c)], [("ps", 0)])
        B.act(lambda e: e.activation(out=rstd[:], in_=ps[0][:], func=AF.Sqrt, scale=1.0 / D, bias=epsb[:, 0:1]),
              [("ps", 0)], ["rstd"])
        B.dve(lambda e: e.reciprocal(out=rstd[:], in_=rstd[:]), ["rstd"], ["rstd"])
        for c in range(NDC):
            B.dve(lambda e, c=c: e.scalar_tensor_tensor(out=hT[:, c, :], in0=X[:, c, :], scalar=gvec[:, gi, c:c + 1],
                                                        in1=rstd[:], op0=ALU.mult, op1=ALU.mult),
                  [xk, "rstd"], [("hT", c)])

    def ffn_phase(l, wi, src, dst, first):
        nm = ("ffn1", "ffn2")[wi]
        w = ffn_w[nm]
        gi = l * 3 + (0, 2)[wi]
        Wg = B.sbuf("Wg", [128, NDC, DFF], BF16)
        Wu = B.sbuf("Wu", [128, NDC, DFF], BF16)
        Wd = B.sbuf("Wd", [128, NFC, D], BF16)
        xt = [B.sbuf(f"xt{i}", [128, NDC, TT], F32) for i in range(2)]
        hT = B.sbuf("hT", [128, NDC, TT], BF16)
        actT = B.sbuf("actT", [128, NFC, TT], BF16)
        sq = actT
        rstd = B.sbuf("rstd", [128, TT], F32)
        sil = [B.sbuf(f"sil{i}", [128, TT], F32) for i in range(2)]
        for c in range(NDC):
            B.dma("pool", Wg[:, c, :], w["wg"][l, c * 128:(c + 1) * 128, :], [], [("Wg", c)], "w0")
            B.dma("pool", Wu[:, c, :], w["wu"][l, c * 128:(c + 1) * 128, :], [], [("Wu", c)], "w1")
        for j in range(NFC):
            B.dma("pool", Wd[:, j, :], w["wd"][l, j * 128:(j + 1) * 128, :], [], [("Wd", j)], "w2")
        B.group([("Wg", c) for c in range(NDC)], "w0")
        B.group([("Wu", c) for c in range(NDC)], "w1")
        B.group([("Wd", j) for j in range(NFC)], "w2")

        def load_x(i):
            t0 = i * TT
            B.dma("sp", xt[i % 2][:], src[:, t0:t0 + TT].rearrange("(c p) t -> p c t", p=128),
                  [], [("xt", i % 2)], ("xt", i % 2))

        load_x(0)
        for i in range(NTT):
            if i + 1 < NTT:
                load_x(i + 1)
            X = xt[i % 2]
            xk = ("xt", i % 2)
            rmsnorm_tile(X, xk, gi, hT, sq, rstd)
            for j in range(NFC):
                pa = 1 + (j % 2)
                pu = 3 + (j % 2)
                for c in range(NDC):
                    B.pe(lambda e, c=c, j=j, pa=pa: e.matmul(ps[pa][:], Wg[:, c, j * 128:(j + 1) * 128], hT[:, c, :],
                                                             start=(c == 0), stop=(c == NDC - 1)),
                         [("Wg", c), ("hT", c)], [("ps", pa)])
                for c in range(NDC):
                    B.pe(lambda e, c=c, j=j, pu=pu: e.matmul(ps[pu][:], Wu[:, c, j * 128:(j + 1) * 128], hT[:, c, :],
                                                             start=(c == 0), stop=(c == NDC - 1)),
                         [("Wu", c), ("hT", c)], [("ps", pu)])
                B.act(lambda e, j=j, pa=pa: e.activation(out=sil[j % 2][:], in_=ps[pa][:], func=AF.Silu),
                      [("ps", pa)], [("sil", j % 2)])
                B.dve(lambda e, j=j, pu=pu: e.tensor_tensor(out=actT[:, j, :], in0=ps[pu][:], in1=sil[j % 2][:], op=ALU.mult),
                      [("ps", pu), ("sil", j % 2)], [("sq", j)])
            for m in range(NDC):
                py = 5 + (m % 2)
                for j in range(NFC):
                    B.pe(lambda e, m=m, j=j, py=py: e.matmul(ps[py][:], Wd[:, j, m * 128:(m + 1) * 128], actT[:, j, :],
                                                             start=(j == 0), stop=(j == NFC - 1)),
                         [("Wd", j), ("sq", j)], [("ps", py)])
                B.dve(lambda e, m=m, py=py, X=X: e.scalar_tensor_tensor(out=X[:, m, :], in0=ps[py][:], scalar=0.5, in1=X[:, m, :],
                                                                       op0=ALU.mult, op1=ALU.add),
                      [("ps", py), xk], [xk])
            t0 = i * TT
            B.dma("sp", dst[:, t0:t0 + TT].rearrange("(c p) t -> p c t", p=128), X[:],
                  [xk], [], ("xt", i % 2))
        B.end_phase()

    def proj_phase(l, src):
        gi = l * 3 + 1
        NW = C_G
        Win = B.sbuf("Win", [128, NDC, NW], BF16)
        xt = [B.sbuf(f"xt{i}", [128, NDC, TT], F32) for i in range(2)]
        hT = B.sbuf("hT", [128, NDC, TT], BF16)
        sq = B.sbuf("sq", [128, NDC, TT], BF16)
        rstd = B.sbuf("rstd", [128, TT], F32)
        q32_ = [B.sbuf("q32%d" % i_, [128, TT], F32) for i_ in range(2)]
        qsq_ = [B.sbuf("qsq%d" % i_, [128, TT], BF16) for i_ in range(2)]
        qr_ = [B.sbuf("qr%d" % i_, [128, TT], F32) for i_ in range(2)]
        qn32_ = [B.sbuf("qn32%d" % i_, [128, TT], F32) for i_ in range(2)]
        qnt_ = [B.sbuf("qnt%d" % i_, [128, TT], BF16) for i_ in range(2)]
        t1_ = [B.sbuf("t1%d" % i_, [128, TT], F32) for i_ in range(2)]
        t2_ = [B.sbuf("t2%d" % i_, [128, TT], F32) for i_ in range(2)]
        qnb = [B.sbuf(f"qnb{i}", [128, TT], BF16) for i in range(2)]
        cosT = B.sbuf("cosT", [128, TT], F32)
        sinT = B.sbuf("sinT", [128, TT], F32)
        vst = B.sbuf("vst", [128, 4, 1024], BF16)
        wist = B.sbuf("wist", [128, 4, 4], F32)
        lfe = B.sbuf("lfe", [6, TT], F32)
        nlft = B.sbuf("nlft", [6, TT], F32)

        for c in range(NDC):
            B.dma("pool", Win[:, c, :], w_in[l, c * 128:(c + 1) * 128, 0:NW], [], [("Win", c)], "w0")
        B.group([("Win", c) for c in range(NDC)], "w0")

        def load_x(i):
            t0 = i * TT
            B.dma("sp", xt[i % 2][:], src[:, t0:t0 + TT].rearrange("(c p) t -> p c t", p=128),
                  [], [("xt", i % 2)], ("xt", i % 2))

        pairs = []
        for p in range(3):
            pairs.append((C_QF + 128 * p, 128, 0, False, ("A", QfA, 2 * p)))
            pairs.append((C_KF + 128 * p, 128, 1, False, ("A", KfA, 2 * p)))
            pairs.append((C_QS + 128 * p, 128, 2, False, ("T", QsT, 2 * p)))
            pairs.append((C_KS + 128 * p, 128, 3, False, ("T", KsT, 2 * p)))
        for p in range(2):
            pairs.append((C_QC + 128 * p, 128, 4, True, ("T", QcT, 2 * p)))
            pairs.append((C_KC + 128 * p, 128, 5, True, ("T", KcT, 2 * p)))
            pairs.append((C_QI + 128 * p, 128, None, True, ("T", QiT, 2 * p)))
        pairs.append((C_KI, 64, None, True, ("K", KiT, 0)))

        load_x(0)
        for i in range(NTT):
            t0 = i * TT
            if i + 1 < NTT:
                load_x(i + 1)
            X = xt[i % 2]
            xk = ("xt", i % 2)
            B.dma("sp", cosT[:], CosD[:, t0:t0 + TT], [], ["cosT"], "cosT")
            B.dma("sp", sinT[:], SinD[:, t0:t0 + TT], [], ["sinT"], "sinT")
            rmsnorm_tile(X, xk, gi, hT, sq, rstd)
            def proj_mm(pi):
                col0, M = pairs[pi][0], pairs[pi][1]
                pb = 1 + (pi % 2)
                for c in range(NDC):
                    B.pe(lambda e, c=c, col0=col0, M=M, pb=pb: e.matmul(ps[pb][0:M, :], Win[:, c, col0:col0 + M], hT[:, c, :],
                                                                         start=(c == 0), stop=(c == NDC - 1)),
                         [("Win", c), ("hT", c)], [("ps", pb)])

            proj_mm(0)
            def pair_body(pi, col0, M, gidx, rope, dstd):
                pb = 1 + (pi % 2)
                slot = pi % 2
                if pi + 1 < len(pairs):
                    proj_mm(pi + 1)
                q32, qsq, qr, qn32, qnt, t1, t2 = (q32_[slot], qsq_[slot], qr_[slot], qn32_[slot], qnt_[slot], t1_[slot], t2_[slot])
                final = qnb[slot]
                fk = ("qnb", slot)
                if gidx is not None:
                    B.act(lambda e, pb=pb: e.activation(out=q32[:], in_=ps[pb][:], func=AF.Copy), [("ps", pb)], [("q32", slot)])
                    B.dve(lambda e: e.tensor_tensor(out=qsq[:], in0=q32[:], in1=q32[:], op=ALU.mult), [("q32", slot)], [("qsq", slot)])
                    B.pe(lambda e: e.matmul(ps[3][:], blockones[:], qsq[:], start=True, stop=True), [("qsq", slot)], [("ps", 3)])
                    B.act(lambda e: e.activation(out=qr[:], in_=ps[3][:], func=AF.Sqrt, scale=1.0 / 64, bias=epsb[:, 0:1]),
                          [("ps", 3)], [("qr", slot)])
                    B.dve(lambda e: e.reciprocal(out=qr[:], in_=qr[:]), [("qr", slot)], [("qr", slot)])
                    tgt = qn32 if rope else final
                    B.dve(lambda e, tgt=tgt, gidx=gidx: e.scalar_tensor_tensor(out=tgt[:], in0=q32[:], scalar=hg[:, l, gidx:gidx + 1],
                                                                              in1=qr[:], op0=ALU.mult, op1=ALU.mult),
                          [("q32", slot), ("qr", slot)], [("qn32", slot) if rope else fk])
                else:
                    B.act(lambda e, pb=pb, M=M: e.activation(out=qn32[0:M, :], in_=ps[pb][0:M, :], func=AF.Copy),
                          [("ps", pb)], [("qn32", slot)])
                if rope:
                    B.dve(lambda e, M=M: e.tensor_copy(out=qnt[0:M, :], in_=qn32[0:M, :]), [("qn32", slot)], [("qnt", slot)])
                    B.pe(lambda e, M=M: e.matmul(ps[4][0:M, :], rmat[0:M, 0:M], qnt[0:M, :], start=True, stop=True),
                         [("qnt", slot)], [("ps", 4)])
                    B.dve(lambda e, M=M: e.tensor_tensor(out=t1[0:M, :], in0=qn32[0:M, :], in1=cosT[0:M, :], op=ALU.mult),
                          [("qn32", slot), "cosT"], [("t1", slot)])
                    B.dve(lambda e, M=M: e.tensor_tensor(out=t2[0:M, :], in0=ps[4][0:M, :], in1=sinT[0:M, :], op=ALU.mult),
                          [("ps", 4), "sinT"], [("t2", slot)])
                    B.dve(lambda e, M=M, final=final: e.tensor_tensor(out=final[0:M, :], in0=t1[0:M, :], in1=t2[0:M, :], op=ALU.add),
                          [("t1", slot), ("t2", slot)], [fk])
                kind, dt_, h0 = dstd
                if kind == "A":
                    for hh in range(2):
                        B.dma("sp", dt_[h0 + hh, 0:64, t0:t0 + TT], final[hh * 64:(hh + 1) * 64, :], [fk], [], fk)
                elif kind == "T":
                    B.dma("sp", dt_[h0:h0 + 2, :, t0:t0 + TT].rearrange("h d t -> (h d) t"), final[:], [fk], [], fk)
                else:
                    B.dma("sp", dt_[:, t0:t0 + TT], final[0:64, :], [fk], [], fk)
            for pi_, pr_ in enumerate(pairs):
                pair_body(pi_, *pr_)
            k = 0
            for sub in range(4):
                for (col, n, d0) in ((C_VF, 384, 0), (C_VS, 384, 384), (C_VC, 256, 768)):
                    pv = 5 + (k % 2)
                    k += 1
                    for c in range(NDC):
                        B.pe(lambda e, c=c, sub=sub, col=col, n=n, pv=pv: e.matmul(
                            ps[pv][:, 0:n], hT[:, c, sub * 128:(sub + 1) * 128], Win[:, c, col:col + n],
                            start=(c == 0), stop=(c == NDC - 1)), [("Win", c), ("hT", c)], [("ps", pv)])
                    B.act(lambda e, sub=sub, n=n, d0=d0, pv=pv: e.activation(out=vst[:, sub, d0:d0 + n], in_=ps[pv][:, 0:n], func=AF.Copy),
                          [("ps", pv)], ["vst"])
                for c in range(NDC):
                    B.pe(lambda e, c=c, sub=sub: e.matmul(ps[7][:, 0:4], hT[:, c, sub * 128:(sub + 1) * 128], Win[:, c, C_WI:C_WI + 4],
                                                          start=(c == 0), stop=(c == NDC - 1)), [("Win", c), ("hT", c)], [("ps", 7)])
                B.dve(lambda e, sub=sub: e.tensor_scalar(out=wist[:, sub, :], in0=ps[7][:, 0:4], scalar1=1.0 / 16, scalar2=None, op0=ALU.mult),
                      [("ps", 7)], ["wist"])
            B.dma("sp", Vtok[t0:t0 + TT, :].rearrange("(s p) n -> p s n", p=128), vst[:], ["vst"], [], "vst")
            B.dma("sp", WiD[t0:t0 + TT, :].rearrange("(s p) n -> p s n", p=128), wist[:], ["wist"], [], "wist", slow=True)
            for c in range(NDC):
                B.pe(lambda e, c=c: e.matmul(ps[0][0:6, :], Win[:, c, C_FF:C_FF + 6], hT[:, c, :], start=(c == 0), stop=(c == NDC - 1)),
                     [("Win", c), ("hT", c)], [("ps", 0)])
            B.act(lambda e: e.activation(out=lfe[:], in_=ps[0][0:6, :], func=AF.Exp, scale=-1.0, bias=negb[:, l:l + 1]),
                  [("ps", 0), "negb"], ["lfe"])
            B.act(lambda e: e.activation(out=nlft[:], in_=lfe[:], func=AF.Ln, bias=oneb[0:6, 0:1], scale=1.0),
                  ["lfe"], ["nlft"])
            B.dma("sp", NLF[:, t0:t0 + TT], nlft[:], ["nlft"], [], "nlft")
        B.end_phase()
        nlf = B.sbuf("nlf", [6, S], F32)
        ncum = B.sbuf("ncum", [6, S], F32)
        one6 = B.sbuf("one6", [6, S], F32)
        hi6 = B.sbuf("hi6", [6, S], BF16)
        lo6 = B.sbuf("lo6", [6, S], BF16)
        nhi6 = B.sbuf("nhi6", [6, S], BF16)
        nlo6 = B.sbuf("nlo6", [6, S], BF16)
        ob6 = B.sbuf("ob6", [6, S], BF16)
        B.dma("sp", nlf[:], NLF[:, :], [], ["nlf"], "nlf")
        B.dve(lambda e: e.memset(one6[:], 1.0), [], ["one6"])
        B.dve(lambda e: e.memset(ob6[:], 1.0), [], ["ob6"])
        B.dve(lambda e: e.tensor_tensor_scan(out=ncum[:], data0=one6[:], data1=nlf[:], initial=0.0, op0=ALU.mult, op1=ALU.add),
              ["one6", "nlf"], ["ncum"])
        B.dve(lambda e: e.tensor_copy(out=hi6[:], in_=ncum[:]), ["ncum"], ["hi6"])
        B.dve(lambda e: e.tensor_tensor(out=lo6[:], in0=ncum[:], in1=hi6[:], op=ALU.subtract), ["ncum", "hi6"], ["lo6"])
        B.dve(lambda e: e.tensor_scalar(out=nhi6[:], in0=hi6[:], scalar1=-1.0, scalar2=None, op0=ALU.mult), ["hi6"], ["nhi6"])
        B.dve(lambda e: e.tensor_scalar(out=nlo6[:], in0=lo6[:], scalar1=-1.0, scalar2=None, op0=ALU.mult), ["lo6"], ["nlo6"])
        for row, t_, k_ in ((64, nhi6, "nhi6"), (65, nlo6, "nlo6"), (66, ob6, "ob6"), (67, ob6, "ob6")):
            B.dma("sp", QfA[:, row, :], t_[:], [k_], [], "aug")
        for row, t_, k_ in ((64, ob6, "ob6"), (65, ob6, "ob6"), (66, hi6, "hi6"), (67, lo6, "lo6")):
            B.dma("sp", KfA[:, row, :], t_[:], [k_], [], "aug")
        B.end_phase()

    def run_pipeline(items, LA):
        n = len(items)
        for j in range(min(LA, n)):
            items[j][0]()
        for i in range(n):
            if i + LA < n:
                items[i + LA][0]()
            items[i][1]()

    def nop():
        pass

    def fox_phase():
        NS = 3
        Ka = [B.sbuf(f"Ka{i}", [68, S], BF16) for i in range(2)]
        Qa = [B.sbuf(f"Qa{i}", [68, S], BF16) for i in range(2)]
        V = [B.sbuf(f"V{i}", [128, NKT, 65], BF16) for i in range(2)]
        mb = B.sbuf("mb", [128, 4, 512], BF16)
        ed = B.sbuf("ed", [65, 64], F32)
        pT = [B.sbuf(f"pT{i}", [128, 512], BF16) for i in range(NS)]
        osb = [B.sbuf(f"osb{i}", [65, 512], F32) for i in range(2)]
        rec = B.sbuf("rec", [64, 512], F32)
        ost = [B.sbuf(f"ost{i}", [64, 512], BF16) for i in range(2)]
        B.dma("pool", mb[:], cd["c_mbig_fox"][:, :, :], [], ["mb"], "c0")
        B.dma("sp", ed[:], cd["c_edenom"][:, :], [], ["ed"], "c2")
        for i in range(2):
            B.dve(lambda e, i=i: e.memset(V[i][:, :, 64:65], 1.0), [], [("Vone", i)])

        def load_head(h):
            b = h % 2
            B.dma("sp", Ka[b][:], KfA[h, :, :], [], [("Ka", b)], ("Ka", b))
            B.dma("sp", Qa[b][:], QfA[h, :, :], [], [("Qa", b)], ("Qa", b))
            B.dma("sp", V[b][:, :, 0:64], Vtok[:, h * 64:(h + 1) * 64].rearrange("(kt p) d -> p kt d", p=128),
                  [], [("V", b)], ("V", b))

        items = []
        cnt = [0, 0]

        def mk(h, qb, kt, nk):
            b = h % 2
            pb = cnt[0] % NS
            cnt[0] += 1
            grp = h * 8 + qb
            po = 4 + (grp % 2)
            so = grp % 2
            diag = kt >= 4 * qb

            def front():
                B.pe(lambda e: e.matmul(ps[pb][:], Ka[b][:, kt * 128:(kt + 1) * 128], Qa[b][:, qb * 512:(qb + 1) * 512],
                                        start=True, stop=not diag), [("Ka", b), ("Qa", b)], [("ps", pb)])
                if diag:
                    B.pe(lambda e: e.matmul(ps[pb][:], ident[:], mb[:, kt - 4 * qb, :], start=False, stop=True),
                         ["mb"], [("ps", pb)])

            def back():
                if kt == 0 and qb == 0 and h + 1 < 6:
                    load_head(h + 1)
                B.act(lambda e: e.activation(out=pT[pb][:], in_=ps[pb][:], func=AF.Exp), [("ps", pb)], [("pT", pb)])
                B.pe(lambda e: e.matmul(ps[po][0:65, :], V[b][:, kt, :], pT[pb][:], start=(kt == 0), stop=(kt == nk - 1)),
                     [("V", b), ("Vone", b), ("pT", pb)], [("ps", po)])
                if kt == nk - 1:
                    B.act(lambda e: e.activation(out=osb[so][:], in_=ps[po][0:65, :], func=AF.Copy), [("ps", po)], [("osb", so)])
                    B.pe(lambda e: e.matmul(ps[6][0:64, :], ed[:], osb[so][:], start=True, stop=True), ["ed", ("osb", so)], [("ps", 6)])
                    B.dve(lambda e: e.reciprocal(out=rec[:], in_=ps[6][0:64, :]), [("ps", 6)], ["rec"])
                    B.dve(lambda e: e.tensor_tensor(out=ost[so][:], in0=osb[so][0:64, :], in1=rec[:], op=ALU.mult),
                          [("osb", so), "rec"], [("ost", so)])
                    B.dma("sp", OT[h * 64:(h + 1) * 64, qb * 512:(qb + 1) * 512], ost[so][:], [("ost", so)], [], ("ost", so))

            return (front, back)

        load_head(0)
        for h in range(6):
            for qb in range(8):
                nk = 4 * (qb + 1)
                for kt in range(nk):
                    items.append(mk(h, qb, kt, nk))
        run_pipeline(items, NS - 1)
        B.end_phase()

    def sb_phase():
        Kt = [B.sbuf(f"Kt{i}", [64, S], BF16) for i in range(2)]
        Qt = [B.sbuf(f"Qt{i}", [64, S], BF16) for i in range(2)]
        V = [B.sbuf(f"V{i}", [128, NKT, 64], BF16) for i in range(2)]
        mb = B.sbuf("mb", [128, 4, 512], BF16)
        m01 = B.sbuf("m01", [128, 4, 512], BF16)
        esel = B.sbuf("esel", [128, 32, 64], BF16)
        nsel = B.sbuf("nsel", [64, 32, 128], BF16)
        spT = [B.sbuf(f"spT{i}", [128, NKT, 512], BF16) for i in range(2)]
        e32 = [B.sbuf(f"e32{i}", [128, 512], F32) for i in range(3)]
        csh = [B.sbuf(f"csh{i}", [64, 512], BF16) for i in range(2)]
        hi2 = B.sbuf("hi2", [64, 512], BF16)
        aT = [B.sbuf(f"aT{i}", [128, 512], BF16) for i in range(3)]
        ost = [B.sbuf(f"ost{i}", [64, 512], BF16) for i in range(2)]
        B.dma("pool", mb[:], cd["c_mbig_sb"][:, :, :], [], ["mb"], "c0")
        B.dma("pool", m01[:], cd["c_m01_sb"][:, :, :], [], ["m01"], "c0")
        B.dma("pool", esel[:], cd["c_esel2"][:, :, :], [], ["esel"], "c0")
        B.dma("pool", nsel[:], cd["c_negsel"][:, :, :], [], ["nsel"], "c0")
        B.group(["mb", "m01", "esel", "nsel"], "c0")

        def load_head(h):
            b = h % 2
            B.dma("sp", Kt[b][:], KsT[h, :, :], [], [("Kt", b)], ("Kt", b))
            B.dma("sp", Qt[b][:], QsT[h, :, :], [], [("Qt", b)], ("Qt", b))
            B.dma("sp", V[b][:], Vtok[:, 384 + h * 64:384 + (h + 1) * 64].rearrange("(kt p) d -> p kt d", p=128),
                  [], [("V", b)], ("V", b))

        cnt = [0, 0]

        def mk_p1(h, qb, kt, nk, grp):
            b = h % 2
            pb = cnt[0] % 3
            cnt[0] += 1
            gs = grp % 2
            qs = slice(qb * 512, (qb + 1) * 512)

            def front():
                B.pe(lambda e: e.matmul(ps[pb][:], Kt[b][:, kt * 128:(kt + 1) * 128], Qt[b][:, qs], start=True, stop=True),
                     [("Kt", b), ("Qt", b)], [("ps", pb)])

            def back():
                B.act(lambda e: e.activation(out=e32[pb][:], in_=ps[pb][:], func=AF.Exp), [("ps", pb)], [("e32", pb)])
                B.act(lambda e: e.activation(out=spT[gs][:, kt, :], in_=e32[pb][:], func=AF.Ln, bias=oneb[:, 0:1], scale=1.0),
                      [("e32", pb)], [("sp", gs, kt)])
                if kt >= 4 * qb:
                    B.dve(lambda e: e.tensor_tensor(out=spT[gs][:, kt, :], in0=spT[gs][:, kt, :], in1=m01[:, kt - 4 * qb, :], op=ALU.mult),
                          [("sp", gs, kt), "m01"], [("sp", gs, kt)])
                B.pe(lambda e: e.matmul(ps[3][0:64, :], esel[:, kt, :], spT[gs][:, kt, :], start=(kt == 0), stop=(kt == nk - 1)),
                     ["esel", ("sp", gs, kt)], [("ps", 3)])
                if kt == nk - 1:
                    B.dve(lambda e: e.tensor_copy(out=csh[gs][0:32, :], in_=ps[3][0:32, :]), [("ps", 3)], [("csh0", gs)])
                    B.dve(lambda e: e.tensor_copy(out=hi2[32:64, :], in_=ps[3][32:64, :]), [("ps", 3)], ["hi2"])
                    B.dve(lambda e: e.tensor_tensor(out=csh[gs][32:64, :], in0=ps[3][32:64, :], in1=hi2[32:64, :], op=ALU.subtract),
                          [("ps", 3), "hi2"], [("csh1", gs)])

            return (front, back)

        def mk_p2(h, qb, kt, nk, grp):
            b = h % 2
            pl = 4 + (cnt[1] % 3)
            sl = cnt[1] % 3
            cnt[1] += 1
            gs = grp % 2
            so = grp % 2
            qs = slice(qb * 512, (qb + 1) * 512)
            diag = kt >= 4 * qb

            def front():
                B.pe(lambda e: e.matmul(ps[pl][:], Kt[b][:, kt * 128:(kt + 1) * 128], Qt[b][:, qs], start=True, stop=False),
                     [("Kt", b), ("Qt", b)], [("ps", pl)])
                B.pe(lambda e: e.matmul(ps[pl][:], negtri[:], spT[gs][:, kt, :], start=False, stop=False),
                     [("sp", gs, kt)], [("ps", pl)])
                B.pe(lambda e: e.matmul(ps[pl][:], nsel[:, kt, :], csh[gs][:], start=False, stop=not diag),
                     ["nsel", ("csh0", gs), ("csh1", gs)], [("ps", pl)])
                if diag:
                    B.pe(lambda e: e.matmul(ps[pl][:], ident[:], mb[:, kt - 4 * qb, :], start=False, stop=True),
                         ["mb"], [("ps", pl)])

            def back():
                B.act(lambda e: e.activation(out=aT[sl][:], in_=ps[pl][:], func=AF.Exp), [("ps", pl)], [("aT", sl)])
                B.pe(lambda e: e.matmul(ps[7][0:64, :], V[b][:, kt, :], aT[sl][:], start=(kt == 0), stop=(kt == nk - 1)),
                     [("V", b), ("aT", sl)], [("ps", 7)])
                if kt == nk - 1:
                    B.act(lambda e: e.activation(out=ost[so][:], in_=ps[7][0:64, :], func=AF.Copy), [("ps", 7)], [("ost", so)])
                    B.dma("sp", OT[384 + h * 64:384 + (h + 1) * 64, qs], ost[so][:], [("ost", so)], [], ("ost", so))
                    if qb == 7 and h + 2 < 6:
                        load_head(h + 2)

            return (front, back)

        groups = [(h, qb) for h in range(6) for qb in range(8)]
        items = []

        def add_p1(gi):
            h, qb = groups[gi]
            nk = 4 * (qb + 1)
            for kt in range(nk):
                items.append(mk_p1(h, qb, kt, nk, gi))

        def add_p2(gi):
            h, qb = groups[gi]
            nk = 4 * (qb + 1)
            for kt in range(nk):
                items.append(mk_p2(h, qb, kt, nk, gi))

        load_head(0)
        load_head(1)
        add_p1(0)
        for gi in range(1, len(groups)):
            add_p1(gi)
            add_p2(gi - 1)
        add_p2(len(groups) - 1)
        run_pipeline(items, 2)
        B.end_phase()

    def dsa_phase():
        Kc = B.sbuf("Kc", [64, 4, S], BF16)
        Ki = B.sbuf("Ki", [64, S], BF16)
        V = B.sbuf("V", [128, NKT, 4, 65], BF16)
        Qc = [B.sbuf(f"Qc{i}", [64, 4, 512], BF16) for i in range(2)]
        Qi = [B.sbuf(f"Qi{i}", [64, 4, 512], BF16) for i in range(2)]
        wi = [B.sbuf(f"wi{i}", [128, 4, 4], F32) for i in range(2)]
        score = B.sbuf("score", [128, 4, S], F32)
        Mc = B.sbuf("Mc", [128, 4, S], BF16)
        junk = B.sbuf("junk", [128, S], BF16)
        junk2 = B.sbuf("junk2", [128, S], BF16)
        rl = [B.sbuf(f"rl{i}", [128, 512], F32) for i in range(2)]
        cz = B.sbuf("cz", [128, 128], F32)
        ed = B.sbuf("ed", [65, 64], F32)
        mx = B.sbuf("mx", [128, 4], F32)
        mn = B.sbuf("mn", [128, 4], F32)
        w0 = B.sbuf("w0", [128, 4], F32)
        lo = B.sbuf("lo", [128, 4], F32)
        mid = B.sbuf("mid", [128, 4], F32)
        cnt8 = B.sbuf("cnt8", [128, 8], F32)
        tot = B.sbuf("tot", [128, 4], F32)
        thrv = B.sbuf("thrv", [128, 4], F32)
        prd = B.sbuf("prd", [128, 4], F32)
        pT = [B.sbuf(f"pT{i}", [128, 512], BF16) for i in range(3)]
        osb = [B.sbuf(f"osb{i}", [65, 512], F32) for i in range(2)]
        rec = B.sbuf("rec", [64, 512], F32)
        ost = [B.sbuf(f"ost{i}", [64, 512], BF16) for i in range(2)]
        B.dma("sp", cz[:], cd["c_causal"][:, :], [], ["cz"], "c2")
        B.dma("sp", ed[:], cd["c_edenom"][:, :], [], ["ed"], "c2")
        B.group(["cz", "ed"], "c2")
        for h in range(4):
            B.dma("sp", Kc[:, h, :], KcT[h, :, :], [], [("Kc", h)], "Kc")
        B.group([("Kc", h) for h in range(4)], "Kc")
        B.dma("sp", Ki[:], KiT[:, :], [], ["Ki"], "Ki")
        B.dve(lambda e: e.memset(V[:, :, :, 64:65], 1.0), [], ["Vone"])
        for h in range(4):
            B.dma("sp", V[:, :, h, 0:64], Vtok[:, 768 + h * 64:768 + (h + 1) * 64].rearrange("(kt p) d -> p kt d", p=128),
                  [], [("V", h)], "V")
        B.group([("V", h) for h in range(4)], "V")

        def load_q(qb):
            b = qb % 2
            qs = slice(qb * 512, (qb + 1) * 512)
            for h in range(4):
                B.dma("sp", Qc[b][:, h, :], QcT[h, :, qs], [], [("Qc", b, h)], ("Qc", b))
                B.dma("sp", Qi[b][:, h, :], QiT[h, :, qs], [], [("Qi", b, h)], ("Qi", b))
            B.group([("Qc", b, h) for h in range(4)], ("Qc", b))
            B.group([("Qi", b, h) for h in range(4)], ("Qi", b))
            B.dma("sp", wi[b][:], WiD[qs, :].rearrange("(s p) n -> p s n", p=128), [], [("wi", b)], ("wi", b), slow=True)

        cn = [0, 0, 0]

        def idx_item(qb, g, kc, h, wdt):
            b = qb % 2
            ks = slice(kc * 512, kc * 512 + wdt)
            pb = cn[0] % 2
            cn[0] += 1
            r = cn[1] % 2
            cn[1] += 1

            def front():
                B.pe(lambda e: e.matmul(ps[pb][:, 0:wdt], Qi[b][:, h, g * 128:(g + 1) * 128], Ki[:, ks], start=True, stop=True),
                     [("Qi", b, h), "Ki"], [("ps", pb)])

            def back():
                B.act(lambda e: e.activation(out=rl[r][:, 0:wdt], in_=ps[pb][:, 0:wdt], func=AF.Relu), [("ps", pb)], [("rl", r)])
                if h == 0:
                    B.dve(lambda e: e.tensor_scalar(out=score[:, g, ks], in0=rl[r][:, 0:wdt], scalar1=wi[b][:, g, 0:1], scalar2=None,
                                                    op0=ALU.mult), [("rl", r), ("wi", b)], [("score", g)])
                else:
                    B.dve(lambda e: e.scalar_tensor_tensor(out=score[:, g, ks], in0=rl[r][:, 0:wdt], scalar=wi[b][:, g, h:h + 1],
                                                           in1=score[:, g, ks], op0=ALU.mult, op1=ALU.add),
                          [("rl", r), ("wi", b), ("score", g)], [("score", g)])

            return (front, back)

        def att_item(qb, h, kt, nk):
            b = qb % 2
            pl = 3 + (cn[2] % 3)
            sl = cn[2] % 3
            cn[2] += 1
            po = 6 + (h % 2)
            so = h % 2
            gmin = max(0, kt - 4 * qb)

            def front():
                B.pe(lambda e: e.matmul(ps[pl][:], Kc[:, h, kt * 128:(kt + 1) * 128], Qc[b][:, h, :], start=True, stop=False),
                     [("Kc", h), ("Qc", b, h)], [("ps", pl)])
                if gmin > 0:
                    B.pe(lambda e: e.matmul(ps[pl][:, 0:gmin * 128], ident[:], mbz[:, 0:gmin * 128], start=False, stop=False),
                         [], [("ps", pl)])
                for g in range(gmin, 4):
                    B.pe(lambda e, g=g: e.matmul(ps[pl][:, g * 128:(g + 1) * 128], Mc[:, g, kt * 128:(kt + 1) * 128], nbident[:],
                                                 start=False, stop=(g == 3)), [("Mc", g)], [("ps", pl)])

            def back():
                B.act(lambda e: e.activation(out=pT[sl][:], in_=ps[pl][:], func=AF.Exp), [("ps", pl)], [("pT", sl)])
                B.pe(lambda e: e.matmul(ps[po][0:65, :], V[:, kt, h, :], pT[sl][:], start=(kt == 0), stop=(kt == nk - 1)),
                     [("V", h), "Vone", ("pT", sl)], [("ps", po)])
                if kt == nk - 1:
                    B.act(lambda e: e.activation(out=osb[so][:], in_=ps[po][0:65, :], func=AF.Copy), [("ps", po)], [("osb", so)])
                    B.pe(lambda e: e.matmul(ps[2][0:64, :], ed[:], osb[so][:], start=True, stop=True), ["ed", ("osb", so)], [("ps", 2)])
                    B.dve(lambda e: e.reciprocal(out=rec[:], in_=ps[2][0:64, :]), [("ps", 2)], ["rec"])
                    B.dve(lambda e: e.tensor_tensor(out=ost[so][:], in0=osb[so][0:64, :], in1=rec[:], op=ALU.mult),
                          [("osb", so), "rec"], [("ost", so)])
                    B.dma("sp", OT[768 + h * 64:768 + (h + 1) * 64, qb * 512:(qb + 1) * 512], ost[so][:], [("ost", so)], [], ("ost", so))

            return (front, back)

        def count_pass(qb, g):
            kmax = (qb * 4 + g + 1) * 128
            cD = max(128, int(round(kmax * 0.47 / 128.0)) * 128) if kmax > 128 else kmax
            nA = kmax - cD
            B.dve(lambda e: e.tensor_scalar(out=junk[:, 0:cD], in0=score[:, g, 0:cD], scalar1=mid[:, g:g + 1], scalar2=0.0,
                                            op0=ALU.is_ge, op1=ALU.add, accum_out=cnt8[:, g:g + 1]),
                  [("score", g), "mid", "cnt8"], ["junk", ("cntD", g)])
            if nA > 0:
                B.act(lambda e: e.activation(out=junk2[:, cD:kmax], in_=score[:, g, cD:kmax], func=AF.Sign, bias=mid[:, g:g + 1],
                                             scale=-1.0, accum_out=cnt8[:, 4 + g:5 + g]),
                      [("score", g), "mid", "cnt8"], ["junk2", ("cntA", g)])
            return nA

        load_q(0)
        for qb in range(8):
            if qb + 1 < 8:
                load_q(qb + 1)
            for g in range(4):
                qt = qb * 4 + g
                kmax = (qt + 1) * 128
                nch = (kmax + 511) // 512
                its = []
                for kc in range(nch):
                    wdt = min(512, kmax - kc * 512)
                    for h in range(4):
                        its.append(idx_item(qb, g, kc, h, wdt))
                run_pipeline(its, 1)
                B.dve(lambda e, g=g, kmax=kmax: e.tensor_reduce(out=mx[:, g:g + 1], in_=score[:, g, 0:kmax], axis=AX.X, op=ALU.max),
                      [("score", g)], ["mx"])
                B.dve(lambda e, g=g, kmax=kmax: e.tensor_reduce(out=mn[:, g:g + 1], in_=score[:, g, 0:kmax], axis=AX.X, op=ALU.min),
                      [("score", g)], ["mn"])
                B.dve(lambda e, g=g, kmax=kmax: e.tensor_tensor(out=score[:, g, kmax - 128:kmax], in0=score[:, g, kmax - 128:kmax],
                                                                in1=cz[:], op=ALU.add), [("score", g), "cz"], [("score", g)])
            B.dve(lambda e: e.tensor_tensor(out=w0[:], in0=mx[:], in1=mn[:], op=ALU.subtract), ["mx", "mn"], ["w0"])
            B.dve(lambda e: e.tensor_scalar(out=w0[:], in0=w0[:], scalar1=1.0001, scalar2=1e-6, op0=ALU.mult, op1=ALU.add), ["w0"], ["w0"])
            B.dve(lambda e: e.tensor_copy(out=lo[:], in_=mn[:]), ["mn"], ["lo"])
            for g in range(4):
                kmax = (qb * 4 + g + 1) * 128
                cD = max(128, int(round(kmax * 0.47 / 128.0)) * 128) if kmax > 128 else kmax
                nA = kmax - cD
                B.dve(lambda e, g=g, nA=nA: e.memset(thrv[:, g:g + 1], float(TOPK) - 0.5 * nA), [], ["thrv"])
            for s_ in range(NBIS):
                hf = 0.5 ** (s_ + 1)
                B.dve(lambda e, hf=hf: e.scalar_tensor_tensor(out=mid[:], in0=w0[:], scalar=hf, in1=lo[:], op0=ALU.mult, op1=ALU.add),
                      ["w0", "lo"], ["mid"])
                B.dve(lambda e: e.memset(cnt8[:], 0.0), [("cntD", g) for g in range(4)] + [("cntA", g) for g in range(4)], ["cnt8"])
                for g in (3, 2, 1, 0):
                    count_pass(qb, g)
                B.dve(lambda e: e.scalar_tensor_tensor(out=tot[:], in0=cnt8[:, 4:8], scalar=-0.5, in1=cnt8[:, 0:4], op0=ALU.mult, op1=ALU.add),
                      [("cntD", g) for g in range(4)] + [("cntA", g) for g in range(4)] + ["cnt8"], ["tot"])
                B.dve(lambda e: e.tensor_tensor(out=prd[:], in0=tot[:], in1=thrv[:], op=ALU.is_ge), ["tot", "thrv"], ["prd"])
                B.dve(lambda e: e.tensor_tensor(out=prd[:], in0=prd[:], in1=w0[:], op=ALU.mult), ["prd", "w0"], ["prd"])
                B.dve(lambda e, hf=hf: e.scalar_tensor_tensor(out=lo[:], in0=prd[:], scalar=hf, in1=lo[:], op0=ALU.mult, op1=ALU.add),
                      ["prd", "lo"], ["lo"])
            for g in range(4):
                kmax = (qb * 4 + g + 1) * 128
                B.dve(lambda e, g=g, kmax=kmax: e.tensor_scalar(out=Mc[:, g, 0:kmax], in0=score[:, g, 0:kmax], scalar1=lo[:, g:g + 1],
                                                                scalar2=None, op0=ALU.is_lt), [("score", g), "lo"], [("Mc", g)])
            nk = 4 * (qb + 1)
            its = []
            for h in range(4):
                for kt in range(nk):
                    its.append(att_item(qb, h, kt, nk))
            run_pipeline(its, 2)
        B.end_phase()

    def out_phase(l, xsrc):
        gi = l * 3 + 1
        Wgt = B.sbuf("Wgt", [128, NDC, 3 * D], BF16)
        Wbr = B.sbuf("Wbr", [128, NDC, D], BF16)
        Wo = B.sbuf("Wo", [128, NDC, D], BF16)
        xt = [B.sbuf(f"xt{i}", [128, NDC, TT], F32) for i in range(2)]
        ot = [B.sbuf(f"ot{i}", [128, NDC, TT], BF16) for i in range(2)]
        hT = B.sbuf("hT", [128, NDC, TT], BF16)
        sq = B.sbuf("sq", [128, NDC, TT], BF16)
        rstd = B.sbuf("rstd", [128, TT], F32)
        sg = [B.sbuf(f"sg{i}", [128, TT], F32) for i in range(2)]
        macc = B.sbuf("macc", [128, TT], F32)
        mt = B.sbuf("mt", [128, TT], F32)
        mrg = B.sbuf("mrg", [128, NDC, TT], BF16)
        for c in range(NDC):
            B.dma("pool", Wgt[:, c, :], w_in[l, c * 128:(c + 1) * 128, C_G:NIN], [], [("Wgt", c)], "w0")
            B.dma("pool", Wo[:, c, :], w_out[l, c * 128:(c + 1) * 128, :], [], [("Wo", c)], "w1")
        brch = [(0, 0), (0, 1), (0, 2), (1, 0), (1, 1), (1, 2), (2, 0), (2, 1)]
        for c, (br, j) in enumerate(brch):
            B.dma("pool", Wbr[:, c, :], w_br[br][l, j * 128:(j + 1) * 128, :], [], [("Wbr", c)], "w2")
        B.group([("Wgt", c) for c in range(NDC)], "w0")
        B.group([("Wo", c) for c in range(NDC)], "w1")
        B.group([("Wbr", c) for c in range(NDC)], "w2")

        def load(i):
            t0 = i * TT
            B.dma("sp", xt[i % 2][:], xsrc[:, t0:t0 + TT].rearrange("(c p) t -> p c t", p=128), [], [("xt", i % 2)], ("xt", i % 2))
            B.dma("sp", ot[i % 2][:], OT[:, t0:t0 + TT].rearrange("(c p) t -> p c t", p=128), [], [("ot", i % 2)], ("ot", i % 2))

        load(0)
        k = 0
        for i in range(NTT):
            if i + 1 < NTT:
                load(i + 1)
            X = xt[i % 2]
            xk = ("xt", i % 2)
            O = ot[i % 2]
            ok = ("ot", i % 2)
            rmsnorm_tile(X, xk, gi, hT, sq, rstd)
            brc = ((0, 1, 2), (3, 4, 5), (6, 7))
            for m in range(NDC):
                for br in range(3):
                    pg = 1 + (k % 2)
                    pbb = 3 + (k % 2)
                    sgi = k % 2
                    k += 1
                    col = br * D + m * 128
                    for c in range(NDC):
                        B.pe(lambda e, c=c, col=col, pg=pg: e.matmul(ps[pg][:], Wgt[:, c, col:col + 128], hT[:, c, :],
                                                                      start=(c == 0), stop=(c == NDC - 1)),
                             [("Wgt", c), ("hT", c)], [("ps", pg)])
                    cs_ = brc[br]
                    for ci, c in enumerate(cs_):
                        B.pe(lambda e, c=c, ci=ci, m=m, pbb=pbb, n_=len(cs_), O=O: e.matmul(ps[pbb][:], Wbr[:, c, m * 128:(m + 1) * 128], O[:, c, :],
                                                                                            start=(ci == 0), stop=(ci == n_ - 1)),
                             [("Wbr", c), ok], [("ps", pbb)])
                    B.act(lambda e, pg=pg, sgi=sgi, br=br, m=m: e.activation(out=sg[sgi][:], in_=ps[pg][:], func=AF.Sigmoid,
                                                                            bias=bgate[:, l, br, m:m + 1], scale=1.0),
                          [("ps", pg)], [("sg", sgi)])
                    if br == 0:
                        B.dve(lambda e, pbb=pbb, sgi=sgi: e.tensor_tensor(out=macc[:], in0=ps[pbb][:], in1=sg[sgi][:], op=ALU.mult),
                              [("ps", pbb), ("sg", sgi)], ["macc"])
                    else:
                        B.dve(lambda e, pbb=pbb, sgi=sgi: e.tensor_tensor(out=mt[:], in0=ps[pbb][:], in1=sg[sgi][:], op=ALU.mult),
                              [("ps", pbb), ("sg", sgi)], ["mt"])
                        if br == 1:
                            B.dve(lambda e: e.tensor_tensor(out=macc[:], in0=macc[:], in1=mt[:], op=ALU.add), ["macc", "mt"], ["macc"])
                        else:
                            B.dve(lambda e, m=m: e.tensor_tensor(out=mrg[:, m, :], in0=macc[:], in1=mt[:], op=ALU.add),
                                  ["macc", "mt"], [("mrg", m)])
            for e_ in range(NDC):
                py = 5 + (e_ % 2)
                for m in range(NDC):
                    B.pe(lambda e, e_=e_, m=m, py=py: e.matmul(ps[py][:], Wo[:, m, e_ * 128:(e_ + 1) * 128], mrg[:, m, :],
                                                               start=(m == 0), stop=(m == NDC - 1)),
                         [("Wo", m), ("mrg", m)], [("ps", py)])
                B.dve(lambda e, e_=e_, py=py, X=X: e.tensor_tensor(out=X[:, e_, :], in0=ps[py][:], in1=X[:, e_, :], op=ALU.add),
                      [("ps", py), xk], [xk])
            t0 = i * TT
            B.dma("sp", outT[:, t0:t0 + TT].rearrange("(c p) t -> p c t", p=128), X[:], [xk], [], ("xt", i % 2))
        B.end_phase()

    phase0()
    for l in range(DEPTH):
        if on(f"ffn1_{l}"):
            ffn_phase(l, 0, xT_in if l == 0 else outT, outT, l == 0)
        if on(f"proj_{l}"):
            proj_phase(l, outT)
        if on(f"fox_{l}"):
            fox_phase()
        if on(f"sb_{l}"):
            sb_phase()
        if on(f"dsa_{l}"):
            dsa_phase()
        if on(f"out_{l}"):
            out_phase(l, outT)
        if on(f"ffn2_{l}"):
            ffn_phase(l, 1, outT, outT, False)

    B.sc.close()
    for g in reversed(psg):
        g.__exit__(None, None, None)
    for g in reversed(B._glob):
        g.__exit__(None, None, None)
    return nc


_WNAMES = ("ffn1_norm", "ffn1_w_gate", "ffn1_w_up", "ffn1_w_down", "mix_norm", "w_in", "b_forget", "b_gates",
           "q_norm_fox", "k_norm_fox", "q_norm_sb", "k_norm_sb", "q_norm_dsa", "k_norm_dsa",
           "w_branch_fox", "w_branch_sb", "w_branch_dsa", "w_out",
           "ffn2_norm", "ffn2_w_gate", "ffn2_w_up", "ffn2_w_down")


def kernel(**inputs):
    n = 8
    phases = inputs.pop("_phases", ("all",))
    ncores = inputs.pop("_ncores", n)
    nc = build_program(phases)
    x = np.asarray(inputs["x"])
    pos = np.asarray(inputs["positions"]).astype(np.int32)
    shared = {k: np.ascontiguousarray(np.asarray(inputs[k], dtype=np.float32)) for k in _WNAMES}
    shared.update(_CONST)
    in_maps = []
    for b in range(ncores):
        m = dict(shared)
        m["xT"] = np.ascontiguousarray(x[b].T)
        m["pos"] = np.ascontiguousarray(pos[b:b + 1])
        in_maps.append(m)
    res = run_bass_kernel_spmd(nc, in_maps, core_ids=list(range(ncores)))
    if any(p.startswith("dbgOT") for p in phases):
        return res.results[0]["OT"]
    out = np.stack([np.ascontiguousarray(r["outT"].T) for r in res.results], axis=0)
    return out.astype(np.float32, copy=False)
```

```python
import numpy as np
import concourse.bass as bass
import concourse.mybir as mybir
from concourse.bass_utils import run_bass_kernel_spmd

F32 = mybir.dt.float32
BF16 = mybir.dt.bfloat16
I32 = mybir.dt.int32
AF = mybir.ActivationFunctionType
ALU = mybir.AluOpType
AX = mybir.AxisListType

D = 1024
S = 4096
DEPTH = 2
DFF = 2816
NFC = DFF // 128
NDC = D // 128
TT = 512
NTT = S // TT
NKT = S // 128
EPS = 1e-6
BIG = 30000.0
TOPK = 256
NBIS = 24

C_QF, C_KF, C_VF, C_FF = 0, 384, 768, 1152
C_QS, C_KS, C_VS = 1158, 1542, 1926
C_QC, C_KC, C_VC = 2310, 2566, 2822
C_QI, C_KI, C_WI = 3078, 3334, 3398
C_G = 3402
NIN = 6474


class Sched:
    ENG = ("pe", "act", "dve", "pool", "sp")

    def __init__(self, nc):
        self.nc = nc
        self._new_phase()
        self._reset()

    def _new_phase(self):
        self.known = {e: {} for e in self.ENG}
        self.chan_n = {}
        self.n = {e: 0 for e in self.ENG}
        self.sig_base = {e: 0 for e in self.ENG}
        self.sems = {}
        self.chan_sems = {}

    def _reset(self):
        self.ops = {e: [] for e in self.ENG}
        self.lastw = {}
        self.readers = {}
        self.signal = set()

    def add(self, eng, emit, reads=(), writes=(), dma=False, chan=None):
        deps = {}

        def need(d):
            for sk, o in d.items():
                if deps.get(sk, 0) < o:
                    deps[sk] = o

        for r in reads:
            need(self.lastw.get(r, {}))
        for w in writes:
            need(self.lastw.get(w, {}))
            need(self.readers.get(w, {}))
        if dma:
            self.chan_n[chan] = self.chan_n.get(chan, 0) + 1
            me = (("ch", chan), self.chan_n[chan])
        else:
            self.n[eng] += 1
            me = (eng, self.n[eng])
        waits = []
        kn = self.known[eng]
        for sk, o in deps.items():
            if sk == "pe" and eng == "pe" and not dma:
                continue
            if kn.get(sk, 0) >= o:
                continue
            kn[sk] = o
            waits.append((sk, o))
            if not isinstance(sk, tuple):
                self.signal.add((sk, o))
        for w in writes:
            self.lastw[w] = {me[0]: me[1]}
            self.readers[w] = {}
        for r in reads:
            rd = self.readers.setdefault(r, {})
            if rd.get(me[0], 0) < me[1]:
                rd[me[0]] = me[1]
        self.ops[eng].append(dict(emit=emit, waits=waits, me=me, dma=dma))
        return me

    def end_phase(self):
        nc = self.nc
        waits = []
        for chan, n in self.chan_n.items():
            sk = ("ch", chan)
            if self.known["sp"].get(sk, 0) < n:
                waits.append((sk, n))
                self.known["sp"][sk] = n
        self.ops["sp"].append(dict(emit=None, waits=waits, me=None, dma=False))
        self._pid = getattr(self, "_pid", 0) + 1
        snap = nc.snapshot_sems()
        for e in self.ENG:
            self.sems[e] = nc.alloc_semaphore(f"s{self._pid}_{e}")
        for i, ch in enumerate(self.chan_n):
            self.chan_sems[ch] = nc.alloc_semaphore(f"s{self._pid}_ch{i}")
        sigrank = {}
        cnt = {}
        for e in self.ENG:
            ords = sorted(o for (sk, o) in self.signal if sk == e)
            for i, o in enumerate(ords):
                sigrank[(e, o)] = self.sig_base[e] + i + 1
            cnt[e] = len(ords)

        def semval(sk, o):
            if isinstance(sk, tuple):
                return self.chan_sems[sk[1]], 16 * o
            return self.sems[sk], sigrank[(sk, o)]

        ops = self.ops
        signal = self.signal
        import os
        if os.environ.get("KDBG"):
            print("PHASE", self._pid, {e: (len(ops[e]), cnt[e]) for e in self.ENG}, "chan max", max([16 * n for n in self.chan_n.values()] + [0]),
                  "nchan", len(self.chan_n), flush=True)

        def run(eng_name):
            def body(eng):
                for op in ops[eng_name]:
                    for sk, o in op["waits"]:
                        s, v = semval(sk, o)
                        eng.wait_ge(s, v)
                    if op["emit"] is None:
                        continue
                    ins = op["emit"](eng)
                    me = op["me"]
                    if op["dma"]:
                        ins.then_inc(self.chan_sems[me[0][1]], 16)
                    elif me in signal:
                        ins.then_inc(self.sems[me[0]], 1)
            return body

        with nc.Block() as block:
            block.tensor(run("pe"))
            block.scalar(run("act"))
            block.vector(run("dve"))
            block.gpsimd(run("pool"))
            block.sync(run("sp"))
        nc.clear_and_free_semaphores(nc.allocated_since(snap))
        nc.all_engine_barrier()
        self._new_phase()
        self._reset()

    def close(self):
        pass


class Builder:
    def __init__(self):
        self.nc = bass.Bass("TRN2", target_bir_lowering=False)
        self.sc = Sched(self.nc)
        self._glob = []
        self._scope = []

    def din(self, name, shape, dt=F32):
        return self.nc.dram_tensor(name, list(shape), dt, kind="ExternalInput").ap()

    def dout(self, name, shape, dt=F32):
        return self.nc.dram_tensor(name, list(shape), dt, kind="ExternalOutput").ap()

    def dscr(self, name, shape, dt=F32):
        return self.nc.dram_tensor(name, list(shape), dt, kind="Internal").ap()

    def sbuf(self, name, shape, dt, glob=False):
        self._uid = getattr(self, "_uid", 0) + 1
        g = self.nc.sbuf_tensor(f"{name}_{self._uid}", list(shape), dt)
        t = g.__enter__()
        (self._glob if glob else self._scope).append(g)
        return t

    def end_phase(self):
        self.sc.end_phase()
        for g in reversed(self._scope):
            g.__exit__(None, None, None)
        self._scope = []

    def pe(self, emit, reads, writes):
        return self.sc.add("pe", emit, reads, writes)

    def act(self, emit, reads, writes):
        return self.sc.add("act", emit, reads, writes)

    def dve(self, emit, reads, writes):
        return self.sc.add("dve", emit, reads, writes)

    def pool(self, emit, reads, writes):
        return self.sc.add("pool", emit, reads, writes)

    def group(self, keys, chan):
        n = self.sc.chan_n[chan]
        for k in keys:
            self.sc.lastw[k] = {("ch", chan): n}

    def dma(self, q, out, in_, reads, writes, chan, slow=False):
        return self.sc.add(q, lambda e: e.dma_start(out=out, in_=in_, allow_slow_non_contiguous=slow),
                           reads, writes, dma=True, chan=chan)


def _consts():
    c = {}
    c["c_ones"] = np.ones((128, 128), np.float32)
    bo = np.zeros((128, 128), np.float32)
    bo[:64, :64] = 1
    bo[64:, 64:] = 1
    c["c_blockones"] = bo
    rm = np.zeros((128, 128), np.float32)
    for hh in (0, 64):
        for j in range(8):
            rm[hh + 8 + j, hh + j] = -1.0
            rm[hh + j, hh + 8 + j] = 1.0
    c["c_rmat"] = rm
    c["c_ident"] = np.eye(128, dtype=np.float32)
    j = np.arange(128)[:, None]
    s = np.arange(128)[None, :]
    c["c_negtri"] = -(j >= s).astype(np.float32)
    t = np.arange(512)[None, None, :]
    i4 = np.arange(4)[None, :, None]
    jj = np.arange(128)[:, None, None]
    kg = 128 * i4 + jj
    c["c_mbig_fox"] = (-BIG * (kg > t)).astype(np.float32)
    c["c_mbig_sb"] = (-BIG * (kg >= t)).astype(np.float32)
    c["c_m01_sb"] = (kg < t).astype(np.float32)
    es = np.zeros((128, 32, 72), np.float32)
    for kt in range(32):
        es[:, kt, kt] = 1
        es[:, kt, 32 + kt] = 1
    c["c_esel2"] = es
    ns = np.zeros((72, 32, 128), np.float32)
    for kt in range(32):
        for r in range(64):
            if (r % 32) > kt:
                ns[r, kt, :] = -1
    c["c_negsel"] = ns
    c["c_nbident"] = (-BIG * np.eye(128)).astype(np.float32)
    q = np.arange(128)[:, None]
    k = np.arange(128)[None, :]
    c["c_causal"] = (-1e30 * (k > q)).astype(np.float32)
    invf = np.zeros((1, 128), np.float32)
    half = 8
    f = (500000.0 ** (-np.arange(half, dtype=np.float32) * 2.0 / 16.0)).astype(np.float32)
    for hh in (0, 64):
        invf[0, hh:hh + 8] = f
        invf[0, hh + 8:hh + 16] = f
    c["c_invf"] = invf
    ed = np.zeros((65, 64), np.float32)
    ed[64, :] = 1
    c["c_edenom"] = ed
    return c


_CONST = _consts()


def build_program(phases=("all",)):
    B = Builder()
    nc = B.nc
    ALLP = "all" in phases

    def on(p):
        return ALLP or p in phases

    xT_in = B.din("xT", [D, S])
    pos_in = B.din("pos", [1, S], I32)
    ffn_w = {}
    for nm in ("ffn1", "ffn2"):
        ffn_w[nm] = dict(
            norm=B.din(nm + "_norm", [DEPTH, D]),
            wg=B.din(nm + "_w_gate", [DEPTH, D, DFF]),
            wu=B.din(nm + "_w_up", [DEPTH, D, DFF]),
            wd=B.din(nm + "_w_down", [DEPTH, DFF, D]),
        )
    mix_norm = B.din("mix_norm", [DEPTH, D])
    w_in = B.din("w_in", [DEPTH, D, NIN])
    b_forget = B.din("b_forget", [DEPTH, 6])
    b_gates = B.din("b_gates", [DEPTH, 3, D])
    hn_names = ("q_norm_fox", "k_norm_fox", "q_norm_sb", "k_norm_sb", "q_norm_dsa", "k_norm_dsa")
    hn = {n: B.din(n, [DEPTH, 64]) for n in hn_names}
    w_br = [B.din("w_branch_fox", [DEPTH, 384, D]), B.din("w_branch_sb", [DEPTH, 384, D]),
            B.din("w_branch_dsa", [DEPTH, 256, D])]
    w_out = B.din("w_out", [DEPTH, D, D])
    cd = {k: B.din(k, list(v.shape)) for k, v in _CONST.items()}
    outT = B.dout("outT", [D, S])

    QfA = B.dscr("QfA", [6, 68, S], BF16)
    KfA = B.dscr("KfA", [6, 68, S], BF16)
    QsT = B.dscr("QsT", [6, 64, S], BF16)
    KsT = B.dscr("KsT", [6, 64, S], BF16)
    QcT = B.dscr("QcT", [4, 64, S], BF16)
    KcT = B.dscr("KcT", [4, 64, S], BF16)
    QiT = B.dscr("QiT", [4, 64, S], BF16)
    KiT = B.dscr("KiT", [64, S], BF16)
    Vtok = B.dscr("Vtok", [S, 1024], BF16)
    WiD = B.dscr("WiD", [S, 4], F32)
    NLF = B.dscr("NLF", [6, S], F32)
    CosD = B.dscr("CosD", [128, S], F32)
    SinD = B.dscr("SinD", [128, S], F32)
    dbg = [p for p in phases if p.startswith("dbgOT")]
    if dbg:
        OT = B.dout("OT", [D, S], BF16)
    else:
        OT = B.dscr("OT", [D, S], BF16)

    ones = B.sbuf("ones", [128, 128], BF16, glob=True)
    blockones = B.sbuf("blockones", [128, 128], BF16, glob=True)
    rmat = B.sbuf("rmat", [128, 128], BF16, glob=True)
    ident = B.sbuf("ident", [128, 128], BF16, glob=True)
    negtri = B.sbuf("negtri", [128, 128], BF16, glob=True)
    nbident = B.sbuf("nbident", [128, 128], BF16, glob=True)
    mbz = B.sbuf("mbz", [128, 512], BF16, glob=True)
    epsb = B.sbuf("epsb", [128, 1], F32, glob=True)
    oneb = B.sbuf("oneb", [128, 1], F32, glob=True)
    negpi = B.sbuf("negpi", [128, 1], F32, glob=True)
    gvec = B.sbuf("gvec", [128, 6, NDC], F32, glob=True)
    hg = B.sbuf("hg", [128, DEPTH, 6], F32, glob=True)
    negb = B.sbuf("negb", [6, DEPTH], F32, glob=True)
    bgate = B.sbuf("bgate", [128, DEPTH, 3, NDC], F32, glob=True)

    psg = [nc.psum_tensor(f"ps{i}", [128, 512], F32) for i in range(8)]
    ps = [g.__enter__() for g in psg]

    def phase0():
        B.dma("pool", ones[:], cd["c_ones"][:, :], [], ["c"], "c0")
        B.dma("pool", blockones[:], cd["c_blockones"][:, :], [], ["c"], "c0")
        B.dma("pool", rmat[:], cd["c_rmat"][:, :], [], ["c"], "c0")
        B.dma("pool", ident[:], cd["c_ident"][:, :], [], ["c"], "c0")
        B.dma("pool", negtri[:], cd["c_negtri"][:, :], [], ["c"], "c0")
        B.dma("pool", nbident[:], cd["c_nbident"][:, :], [], ["c"], "c0")
        B.dve(lambda e: e.memset(epsb[:], EPS), [], ["epsb"])
        B.dve(lambda e: e.memset(oneb[:], 1.0), [], ["oneb"])
        B.dve(lambda e: e.memset(negpi[:], -float(np.pi)), [], ["negpi"])
        B.dve(lambda e: e.memset(mbz[:], -BIG), [], ["mbz"])
        for l in range(DEPTH):
            for wi, src in enumerate((ffn_w["ffn1"]["norm"], mix_norm, ffn_w["ffn2"]["norm"])):
                B.dma("sp", gvec[:, l * 3 + wi, :], src[l].rearrange("(c p) -> p c", p=128),
                      [], [("gvec", l, wi)], "c_gvec", slow=True)
            for ni, n in enumerate(hn_names):
                for hh in (0, 64):
                    B.dma("sp", hg[hh:hh + 64, l, ni:ni + 1], hn[n][l].rearrange("(d o) -> d o", o=1),
                          [], [("hg", l, ni, hh)], "c_hg", slow=True)
            for br in range(3):
                B.dma("sp", bgate[:, l, br, :], b_gates[l, br].rearrange("(c p) -> p c", p=128),
                      [], [("bgate", l, br)], "c_bgate", slow=True)
        B.group(["hg"], "c_hg")
        B.dma("sp", negb[:], b_forget.rearrange("l h -> h l"), [], ["negb"], "c_negb", slow=True)
        B.dve(lambda e: e.tensor_scalar(out=negb[:], in0=negb[:], scalar1=-1.0, scalar2=None, op0=ALU.mult),
              ["negb"], ["negb"])
        for ni in (0, 2, 4):
            B.dve(lambda e, ni=ni: e.tensor_scalar(out=hg[:, :, ni:ni + 1], in0=hg[:, :, ni:ni + 1], scalar1=0.125,
                                                   scalar2=None, op0=ALU.mult), ["hg"], ["hg"])
        posi = B.sbuf("posi", [1, S], I32)
        posf = B.sbuf("posf", [1, S], F32)
        invf = B.sbuf("invf", [1, 128], F32)
        ang = B.sbuf("ang", [128, 512], F32)
        angi = B.sbuf("angi", [128, 512], I32)
        angf = B.sbuf("angf", [128, 512], F32)
        tb = [B.sbuf(f"tb{i}", [128, 512], F32) for i in range(2)]
        B.dma("sp", posi[:], pos_in[:, :], [], ["posi"], "c2")
        B.dma("sp", invf[:], cd["c_invf"][:, :], [], ["invf"], "c2")
        B.dve(lambda e: e.tensor_copy(out=posf[:], in_=posi[:]), ["posi"], ["posf"])
        for i in range(NTT):
            t0 = i * TT
            B.pe(lambda e, t0=t0: e.matmul(ps[0][:], invf[:], posf[:, t0:t0 + TT], start=True, stop=True),
                 ["invf", "posf"], [("ps", 0)])
            for k, (shift, dst) in enumerate(((0.0, SinD), (0.25, CosD))):
                B.dve(lambda e, shift=shift: e.tensor_scalar(out=ang[:], in0=ps[0][:], scalar1=float(1.0 / (2 * np.pi)),
                                                             scalar2=float(shift), op0=ALU.mult, op1=ALU.add),
                      [("ps", 0)], ["ang"])
                B.dve(lambda e: e.tensor_copy(out=angi[:], in_=ang[:]), ["ang"], ["angi"])
                B.dve(lambda e: e.tensor_copy(out=angf[:], in_=angi[:]), ["angi"], ["angf"])
                B.dve(lambda e: e.tensor_tensor(out=ang[:], in0=ang[:], in1=angf[:], op=ALU.subtract), ["ang", "angf"], ["ang"])
                B.dve(lambda e: e.tensor_scalar(out=angf[:], in0=ang[:], scalar1=0.5, scalar2=None, op0=ALU.is_gt), ["ang"], ["angf"])
                B.dve(lambda e: e.tensor_tensor(out=ang[:], in0=ang[:], in1=angf[:], op=ALU.subtract), ["ang", "angf"], ["ang"])
                B.act(lambda e, k=k: e.activation(out=tb[k][:], in_=ang[:], func=AF.Sin, scale=float(2 * np.pi)),
                      ["ang"], [("tb", k)])
                B.dma("sp", dst[:, t0:t0 + TT], tb[k][:], [("tb", k)], [], ("tb", k))
        B.end_phase()

    def rmsnorm_tile(X, xk, gi, hT, sq, rstd):
        for c in range(NDC):
            B.dve(lambda e, c=c: e.tensor_tensor(out=sq[:, c, :], in0=X[:, c, :], in1=X[:, c, :], op=ALU.mult),
                  [xk], [("sq", c)])
        for c in range(NDC):
            B.pe(lambda e, c=c: e.matmul(ps[0][:], ones[:], sq[:, c, :], start=(c == 0), stop=(c == NDC - 1)),
                 [("sq", c)], [("ps", 0)])
        B.act(lambda e: e.activation(out=rstd[:], in_=ps[0][:], func=AF.Sqrt, scale=1.0 / D, bias=epsb[:, 0:1]),
              [("ps", 0)], ["rstd"])
        B.dve(lambda e: e.reciprocal(out=rstd[:], in_=rstd[:]), ["rstd"], ["rstd"])
        for c in range(NDC):
            B.dve(lambda e, c=c: e.scalar_tensor_tensor(out=hT[:, c, :], in0=X[:, c, :], scalar=gvec[:, gi, c:c + 1],
                                                        in1=rstd[:], op0=ALU.mult, op1=ALU.mult),
                  [xk, "rstd"], [("hT", c)])

    def ffn_phase(l, wi, src, dst, first):
        nm = ("ffn1", "ffn2")[wi]
        w = ffn_w[nm]
        gi = l * 3 + (0, 2)[wi]
        Wg = B.sbuf("Wg", [128, NDC, DFF], BF16)
        Wu = B.sbuf("Wu", [128, NDC, DFF], BF16)
        Wd = B.sbuf("Wd", [128, NFC, D], BF16)
        xt = [B.sbuf(f"xt{i}", [128, NDC, TT], F32) for i in range(2)]
        hT = B.sbuf("hT", [128, NDC, TT], BF16)
        actT = B.sbuf("actT", [128, NFC, TT], BF16)
        sq = actT
        rstd = B.sbuf("rstd", [128, TT], F32)
        sil = [B.sbuf(f"sil{i}", [128, TT], F32) for i in range(2)]
        for c in range(NDC):
            B.dma("pool", Wg[:, c, :], w["wg"][l, c * 128:(c + 1) * 128, :], [], [("Wg", c)], "w0")
            B.dma("pool", Wu[:, c, :], w["wu"][l, c * 128:(c + 1) * 128, :], [], [("Wu", c)], "w1")
        for j in range(NFC):
            B.dma("pool", Wd[:, j, :], w["wd"][l, j * 128:(j + 1) * 128, :], [], [("Wd", j)], "w2")
        B.group([("Wg", c) for c in range(NDC)], "w0")
        B.group([("Wu", c) for c in range(NDC)], "w1")
        B.group([("Wd", j) for j in range(NFC)], "w2")

        def load_x(i):
            t0 = i * TT
            B.dma("sp", xt[i % 2][:], src[:, t0:t0 + TT].rearrange("(c p) t -> p c t", p=128),
                  [], [("xt", i % 2)], ("xt", i % 2))

        load_x(0)
        for i in range(NTT):
            if i + 1 < NTT:
                load_x(i + 1)
            X = xt[i % 2]
            xk = ("xt", i % 2)
            rmsnorm_tile(X, xk, gi, hT, sq, rstd)
            for j in range(NFC):
                pa = 1 + (j % 2)
                pu = 3 + (j % 2)
                for c in range(NDC):
                    B.pe(lambda e, c=c, j=j, pa=pa: e.matmul(ps[pa][:], Wg[:, c, j * 128:(j + 1) * 128], hT[:, c, :],
                                                             start=(c == 0), stop=(c == NDC - 1)),
                         [("Wg", c), ("hT", c)], [("ps", pa)])
                for c in range(NDC):
                    B.pe(lambda e, c=c, j=j, pu=pu: e.matmul(ps[pu][:], Wu[:, c, j * 128:(j + 1) * 128], hT[:, c, :],
                                                             start=(c == 0), stop=(c == NDC - 1)),
                         [("Wu", c), ("hT", c)], [("ps", pu)])
                B.act(lambda e, j=j, pa=pa: e.activation(out=sil[j % 2][:], in_=ps[pa][:], func=AF.Silu),
                      [("ps", pa)], [("sil", j % 2)])
                B.dve(lambda e, j=j, pu=pu: e.tensor_tensor(out=actT[:, j, :], in0=ps[pu][:], in1=sil[j % 2][:], op=ALU.mult),
                      [("ps", pu), ("sil", j % 2)], [("sq", j)])
            for m in range(NDC):
                py = 5 + (m % 2)
                for j in range(NFC):
                    B.pe(lambda e, m=m, j=j, py=py: e.matmul(ps[py][:], Wd[:, j, m * 128:(m + 1) * 128], actT[:, j, :],
                                                             start=(j == 0), stop=(j == NFC - 1)),
                         [("Wd", j), ("sq", j)], [("ps", py)])
                B.dve(lambda e, m=m, py=py, X=X: e.scalar_tensor_tensor(out=X[:, m, :], in0=ps[py][:], scalar=0.5, in1=X[:, m, :],
                                                                       op0=ALU.mult, op1=ALU.add),
                      [("ps", py), xk], [xk])
            t0 = i * TT
            B.dma("sp", dst[:, t0:t0 + TT].rearrange("(c p) t -> p c t", p=128), X[:],
                  [xk], [], ("xt", i % 2))
        B.end_phase()

    def proj_phase(l, src):
        gi = l * 3 + 1
        NW = C_G
        Win = B.sbuf("Win", [128, NDC, NW], BF16)
        xt = [B.sbuf(f"xt{i}", [128, NDC, TT], F32) for i in range(2)]
        hT = B.sbuf("hT", [128, NDC, TT], BF16)
        sq = B.sbuf("sq", [128, NDC, TT], BF16)
        rstd = B.sbuf("rstd", [128, TT], F32)
        q32_ = [B.sbuf("q32%d" % i_, [128, TT], F32) for i_ in range(2)]
        qsq_ = [B.sbuf("qsq%d" % i_, [128, TT], BF16) for i_ in range(2)]
        qr_ = [B.sbuf("qr%d" % i_, [128, TT], F32) for i_ in range(2)]
        qn32_ = [B.sbuf("qn32%d" % i_, [128, TT], F32) for i_ in range(2)]
        qnt_ = [B.sbuf("qnt%d" % i_, [128, TT], BF16) for i_ in range(2)]
        t1_ = [B.sbuf("t1%d" % i_, [128, TT], F32) for i_ in range(2)]
        t2_ = [B.sbuf("t2%d" % i_, [128, TT], F32) for i_ in range(2)]
        qnb = [B.sbuf(f"qnb{i}", [128, TT], BF16) for i in range(2)]
        cosT = B.sbuf("cosT", [128, TT], F32)
        sinT = B.sbuf("sinT", [128, TT], F32)
        vst = B.sbuf("vst", [128, 4, 1024], BF16)
        wist = B.sbuf("wist", [128, 4, 4], F32)
        lfe = B.sbuf("lfe", [6, TT], F32)
        nlft = B.sbuf("nlft", [6, TT], F32)

        for c in range(NDC):
            B.dma("pool", Win[:, c, :], w_in[l, c * 128:(c + 1) * 128, 0:NW], [], [("Win", c)], "w0")
        B.group([("Win", c) for c in range(NDC)], "w0")

        def load_x(i):
            t0 = i * TT
            B.dma("sp", xt[i % 2][:], src[:, t0:t0 + TT].rearrange("(c p) t -> p c t", p=128),
                  [], [("xt", i % 2)], ("xt", i % 2))

        pairs = []
        for p in range(3):
            pairs.append((C_QF + 128 * p, 128, 0, False, ("A", QfA, 2 * p)))
            pairs.append((C_KF + 128 * p, 128, 1, False, ("A", KfA, 2 * p)))
            pairs.append((C_QS + 128 * p, 128, 2, False, ("T", QsT, 2 * p)))
            pairs.append((C_KS + 128 * p, 128, 3, False, ("T", KsT, 2 * p)))
        for p in range(2):
            pairs.append((C_QC + 128 * p, 128, 4, True, ("T", QcT, 2 * p)))
            pairs.append((C_KC + 128 * p, 128, 5, True, ("T", KcT, 2 * p)))
            pairs.append((C_QI + 128 * p, 128, None, True, ("T", QiT, 2 * p)))
        pairs.append((C_KI, 64, None, True, ("K", KiT, 0)))

        load_x(0)
        for i in range(NTT):
            t0 = i * TT
            if i + 1 < NTT:
                load_x(i + 1)
            X = xt[i % 2]
            xk = ("xt", i % 2)
            B.dma("sp", cosT[:], CosD[:, t0:t0 + TT], [], ["cosT"], "cosT")
            B.dma("sp", sinT[:], SinD[:, t0:t0 + TT], [], ["sinT"], "sinT")
            rmsnorm_tile(X, xk, gi, hT, sq, rstd)
            def proj_mm(pi):
                col0, M = pairs[pi][0], pairs[pi][1]
                pb = 1 + (pi % 2)
                for c in range(NDC):
                    B.pe(lambda e, c=c, col0=col0, M=M, pb=pb: e.matmul(ps[pb][0:M, :], Win[:, c, col0:col0 + M], hT[:, c, :],
                                                                         start=(c == 0), stop=(c == NDC - 1)),
                         [("Win", c), ("hT", c)], [("ps", pb)])

            proj_mm(0)
            def pair_body(pi, col0, M, gidx, rope, dstd):
                pb = 1 + (pi % 2)
                slot = pi % 2
                if pi + 1 < len(pairs):
                    proj_mm(pi + 1)
                q32, qsq, qr, qn32, qnt, t1, t2 = (q32_[slot], qsq_[slot], qr_[slot], qn32_[slot], qnt_[slot], t1_[slot], t2_[slot])
                final = qnb[slot]
                fk = ("qnb", slot)
                if gidx is not None:
                    B.act(lambda e, pb=pb: e.activation(out=q32[:], in_=ps[pb][:], func=AF.Copy), [("ps", pb)], [("q32", slot)])
                    B.dve(lambda e: e.tensor_tensor(out=qsq[:], in0=q32[:], in1=q32[:], op=ALU.mult), [("q32", slot)], [("qsq", slot)])
                    B.pe(lambda e: e.matmul(ps[3][:], blockones[:], qsq[:], start=True, stop=True), [("qsq", slot)], [("ps", 3)])
                    B.act(lambda e: e.activation(out=qr[:], in_=ps[3][:], func=AF.Sqrt, scale=1.0 / 64, bias=epsb[:, 0:1]),
                          [("ps", 3)], [("qr", slot)])
                    B.dve(lambda e: e.reciprocal(out=qr[:], in_=qr[:]), [("qr", slot)], [("qr", slot)])
                    tgt = qn32 if rope else final
                    B.dve(lambda e, tgt=tgt, gidx=gidx: e.scalar_tensor_tensor(out=tgt[:], in0=q32[:], scalar=hg[:, l, gidx:gidx + 1],
                                                                              in1=qr[:], op0=ALU.mult, op1=ALU.mult),
                          [("q32", slot), ("qr", slot)], [("qn32", slot) if rope else fk])
                else:
                    B.act(lambda e, pb=pb, M=M: e.activation(out=qn32[0:M, :], in_=ps[pb][0:M, :], func=AF.Copy),
                          [("ps", pb)], [("qn32", slot)])
                if rope:
                    B.dve(lambda e, M=M: e.tensor_copy(out=qnt[0:M, :], in_=qn32[0:M, :]), [("qn32", slot)], [("qnt", slot)])
                    B.pe(lambda e, M=M: e.matmul(ps[4][0:M, :], rmat[0:M, 0:M], qnt[0:M, :], start=True, stop=True),
                         [("qnt", slot)], [("ps", 4)])
                    B.dve(lambda e, M=M: e.tensor_tensor(out=t1[0:M, :], in0=qn32[0:M, :], in1=cosT[0:M, :], op=ALU.mult),
                          [("qn32", slot), "cosT"], [("t1", slot)])
                    B.dve(lambda e, M=M: e.tensor_tensor(out=t2[0:M, :], in0=ps[4][0:M, :], in1=sinT[0:M, :], op=ALU.mult),
                          [("ps", 4), "sinT"], [("t2", slot)])
                    B.dve(lambda e, M=M, final=final: e.tensor_tensor(out=final[0:M, :], in0=t1[0:M, :], in1=t2[0:M, :], op=ALU.add),
                          [("t1", slot), ("t2", slot)], [fk])
                kind, dt_, h0 = dstd
                if kind == "A":
                    for hh in range(2):
                        B.dma("sp", dt_[h0 + hh, 0:64, t0:t0 + TT], final[hh * 64:(hh + 1) * 64, :], [fk], [], fk)
                elif kind == "T":
                    B.dma("sp", dt_[h0:h0 + 2, :, t0:t0 + TT].rearrange("h d t -> (h d) t"), final[:], [fk], [], fk)
                else:
                    B.dma("sp", dt_[:, t0:t0 + TT], final[0:64, :], [fk], [], fk)
            for pi_, pr_ in enumerate(pairs):
                pair_body(pi_, *pr_)
            k = 0
            for sub in range(4):
                for (col, n, d0) in ((C_VF, 384, 0), (C_VS, 384, 384), (C_VC, 256, 768)):
                    pv = 5 + (k % 2)
                    k += 1
                    for c in range(NDC):
                        B.pe(lambda e, c=c, sub=sub, col=col, n=n, pv=pv: e.matmul(
                            ps[pv][:, 0:n], hT[:, c, sub * 128:(sub + 1) * 128], Win[:, c, col:col + n],
                            start=(c == 0), stop=(c == NDC - 1)), [("Win", c), ("hT", c)], [("ps", pv)])
                    B.act(lambda e, sub=sub, n=n, d0=d0, pv=pv: e.activation(out=vst[:, sub, d0:d0 + n], in_=ps[pv][:, 0:n], func=AF.Copy),
                          [("ps", pv)], ["vst"])
                for c in range(NDC):
                    B.pe(lambda e, c=c, sub=sub: e.matmul(ps[7][:, 0:4], hT[:, c, sub * 128:(sub + 1) * 128], Win[:, c, C_WI:C_WI + 4],
                                                          start=(c == 0), stop=(c == NDC - 1)), [("Win", c), ("hT", c)], [("ps", 7)])
                B.dve(lambda e, sub=sub: e.tensor_scalar(out=wist[:, sub, :], in0=ps[7][:, 0:4], scalar1=1.0 / 16, scalar2=None, op0=ALU.mult),
                      [("ps", 7)], ["wist"])
            B.dma("sp", Vtok[t0:t0 + TT, :].rearrange("(s p) n -> p s n", p=128), vst[:], ["vst"], [], "vst")
            B.dma("sp", WiD[t0:t0 + TT, :].rearrange("(s p) n -> p s n", p=128), wist[:], ["wist"], [], "wist", slow=True)
            for c in range(NDC):
                B.pe(lambda e, c=c: e.matmul(ps[0][0:6, :], Win[:, c, C_FF:C_FF + 6], hT[:, c, :], start=(c == 0), stop=(c == NDC - 1)),
                     [("Win", c), ("hT", c)], [("ps", 0)])
            B.act(lambda e: e.activation(out=lfe[:], in_=ps[0][0:6, :], func=AF.Exp, scale=-1.0, bias=negb[:, l:l + 1]),
                  [("ps", 0), "negb"], ["lfe"])
            B.act(lambda e: e.activation(out=nlft[:], in_=lfe[:], func=AF.Ln, bias=oneb[0:6, 0:1], scale=1.0),
                  ["lfe"], ["nlft"])
            B.dma("sp", NLF[:, t0:t0 + TT], nlft[:], ["nlft"], [], "nlft")
        B.end_phase()
        nlf = B.sbuf("nlf", [6, S], F32)
        ncum = B.sbuf("ncum", [6, S], F32)
        one6 = B.sbuf("one6", [6, S], F32)
        hi6 = B.sbuf("hi6", [6, S], BF16)
        lo6 = B.sbuf("lo6", [6, S], BF16)
        nhi6 = B.sbuf("nhi6", [6, S], BF16)
        nlo6 = B.sbuf("nlo6", [6, S], BF16)
        ob6 = B.sbuf("ob6", [6, S], BF16)
        B.dma("sp", nlf[:], NLF[:, :], [], ["nlf"], "nlf")
        B.dve(lambda e: e.memset(one6[:], 1.0), [], ["one6"])
        B.dve(lambda e: e.memset(ob6[:], 1.0), [], ["ob6"])
        B.dve(lambda e: e.tensor_tensor_scan(out=ncum[:], data0=one6[:], data1=nlf[:], initial=0.0, op0=ALU.mult, op1=ALU.add),
              ["one6", "nlf"], ["ncum"])
        B.dve(lambda e: e.tensor_copy(out=hi6[:], in_=ncum[:]), ["ncum"], ["hi6"])
        B.dve(lambda e: e.tensor_tensor(out=lo6[:], in0=ncum[:], in1=hi6[:], op=ALU.subtract), ["ncum", "hi6"], ["lo6"])
        B.dve(lambda e: e.tensor_scalar(out=nhi6[:], in0=hi6[:], scalar1=-1.0, scalar2=None, op0=ALU.mult), ["hi6"], ["nhi6"])
        B.dve(lambda e: e.tensor_scalar(out=nlo6[:], in0=lo6[:], scalar1=-1.0, scalar2=None, op0=ALU.mult), ["lo6"], ["nlo6"])
        for row, t_, k_ in ((64, nhi6, "nhi6"), (65, nlo6, "nlo6"), (66, ob6, "ob6"), (67, ob6, "ob6")):
            B.dma("sp", QfA[:, row, :], t_[:], [k_], [], "aug")
        for row, t_, k_ in ((64, ob6, "ob6"), (65, ob6, "ob6"), (66, hi6, "hi6"), (67, lo6, "lo6")):
            B.dma("sp", KfA[:, row, :], t_[:], [k_], [], "aug")
        B.end_phase()

    def run_pipeline(items, LA):
        n = len(items)
        for j in range(min(LA, n)):
            items[j][0]()
        for i in range(n):
            if i + LA < n:
                items[i + LA][0]()
            items[i][1]()

    def nop():
        pass

    def run_pipeline3(items):
        n = len(items)
        for t in range(-2, n):
            if 0 <= t + 2 < n:
                items[t + 2][0]()
            if 0 <= t + 1 < n:
                items[t + 1][1]()
            if 0 <= t < n:
                items[t][2]()

    def fox_phase():
        NS = 3
        Ka = [B.sbuf(f"Ka{i}", [68, S], BF16) for i in range(2)]
        Qa = [B.sbuf(f"Qa{i}", [68, S], BF16) for i in range(2)]
        V = [B.sbuf(f"V{i}", [128, NKT, 65], BF16) for i in range(2)]
        mb = B.sbuf("mb", [128, 4, 512], BF16)
        ed = B.sbuf("ed", [65, 64], F32)
        pT = [B.sbuf(f"pT{i}", [128, 512], BF16) for i in range(NS)]
        osb = [B.sbuf(f"osb{i}", [65, 512], F32) for i in range(2)]
        rec = B.sbuf("rec", [64, 512], F32)
        ost = [B.sbuf(f"ost{i}", [64, 512], BF16) for i in range(2)]
        B.dma("pool", mb[:], cd["c_mbig_fox"][:, :, :], [], ["mb"], "c0")
        B.dma("sp", ed[:], cd["c_edenom"][:, :], [], ["ed"], "c2")
        for i in range(2):
            B.dve(lambda e, i=i: e.memset(V[i][:, :, 64:65], 1.0), [], [("Vone", i)])

        def load_head(h):
            b = h % 2
            B.dma("sp", Ka[b][:], KfA[h, :, :], [], [("Ka", b)], ("Ka", b))
            B.dma("sp", Qa[b][:], QfA[h, :, :], [], [("Qa", b)], ("Qa", b))
            B.dma("sp", V[b][:, :, 0:64], Vtok[:, h * 64:(h + 1) * 64].rearrange("(kt p) d -> p kt d", p=128),
                  [], [("V", b)], ("V", b))

        items = []
        cnt = [0, 0]

        def mk(h, qb, kt, nk):
            b = h % 2
            pb = cnt[0] % NS
            cnt[0] += 1
            grp = h * 8 + qb
            po = 4 + (grp % 2)
            so = grp % 2
            diag = kt >= 4 * qb

            def front():
                B.pe(lambda e: e.matmul(ps[pb][:], Ka[b][:, kt * 128:(kt + 1) * 128], Qa[b][:, qb * 512:(qb + 1) * 512],
                                        start=True, stop=not diag), [("Ka", b), ("Qa", b)], [("ps", pb)])
                if diag:
                    B.pe(lambda e: e.matmul(ps[pb][:], ident[:], mb[:, kt - 4 * qb, :], start=False, stop=True),
                         ["mb"], [("ps", pb)])

            def back():
                if kt == 0 and qb == 0 and h + 1 < 6:
                    load_head(h + 1)
                B.act(lambda e: e.activation(out=pT[pb][:], in_=ps[pb][:], func=AF.Exp), [("ps", pb)], [("pT", pb)])
                B.pe(lambda e: e.matmul(ps[po][0:65, :], V[b][:, kt, :], pT[pb][:], start=(kt == 0), stop=(kt == nk - 1)),
                     [("V", b), ("Vone", b), ("pT", pb)], [("ps", po)])
                if kt == nk - 1:
                    B.act(lambda e: e.activation(out=osb[so][:], in_=ps[po][0:65, :], func=AF.Copy), [("ps", po)], [("osb", so)])
                    B.pe(lambda e: e.matmul(ps[6][0:64, :], ed[:], osb[so][:], start=True, stop=True), ["ed", ("osb", so)], [("ps", 6)])
                    B.dve(lambda e: e.reciprocal(out=rec[:], in_=ps[6][0:64, :]), [("ps", 6)], ["rec"])
                    B.dve(lambda e: e.tensor_tensor(out=ost[so][:], in0=osb[so][0:64, :], in1=rec[:], op=ALU.mult),
                          [("osb", so), "rec"], [("ost", so)])
                    B.dma("sp", OT[h * 64:(h + 1) * 64, qb * 512:(qb + 1) * 512], ost[so][:], [("ost", so)], [], ("ost", so))

            return (front, back)

        load_head(0)
        for h in range(6):
            for qb in range(8):
                nk = 4 * (qb + 1)
                for kt in range(nk):
                    items.append(mk(h, qb, kt, nk))
        run_pipeline(items, NS - 1)
        B.end_phase()

    def sb_phase():
        Kt = [B.sbuf(f"Kt{i}", [72, S], BF16) for i in range(2)]
        Qt = [B.sbuf(f"Qt{i}", [72, S], BF16) for i in range(2)]
        V = [B.sbuf(f"V{i}", [128, NKT, 72], BF16) for i in range(2)]
        mb = B.sbuf("mb", [128, 4, 512], BF16)
        m01 = B.sbuf("m01", [128, 4, 512], BF16)
        esel = B.sbuf("esel", [128, 32, 72], BF16)
        nsel = B.sbuf("nsel", [72, 32, 128], BF16)
        spT = [B.sbuf(f"spT{i}", [128, NKT, 512], BF16) for i in range(2)]
        e32 = [B.sbuf(f"e32{i}", [128, 512], F32) for i in range(3)]
        csh = [B.sbuf(f"csh{i}", [72, 512], BF16) for i in range(2)]
        for i in range(2):
            B.dve(lambda e, i=i: e.memset(Kt[i][64:72, :], 0.0), [], [("Kz", i)])
            B.dve(lambda e, i=i: e.memset(Qt[i][64:72, :], 0.0), [], [("Qz", i)])
            B.dve(lambda e, i=i: e.memset(V[i][:, :, 64:72], 0.0), [], [("Vz", i)])
            B.dve(lambda e, i=i: e.memset(csh[i][64:72, :], 0.0), [], [("cz", i)])
        hi2 = B.sbuf("hi2", [64, 512], BF16)
        aT = [B.sbuf(f"aT{i}", [128, 512], BF16) for i in range(3)]
        ost = [B.sbuf(f"ost{i}", [64, 512], BF16) for i in range(2)]
        B.dma("pool", mb[:], cd["c_mbig_sb"][:, :, :], [], ["mb"], "c0")
        B.dma("pool", m01[:], cd["c_m01_sb"][:, :, :], [], ["m01"], "c0")
        B.dma("pool", esel[:], cd["c_esel2"][:, :, :], [], ["esel"], "c0")
        B.dma("pool", nsel[:], cd["c_negsel"][:, :, :], [], ["nsel"], "c0")
        B.group(["mb", "m01", "esel", "nsel"], "c0")

        def load_head(h):
            b = h % 2
            B.dma("sp", Kt[b][0:64, :], KsT[h, :, :], [], [("Kt", b)], ("Kt", b))
            B.dma("sp", Qt[b][0:64, :], QsT[h, :, :], [], [("Qt", b)], ("Qt", b))
            B.dma("sp", V[b][:, :, 0:64], Vtok[:, 384 + h * 64:384 + (h + 1) * 64].rearrange("(kt p) d -> p kt d", p=128),
                  [], [("V", b)], ("V", b))

        cnt = [0, 0]

        def mk_p1(h, qb, kt, nk, grp):
            b = h % 2
            pb = cnt[0] % 3
            cnt[0] += 1
            gs = grp % 2
            qs = slice(qb * 512, (qb + 1) * 512)

            def front():
                B.pe(lambda e: e.matmul(ps[pb][:], Kt[b][:, kt * 128:(kt + 1) * 128], Qt[b][:, qs], start=True, stop=True),
                     [("Kt", b), ("Qt", b), ("Kz", b), ("Qz", b)], [("ps", pb)])

            def mid():
                B.act(lambda e: e.activation(out=e32[pb][:], in_=ps[pb][:], func=AF.Exp), [("ps", pb)], [("e32", pb)])

            def back():
                B.act(lambda e: e.activation(out=spT[gs][:, kt, :], in_=e32[pb][:], func=AF.Ln, bias=oneb[:, 0:1], scale=1.0),
                      [("e32", pb)], [("sp", gs, kt)])
                if kt >= 4 * qb:
                    B.dve(lambda e: e.tensor_tensor(out=spT[gs][:, kt, :], in0=spT[gs][:, kt, :], in1=m01[:, kt - 4 * qb, :], op=ALU.mult),
                          [("sp", gs, kt), "m01"], [("sp", gs, kt)])
                B.pe(lambda e: e.matmul(ps[3][0:72, :], esel[:, kt, :], spT[gs][:, kt, :], start=(kt == 0), stop=(kt == nk - 1)),
                     ["esel", ("sp", gs, kt)], [("ps", 3)])
                if kt == nk - 1:
                    B.dve(lambda e: e.tensor_copy(out=csh[gs][0:32, :], in_=ps[3][0:32, :]), [("ps", 3)], [("csh0", gs)])
                    B.dve(lambda e: e.tensor_copy(out=hi2[32:64, :], in_=ps[3][32:64, :]), [("ps", 3)], ["hi2"])
                    B.dve(lambda e: e.tensor_tensor(out=csh[gs][32:64, :], in0=ps[3][32:64, :], in1=hi2[32:64, :], op=ALU.subtract),
                          [("ps", 3), "hi2"], [("csh1", gs)])

            return (front, mid, back)

        def mk_p2(h, qb, kt, nk, grp):
            b = h % 2
            pl = 4 + (cnt[1] % 3)
            sl = cnt[1] % 3
            cnt[1] += 1
            gs = grp % 2
            so = grp % 2
            qs = slice(qb * 512, (qb + 1) * 512)
            diag = kt >= 4 * qb

            def front():
                B.pe(lambda e: e.matmul(ps[pl][:], Kt[b][:, kt * 128:(kt + 1) * 128], Qt[b][:, qs], start=True, stop=False),
                     [("Kt", b), ("Qt", b), ("Kz", b), ("Qz", b)], [("ps", pl)])
                B.pe(lambda e: e.matmul(ps[pl][:], negtri[:], spT[gs][:, kt, :], start=False, stop=False),
                     [("sp", gs, kt)], [("ps", pl)])
                B.pe(lambda e: e.matmul(ps[pl][:], nsel[:, kt, :], csh[gs][:], start=False, stop=not diag),
                     ["nsel", ("csh0", gs), ("csh1", gs), ("cz", gs)], [("ps", pl)])
                if diag:
                    B.pe(lambda e: e.matmul(ps[pl][:], ident[:], mb[:, kt - 4 * qb, :], start=False, stop=True),
                         ["mb"], [("ps", pl)])

            def back():
                B.act(lambda e: e.activation(out=aT[sl][:], in_=ps[pl][:], func=AF.Exp), [("ps", pl)], [("aT", sl)])
                B.pe(lambda e: e.matmul(ps[7][0:72, :], V[b][:, kt, :], aT[sl][:], start=(kt == 0), stop=(kt == nk - 1)),
                     [("V", b), ("Vz", b), ("aT", sl)], [("ps", 7)])
                if kt == nk - 1:
                    B.act(lambda e: e.activation(out=ost[so][:], in_=ps[7][0:64, :], func=AF.Copy), [("ps", 7)], [("ost", so)])
                    B.dma("sp", OT[384 + h * 64:384 + (h + 1) * 64, qs], ost[so][:], [("ost", so)], [], ("ost", so))
                    if qb == 7 and h + 2 < 6:
                        load_head(h + 2)

            return (front, nop, back)

        groups = [(h, qb) for h in range(6) for qb in range(8)]
        items = []

        def add_p1(gi):
            h, qb = groups[gi]
            nk = 4 * (qb + 1)
            for kt in range(nk):
                items.append(mk_p1(h, qb, kt, nk, gi))

        def add_p2(gi):
            h, qb = groups[gi]
            nk = 4 * (qb + 1)
            for kt in range(nk):
                items.append(mk_p2(h, qb, kt, nk, gi))

        load_head(0)
        load_head(1)
        add_p1(0)
        for gi in range(1, len(groups)):
            add_p1(gi)
            add_p2(gi - 1)
        add_p2(len(groups) - 1)
        run_pipeline3(items)
        B.end_phase()

    def dsa_phase():
        Kc = B.sbuf("Kc", [64, 4, S], BF16)
        Ki = B.sbuf("Ki", [64, S], BF16)
        V = B.sbuf("V", [128, NKT, 4, 65], BF16)
        Qc = [B.sbuf(f"Qc{i}", [64, 4, 512], BF16) for i in range(2)]
        Qi = [B.sbuf(f"Qi{i}", [64, 4, 512], BF16) for i in range(2)]
        wi = [B.sbuf(f"wi{i}", [128, 4, 4], F32) for i in range(2)]
        score = B.sbuf("score", [128, 4, S], F32)
        Mc = B.sbuf("Mc", [128, 4, S], BF16)
        junk = B.sbuf("junk", [128, S], BF16)
        junk2 = B.sbuf("junk2", [128, S], BF16)
        rl = [B.sbuf(f"rl{i}", [128, 512], F32) for i in range(2)]
        cz = B.sbuf("cz", [128, 128], F32)
        ed = B.sbuf("ed", [65, 64], F32)
        mx = B.sbuf("mx", [128, 4], F32)
        mn = B.sbuf("mn", [128, 4], F32)
        w0 = B.sbuf("w0", [128, 4], F32)
        lo = B.sbuf("lo", [128, 4], F32)
        mid = B.sbuf("mid", [128, 4], F32)
        cnt8 = B.sbuf("cnt8", [128, 8], F32)
        tot = B.sbuf("tot", [128, 4], F32)
        thrv = B.sbuf("thrv", [128, 4], F32)
        prd = B.sbuf("prd", [128, 4], F32)
        pT = [B.sbuf(f"pT{i}", [128, 512], BF16) for i in range(3)]
        osb = [B.sbuf(f"osb{i}", [65, 512], F32) for i in range(2)]
        rec = B.sbuf("rec", [64, 512], F32)
        ost = [B.sbuf(f"ost{i}", [64, 512], BF16) for i in range(2)]
        B.dma("sp", cz[:], cd["c_causal"][:, :], [], ["cz"], "c2")
        B.dma("sp", ed[:], cd["c_edenom"][:, :], [], ["ed"], "c2")
        B.group(["cz", "ed"], "c2")
        for h in range(4):
            B.dma("sp", Kc[:, h, :], KcT[h, :, :], [], [("Kc", h)], "Kc")
        B.group([("Kc", h) for h in range(4)], "Kc")
        B.dma("sp", Ki[:], KiT[:, :], [], ["Ki"], "Ki")
        B.dve(lambda e: e.memset(V[:, :, :, 64:65], 1.0), [], ["Vone"])
        for h in range(4):
            B.dma("sp", V[:, :, h, 0:64], Vtok[:, 768 + h * 64:768 + (h + 1) * 64].rearrange("(kt p) d -> p kt d", p=128),
                  [], [("V", h)], "V")
        B.group([("V", h) for h in range(4)], "V")

        def load_q(qb):
            b = qb % 2
            qs = slice(qb * 512, (qb + 1) * 512)
            for h in range(4):
                B.dma("sp", Qc[b][:, h, :], QcT[h, :, qs], [], [("Qc", b, h)], ("Qc", b))
                B.dma("sp", Qi[b][:, h, :], QiT[h, :, qs], [], [("Qi", b, h)], ("Qi", b))
            B.group([("Qc", b, h) for h in range(4)], ("Qc", b))
            B.group([("Qi", b, h) for h in range(4)], ("Qi", b))
            B.dma("sp", wi[b][:], WiD[qs, :].rearrange("(s p) n -> p s n", p=128), [], [("wi", b)], ("wi", b), slow=True)

        cn = [0, 0, 0]

        def idx_item(qb, g, kc, h, wdt):
            b = qb % 2
            ks = slice(kc * 512, kc * 512 + wdt)
            pb = cn[0] % 2
            cn[0] += 1
            r = cn[1] % 2
            cn[1] += 1

            def front():
                B.pe(lambda e: e.matmul(ps[pb][:, 0:wdt], Qi[b][:, h, g * 128:(g + 1) * 128], Ki[:, ks], start=True, stop=True),
                     [("Qi", b, h), "Ki"], [("ps", pb)])

            def back():
                B.act(lambda e: e.activation(out=rl[r][:, 0:wdt], in_=ps[pb][:, 0:wdt], func=AF.Relu), [("ps", pb)], [("rl", r)])
                if h == 0:
                    B.dve(lambda e: e.tensor_scalar(out=score[:, g, ks], in0=rl[r][:, 0:wdt], scalar1=wi[b][:, g, 0:1], scalar2=None,
                                                    op0=ALU.mult), [("rl", r), ("wi", b)], [("score", g)])
                else:
                    B.dve(lambda e: e.scalar_tensor_tensor(out=score[:, g, ks], in0=rl[r][:, 0:wdt], scalar=wi[b][:, g, h:h + 1],
                                                           in1=score[:, g, ks], op0=ALU.mult, op1=ALU.add),
                          [("rl", r), ("wi", b), ("score", g)], [("score", g)])

            return (front, back)

        def att_item(qb, h, kt, nk):
            b = qb % 2
            pl = 3 + (cn[2] % 3)
            sl = cn[2] % 3
            cn[2] += 1
            po = 6 + (h % 2)
            so = h % 2
            gmin = max(0, kt - 4 * qb)

            def front():
                B.pe(lambda e: e.matmul(ps[pl][:], Kc[:, h, kt * 128:(kt + 1) * 128], Qc[b][:, h, :], start=True, stop=False),
                     [("Kc", h), ("Qc", b, h)], [("ps", pl)])
                if gmin > 0:
                    B.pe(lambda e: e.matmul(ps[pl][:, 0:gmin * 128], ident[:], mbz[:, 0:gmin * 128], start=False, stop=False),
                         [], [("ps", pl)])
                for g in range(gmin, 4):
                    B.pe(lambda e, g=g: e.matmul(ps[pl][:, g * 128:(g + 1) * 128], Mc[:, g, kt * 128:(kt + 1) * 128], nbident[:],
                                                 start=False, stop=(g == 3)), [("Mc", g)], [("ps", pl)])

            def back():
                B.act(lambda e: e.activation(out=pT[sl][:], in_=ps[pl][:], func=AF.Exp), [("ps", pl)], [("pT", sl)])
                B.pe(lambda e: e.matmul(ps[po][0:65, :], V[:, kt, h, :], pT[sl][:], start=(kt == 0), stop=(kt == nk - 1)),
                     [("V", h), "Vone", ("pT", sl)], [("ps", po)])
                if kt == nk - 1:
                    B.act(lambda e: e.activation(out=osb[so][:], in_=ps[po][0:65, :], func=AF.Copy), [("ps", po)], [("osb", so)])
                    B.pe(lambda e: e.matmul(ps[2][0:64, :], ed[:], osb[so][:], start=True, stop=True), ["ed", ("osb", so)], [("ps", 2)])
                    B.dve(lambda e: e.reciprocal(out=rec[:], in_=ps[2][0:64, :]), [("ps", 2)], ["rec"])
                    B.dve(lambda e: e.tensor_tensor(out=ost[so][:], in0=osb[so][0:64, :], in1=rec[:], op=ALU.mult),
                          [("osb", so), "rec"], [("ost", so)])
                    B.dma("sp", OT[768 + h * 64:768 + (h + 1) * 64, qb * 512:(qb + 1) * 512], ost[so][:], [("ost", so)], [], ("ost", so))

            return (front, back)

        def count_pass(qb, g):
            kmax = (qb * 4 + g + 1) * 128
            cD = max(128, int(round(kmax * 0.47 / 128.0)) * 128) if kmax > 128 else kmax
            nA = kmax - cD
            B.dve(lambda e: e.tensor_scalar(out=junk[:, 0:cD], in0=score[:, g, 0:cD], scalar1=mid[:, g:g + 1], scalar2=0.0,
                                            op0=ALU.is_ge, op1=ALU.add, accum_out=cnt8[:, g:g + 1]),
                  [("score", g), "mid", "cnt8"], ["junk", ("cntD", g)])
            if nA > 0:
                B.act(lambda e: e.activation(out=junk2[:, cD:kmax], in_=score[:, g, cD:kmax], func=AF.Sign, bias=mid[:, g:g + 1],
                                             scale=-1.0, accum_out=cnt8[:, 4 + g:5 + g]),
                      [("score", g), "mid", "cnt8"], ["junk2", ("cntA", g)])
            return nA

        load_q(0)
        for qb in range(8):
            if qb + 1 < 8:
                load_q(qb + 1)
            for g in range(4):
                qt = qb * 4 + g
                kmax = (qt + 1) * 128
                nch = (kmax + 511) // 512
                its = []
                for kc in range(nch):
                    wdt = min(512, kmax - kc * 512)
                    for h in range(4):
                        its.append(idx_item(qb, g, kc, h, wdt))
                run_pipeline(its, 1)
                B.dve(lambda e, g=g, kmax=kmax: e.tensor_reduce(out=mx[:, g:g + 1], in_=score[:, g, 0:kmax], axis=AX.X, op=ALU.max),
                      [("score", g)], ["mx"])
                B.dve(lambda e, g=g, kmax=kmax: e.tensor_reduce(out=mn[:, g:g + 1], in_=score[:, g, 0:kmax], axis=AX.X, op=ALU.min),
                      [("score", g)], ["mn"])
                B.dve(lambda e, g=g, kmax=kmax: e.tensor_tensor(out=score[:, g, kmax - 128:kmax], in0=score[:, g, kmax - 128:kmax],
                                                                in1=cz[:], op=ALU.add), [("score", g), "cz"], [("score", g)])
            B.dve(lambda e: e.tensor_tensor(out=w0[:], in0=mx[:], in1=mn[:], op=ALU.subtract), ["mx", "mn"], ["w0"])
            B.dve(lambda e: e.tensor_scalar(out=w0[:], in0=w0[:], scalar1=1.0001, scalar2=1e-6, op0=ALU.mult, op1=ALU.add), ["w0"], ["w0"])
            B.dve(lambda e: e.tensor_copy(out=lo[:], in_=mn[:]), ["mn"], ["lo"])
            for g in range(4):
                kmax = (qb * 4 + g + 1) * 128
                cD = max(128, int(round(kmax * 0.47 / 128.0)) * 128) if kmax > 128 else kmax
                nA = kmax - cD
                B.dve(lambda e, g=g, nA=nA: e.memset(thrv[:, g:g + 1], float(TOPK) - 0.5 * nA), [], ["thrv"])
            for s_ in range(NBIS):
                hf = 0.5 ** (s_ + 1)
                B.dve(lambda e, hf=hf: e.scalar_tensor_tensor(out=mid[:], in0=w0[:], scalar=hf, in1=lo[:], op0=ALU.mult, op1=ALU.add),
                      ["w0", "lo"], ["mid"])
                B.dve(lambda e: e.memset(cnt8[:], 0.0), [("cntD", g) for g in range(4)] + [("cntA", g) for g in range(4)], ["cnt8"])
                for g in (3, 2, 1, 0):
                    count_pass(qb, g)
                B.dve(lambda e: e.scalar_tensor_tensor(out=tot[:], in0=cnt8[:, 4:8], scalar=-0.5, in1=cnt8[:, 0:4], op0=ALU.mult, op1=ALU.add),
                      [("cntD", g) for g in range(4)] + [("cntA", g) for g in range(4)] + ["cnt8"], ["tot"])
                B.dve(lambda e: e.tensor_tensor(out=prd[:], in0=tot[:], in1=thrv[:], op=ALU.is_ge), ["tot", "thrv"], ["prd"])
                B.dve(lambda e: e.tensor_tensor(out=prd[:], in0=prd[:], in1=w0[:], op=ALU.mult), ["prd", "w0"], ["prd"])
                B.dve(lambda e, hf=hf: e.scalar_tensor_tensor(out=lo[:], in0=prd[:], scalar=hf, in1=lo[:], op0=ALU.mult, op1=ALU.add),
                      ["prd", "lo"], ["lo"])
            for g in range(4):
                kmax = (qb * 4 + g + 1) * 128
                B.dve(lambda e, g=g, kmax=kmax: e.tensor_scalar(out=Mc[:, g, 0:kmax], in0=score[:, g, 0:kmax], scalar1=lo[:, g:g + 1],
                                                                scalar2=None, op0=ALU.is_lt), [("score", g), "lo"], [("Mc", g)])
            nk = 4 * (qb + 1)
            its = []
            for h in range(4):
                for kt in range(nk):
                    its.append(att_item(qb, h, kt, nk))
            run_pipeline(its, 2)
        B.end_phase()

    def out_phase(l, xsrc):
        gi = l * 3 + 1
        Wgt = B.sbuf("Wgt", [128, NDC, 3 * D], BF16)
        Wbr = B.sbuf("Wbr", [128, NDC, D], BF16)
        Wo = B.sbuf("Wo", [128, NDC, D], BF16)
        xt = [B.sbuf(f"xt{i}", [128, NDC, TT], F32) for i in range(2)]
        ot = [B.sbuf(f"ot{i}", [128, NDC, TT], BF16) for i in range(2)]
        hT = B.sbuf("hT", [128, NDC, TT], BF16)
        sq = B.sbuf("sq", [128, NDC, TT], BF16)
        rstd = B.sbuf("rstd", [128, TT], F32)
        sg = [B.sbuf(f"sg{i}", [128, TT], F32) for i in range(2)]
        macc = B.sbuf("macc", [128, TT], F32)
        mt = B.sbuf("mt", [128, TT], F32)
        mrg = B.sbuf("mrg", [128, NDC, TT], BF16)
        for c in range(NDC):
            B.dma("pool", Wgt[:, c, :], w_in[l, c * 128:(c + 1) * 128, C_G:NIN], [], [("Wgt", c)], "w0")
            B.dma("pool", Wo[:, c, :], w_out[l, c * 128:(c + 1) * 128, :], [], [("Wo", c)], "w1")
        brch = [(0, 0), (0, 1), (0, 2), (1, 0), (1, 1), (1, 2), (2, 0), (2, 1)]
        for c, (br, j) in enumerate(brch):
            B.dma("pool", Wbr[:, c, :], w_br[br][l, j * 128:(j + 1) * 128, :], [], [("Wbr", c)], "w2")
        B.group([("Wgt", c) for c in range(NDC)], "w0")
        B.group([("Wo", c) for c in range(NDC)], "w1")
        B.group([("Wbr", c) for c in range(NDC)], "w2")

        def load(i):
            t0 = i * TT
            B.dma("sp", xt[i % 2][:], xsrc[:, t0:t0 + TT].rearrange("(c p) t -> p c t", p=128), [], [("xt", i % 2)], ("xt", i % 2))
            B.dma("sp", ot[i % 2][:], OT[:, t0:t0 + TT].rearrange("(c p) t -> p c t", p=128), [], [("ot", i % 2)], ("ot", i % 2))

        load(0)
        k = 0
        for i in range(NTT):
            if i + 1 < NTT:
                load(i + 1)
            X = xt[i % 2]
            xk = ("xt", i % 2)
            O = ot[i % 2]
            ok = ("ot", i % 2)
            rmsnorm_tile(X, xk, gi, hT, sq, rstd)
            brc = ((0, 1, 2), (3, 4, 5), (6, 7))
            for m in range(NDC):
                for br in range(3):
                    pg = 1 + (k % 2)
                    pbb = 3 + (k % 2)
                    sgi = k % 2
                    k += 1
                    col = br * D + m * 128
                    for c in range(NDC):
                        B.pe(lambda e, c=c, col=col, pg=pg: e.matmul(ps[pg][:], Wgt[:, c, col:col + 128], hT[:, c, :],
                                                                      start=(c == 0), stop=(c == NDC - 1)),
                             [("Wgt", c), ("hT", c)], [("ps", pg)])
                    cs_ = brc[br]
                    for ci, c in enumerate(cs_):
                        B.pe(lambda e, c=c, ci=ci, m=m, pbb=pbb, n_=len(cs_), O=O: e.matmul(ps[pbb][:], Wbr[:, c, m * 128:(m + 1) * 128], O[:, c, :],
                                                                                            start=(ci == 0), stop=(ci == n_ - 1)),
                             [("Wbr", c), ok], [("ps", pbb)])
                    B.act(lambda e, pg=pg, sgi=sgi, br=br, m=m: e.activation(out=sg[sgi][:], in_=ps[pg][:], func=AF.Sigmoid,
                                                                            bias=bgate[:, l, br, m:m + 1], scale=1.0),
                          [("ps", pg)], [("sg", sgi)])
                    if br == 0:
                        B.dve(lambda e, pbb=pbb, sgi=sgi: e.tensor_tensor(out=macc[:], in0=ps[pbb][:], in1=sg[sgi][:], op=ALU.mult),
                              [("ps", pbb), ("sg", sgi)], ["macc"])
                    else:
                        B.dve(lambda e, pbb=pbb, sgi=sgi: e.tensor_tensor(out=mt[:], in0=ps[pbb][:], in1=sg[sgi][:], op=ALU.mult),
                              [("ps", pbb), ("sg", sgi)], ["mt"])
                        if br == 1:
                            B.dve(lambda e: e.tensor_tensor(out=macc[:], in0=macc[:], in1=mt[:], op=ALU.add), ["macc", "mt"], ["macc"])
                        else:
                            B.dve(lambda e, m=m: e.tensor_tensor(out=mrg[:, m, :], in0=macc[:], in1=mt[:], op=ALU.add),
                                  ["macc", "mt"], [("mrg", m)])
            for e_ in range(NDC):
                py = 5 + (e_ % 2)
                for m in range(NDC):
                    B.pe(lambda e, e_=e_, m=m, py=py: e.matmul(ps[py][:], Wo[:, m, e_ * 128:(e_ + 1) * 128], mrg[:, m, :],
                                                               start=(m == 0), stop=(m == NDC - 1)),
                         [("Wo", m), ("mrg", m)], [("ps", py)])
                B.dve(lambda e, e_=e_, py=py, X=X: e.tensor_tensor(out=X[:, e_, :], in0=ps[py][:], in1=X[:, e_, :], op=ALU.add),
                      [("ps", py), xk], [xk])
            t0 = i * TT
            B.dma("sp", outT[:, t0:t0 + TT].rearrange("(c p) t -> p c t", p=128), X[:], [xk], [], ("xt", i % 2))
        B.end_phase()

    phase0()
    for l in range(DEPTH):
        if on(f"ffn1_{l}"):
            ffn_phase(l, 0, xT_in if l == 0 else outT, outT, l == 0)
        if on(f"proj_{l}"):
            proj_phase(l, outT)
        if on(f"fox_{l}"):
            fox_phase()
        if on(f"sb_{l}"):
            sb_phase()
        if on(f"dsa_{l}"):
            dsa_phase()
        if on(f"out_{l}"):
            out_phase(l, outT)
        if on(f"ffn2_{l}"):
            ffn_phase(l, 1, outT, outT, False)

    B.sc.close()
    for g in reversed(psg):
        g.__exit__(None, None, None)
    for g in reversed(B._glob):
        g.__exit__(None, None, None)
    return nc


_WNAMES = ("ffn1_norm", "ffn1_w_gate", "ffn1_w_up", "ffn1_w_down", "mix_norm", "w_in", "b_forget", "b_gates",
           "q_norm_fox", "k_norm_fox", "q_norm_sb", "k_norm_sb", "q_norm_dsa", "k_norm_dsa",
           "w_branch_fox", "w_branch_sb", "w_branch_dsa", "w_out",
           "ffn2_norm", "ffn2_w_gate", "ffn2_w_up", "ffn2_w_down")


def kernel(**inputs):
    n = 8
    phases = inputs.pop("_phases", ("all",))
    ncores = inputs.pop("_ncores", n)
    nc = build_program(phases)
    x = np.asarray(inputs["x"])
    pos = np.asarray(inputs["positions"]).astype(np.int32)
    shared = {k: np.ascontiguousarray(np.asarray(inputs[k], dtype=np.float32)) for k in _WNAMES}
    shared.update(_CONST)
    in_maps = []
    for b in range(ncores):
        m = dict(shared)
        m["xT"] = np.ascontiguousarray(x[b].T)
        m["pos"] = np.ascontiguousarray(pos[b:b + 1])
        in_maps.append(m)
    res = run_bass_kernel_spmd(nc, in_maps, core_ids=list(range(ncores)))
    if any(p.startswith("dbgOT") for p in phases):
        return res.results[0]["OT"]
    out = np.stack([np.ascontiguousarray(r["outT"].T) for r in res.results], axis=0)
    return out.astype(np.float32, copy=False)
```
